# Optimizing a Trainium2 kernel written in Bass

```python
import math
import jax, jax.numpy as jnp
from jax import lax
import numpy as np

D_MODEL = 1024
BATCH = 4
SEQ = 4096
DEPTH = 4

MIX_WIDTH = D_MODEL
ATTN_HEAD_DIM = 64
ATTN_WIDTH = D_MODEL // 2
N_ATTN_HEADS = ATTN_WIDTH // ATTN_HEAD_DIM
DILATED_PATTERNS = ((128, 1), (512, 4), (2048, 16))
N_MLSTM_HEADS = 4
MLSTM_WIDTH = MIX_WIDTH - ATTN_WIDTH
MLSTM_HEAD_DIM = MLSTM_WIDTH // N_MLSTM_HEADS
MLSTM_CHUNK = 128
MLSTM_CONV = 4
IN_COLS = 3 * ATTN_WIDTH + 4 * MLSTM_WIDTH + 2 * N_MLSTM_HEADS
N_BUCKETS = 32
MAX_DISTANCE = 2048
N_MEM = 256
N_XHEADS = 4
XHEAD_DIM = D_MODEL // N_XHEADS
D_FF = 2816
FFN_CONV = 3
EPS = 1e-6

kernel_name = "hybrid_dilattn_mlstm_memxattn_convffn"


def rmsnorm(x, g):
    x32 = x.astype(jnp.float32)
    y = x32 * lax.rsqrt(jnp.mean(x32 * x32, axis=-1, keepdims=True) + EPS)
    return (y * g.astype(jnp.float32)).astype(x.dtype)


def causal_dwconv(x, w, b):
    K = w.shape[0]
    S = x.shape[1]
    xp = jnp.pad(x, ((0, 0), (K - 1, 0), (0, 0)))
    y = b + xp[:, 0:S] * w[0]
    for j in range(1, K):
        y = y + xp[:, j:j + S] * w[j]
    return y


def t5_causal_bucket(dist):
    max_exact = N_BUCKETS // 2
    d_f = jnp.maximum(dist, 1).astype(jnp.float32)
    large = max_exact + (jnp.log(d_f / max_exact) / math.log(MAX_DISTANCE / max_exact)
                         * (N_BUCKETS - max_exact)).astype(jnp.int32)
    large = jnp.minimum(large, N_BUCKETS - 1)
    return jnp.where(dist < max_exact, dist, large)


def dilated_window_attention(q, k, v, bias_sub, dil):
    B, H, S, Dh = q.shape
    n = bias_sub.shape[1] - 1
    blk = n
    span = dil * blk
    L = -(-S // span) * span
    M = L // dil
    nb = M // blk

    def to_blocks(t):
        t = jnp.pad(t, ((0, 0), (0, 0), (0, L - S), (0, 0)))
        t = t.reshape(B, H, M, dil, Dh).swapaxes(2, 3)
        return t.reshape(B, H, dil, nb, blk, Dh)

    def with_prev(t):
        prev = jnp.pad(t, ((0, 0), (0, 0), (0, 0), (1, 0), (0, 0), (0, 0)))[:, :, :, :nb]
        return jnp.concatenate([prev, t], axis=-2)

    qb = to_blocks(q)
    kw = with_prev(to_blocks(k))
    vw = with_prev(to_blocks(v))
    logits = jnp.einsum("bhcnqd,bhcnkd->bhcnqk", qb, kw) * (Dh ** -0.5)
    qi = jnp.arange(blk)[:, None]
    kj = jnp.arange(2 * blk)[None, :]
    dist = qi + blk - kj
    band = (dist >= 0) & (dist <= n)
    bias = bias_sub[:, jnp.clip(dist, 0, n)]
    has_prev = (jnp.arange(nb)[:, None, None] > 0) | (kj[None] >= blk)
    valid = band[None] & has_prev
    logits = jnp.where(valid, logits + bias[:, None, None], -jnp.inf)
    m = jnp.max(logits, axis=-1, keepdims=True)
    p = jnp.exp(logits - m)
    s = jnp.sum(p, axis=-1, keepdims=True)
    o = jnp.einsum("bhcnqk,bhcnkd->bhcnqd", p, vw) / s
    lse = (m + jnp.log(s))[..., 0]
    o = o.reshape(B, H, dil, M, Dh).swapaxes(2, 3).reshape(B, H, L, Dh)[:, :, :S]
    lse = lse.reshape(B, H, dil, M).swapaxes(2, 3).reshape(B, H, L)[:, :, :S]
    return o, lse


def mlstm_chunkwise(q, k, v, i_pre, f_pre):
    B, H, S, Dh = q.shape
    nc = S // MLSTM_CHUNK

    def cs(t):
        return t.reshape((B, H, nc, MLSTM_CHUNK) + t.shape[3:])

    qc, kc, vc = cs(q), cs(k), cs(v)
    ig = cs(i_pre)
    b = jnp.cumsum(cs(jax.nn.log_sigmoid(f_pre)), axis=-1)
    g = b[..., -1]
    w = g[..., None] - b + ig

    def step(carry, inp):
        C, n, m = carry
        k_c, v_c, w_c, g_c = inp
        m_new = jnp.maximum(g_c + m, jnp.max(w_c, axis=-1))
        decay = jnp.exp(g_c + m - m_new)
        wt = jnp.exp(w_c - m_new[..., None])
        C_new = decay[..., None, None] * C + jnp.einsum("bhs,bhsv,bhsk->bhvk", wt, v_c, k_c)
        n_new = decay[..., None] * n + jnp.einsum("bhs,bhsk->bhk", wt, k_c)
        return (C_new, n_new, m_new), (C, n, m)

    init = (jnp.zeros((B, H, Dh, Dh), jnp.float32),
            jnp.zeros((B, H, Dh), jnp.float32),
            jnp.zeros((B, H), jnp.float32))
    lead = lambda t: jnp.moveaxis(t, 2, 0)
    _, (C_prev, n_prev, m_prev) = lax.scan(step, init, (lead(kc), lead(vc), lead(w), lead(g)))
    C_prev = jnp.moveaxis(C_prev, 0, 2)
    n_prev = jnp.moveaxis(n_prev, 0, 2)
    m_prev = jnp.moveaxis(m_prev, 0, 2)

    t_idx = jnp.arange(MLSTM_CHUNK)
    causal = t_idx[:, None] >= t_idx[None, :]
    D = jnp.where(causal, b[..., :, None] - b[..., None, :] + ig[..., None, :], -jnp.inf)
    a = b + m_prev[..., None]
    m_t = jnp.maximum(a, jnp.max(D, axis=-1))
    P = jnp.exp(D - m_t[..., None]) * jnp.einsum("bhntd,bhnsd->bhnts", qc, kc)
    inter = jnp.exp(a - m_t)
    num = (inter[..., None] * jnp.einsum("bhnvk,bhntk->bhntv", C_prev, qc)
           + jnp.einsum("bhnts,bhnsv->bhntv", P, vc))
    den = inter * jnp.einsum("bhnk,bhntk->bhnt", n_prev, qc) + jnp.sum(P, axis=-1)
    h = num / jnp.maximum(jnp.abs(den), jnp.exp(-m_t))[..., None]
    return h.reshape(B, H, S, Dh)


def hybrid_mixer(h, rel_bias, w_in, mconv_w, mconv_b, b_ig, b_fg, g_attn_out, g_mlstm_out, w_out):
    B, S, _ = h.shape
    f32 = jnp.float32
    proj = h @ w_in
    sizes = [ATTN_WIDTH] * 3 + [MLSTM_WIDTH] * 4 + [N_MLSTM_HEADS] * 2
    idx = [int(s) for s in np.cumsum(sizes)[:-1]]
    qa, ka, va, qm, km, vm, om, ig, fg = jnp.split(proj, idx, axis=-1)

    def heads(t, nh):
        return t.reshape(B, S, nh, -1).transpose(0, 2, 1, 3)

    qa_ = heads(qa, N_ATTN_HEADS).astype(f32)
    ka_ = heads(ka, N_ATTN_HEADS).astype(f32)
    va_ = heads(va, N_ATTN_HEADS).astype(f32)
    outs, lses = [], []
    for window, dil in DILATED_PATTERNS:
        n = window // dil
        bias_sub = rel_bias[t5_causal_bucket(jnp.arange(n + 1) * dil)].T.astype(f32)
        o, l = dilated_window_attention(qa_, ka_, va_, bias_sub, dil)
        outs.append(o)
        lses.append(l)
    mix_w = jax.nn.softmax(jnp.stack(lses), axis=0)
    attn = jnp.einsum("pbhs,pbhsd->bhsd", mix_w, jnp.stack(outs))
    attn = attn.transpose(0, 2, 1, 3).reshape(B, S, ATTN_WIDTH).astype(h.dtype)

    qk = jax.nn.silu(causal_dwconv(jnp.concatenate([qm, km], axis=-1), mconv_w, mconv_b))
    qm, km = jnp.split(qk, 2, axis=-1)
    q = heads(qm, N_MLSTM_HEADS).astype(f32)
    k = heads(km, N_MLSTM_HEADS).astype(f32) * (MLSTM_HEAD_DIM ** -0.5)
    v = heads(vm, N_MLSTM_HEADS).astype(f32)
    i_pre = (ig + b_ig).astype(f32).transpose(0, 2, 1)
    f_pre = (fg + b_fg).astype(f32).transpose(0, 2, 1)
    hm = mlstm_chunkwise(q, k, v, i_pre, f_pre)
    hm = jax.nn.sigmoid(om.astype(f32)) * hm.transpose(0, 2, 1, 3).reshape(B, S, MLSTM_WIDTH)

    mixed = jnp.concatenate([rmsnorm(attn, g_attn_out), rmsnorm(hm.astype(h.dtype), g_mlstm_out)], axis=-1)
    return mixed @ w_out


def memory_cross_attention(h, mem, wq, wk, wv, wo):
    B, S, D = h.shape
    f32 = jnp.float32
    q = (h @ wq).reshape(B, S, N_XHEADS, XHEAD_DIM).astype(f32)
    k = (mem @ wk).reshape(B, -1, N_XHEADS, XHEAD_DIM).astype(f32)
    v = (mem @ wv).reshape(B, -1, N_XHEADS, XHEAD_DIM).astype(f32)
    p = jax.nn.softmax(jnp.einsum("bshd,bmhd->bhsm", q, k) * (XHEAD_DIM ** -0.5), axis=-1)
    o = jnp.einsum("bhsm,bmhd->bshd", p, v).reshape(B, S, D).astype(h.dtype)
    return o @ wo


def conv_ffn(h, w_up, conv_w, conv_b, w_down):
    u = causal_dwconv(h @ w_up, conv_w, conv_b)
    a, g = jnp.split(u, 2, axis=-1)
    return (jax.nn.gelu(g, approximate=True) * a) @ w_down


def setup_inputs(seed: int = 0) -> dict:
    key = jax.random.key(seed)
    ks = jax.random.split(key, 32)
    f32 = jnp.float32

    def nrm(k, shape, scale):
        return scale * jax.random.normal(k, shape, f32)

    def gain(k, shape):
        return 1.0 + 0.02 * jax.random.normal(k, shape, f32)

    L, D = DEPTH, D_MODEL
    return {
        "x": nrm(ks[0], (BATCH, SEQ, D), 1.0),
        "mem": nrm(ks[1], (BATCH, N_MEM, D), 1.0),
        "rel_bias": nrm(ks[2], (N_BUCKETS, N_ATTN_HEADS), 0.2),
        "pre_mix_g": gain(ks[3], (L, D)),
        "w_in": nrm(ks[4], (L, D, IN_COLS), D ** -0.5),
        "mconv_w": nrm(ks[5], (L, MLSTM_CONV, 2 * MLSTM_WIDTH), MLSTM_CONV ** -0.5),
        "mconv_b": nrm(ks[6], (L, 2 * MLSTM_WIDTH), 0.02),
        "b_igate": nrm(ks[7], (L, N_MLSTM_HEADS), 0.1),
        "b_fgate": jnp.linspace(3.0, 6.0, N_MLSTM_HEADS, dtype=f32)[None] + nrm(ks[8], (L, N_MLSTM_HEADS), 0.1),
        "attn_out_g": gain(ks[9], (L, ATTN_WIDTH)),
        "mlstm_out_g": gain(ks[10], (L, MLSTM_WIDTH)),
        "w_out": nrm(ks[11], (L, MIX_WIDTH, D), MIX_WIDTH ** -0.5),
        "post_mix_g": gain(ks[12], (L, D)),
        "pre_mem_g": gain(ks[13], (L, D)),
        "wq_mem": nrm(ks[14], (L, D, D), D ** -0.5),
        "wk_mem": nrm(ks[15], (L, D, D), D ** -0.5),
        "wv_mem": nrm(ks[16], (L, D, D), D ** -0.5),
        "wo_mem": nrm(ks[17], (L, D, D), D ** -0.5),
        "post_mem_g": gain(ks[18], (L, D)),
        "pre_ffn_g": gain(ks[19], (L, D)),
        "w_up": nrm(ks[20], (L, D, 2 * D_FF), D ** -0.5),
        "fconv_w": nrm(ks[21], (L, FFN_CONV, 2 * D_FF), FFN_CONV ** -0.5),
        "fconv_b": nrm(ks[22], (L, 2 * D_FF), 0.02),
        "w_down": nrm(ks[23], (L, D_FF, D), D_FF ** -0.5),
        "post_ffn_g": gain(ks[24], (L, D)),
    }


def reference(x, mem, rel_bias, pre_mix_g, w_in, mconv_w, mconv_b, b_igate, b_fgate,
              attn_out_g, mlstm_out_g, w_out, post_mix_g, pre_mem_g, wq_mem, wk_mem,
              wv_mem, wo_mem, post_mem_g, pre_ffn_g, w_up, fconv_w, fconv_b, w_down,
              post_ffn_g):
    for l in range(DEPTH):
        h = hybrid_mixer(rmsnorm(x, pre_mix_g[l]), rel_bias, w_in[l], mconv_w[l], mconv_b[l],
                         b_igate[l], b_fgate[l], attn_out_g[l], mlstm_out_g[l], w_out[l])
        x = x + rmsnorm(h, post_mix_g[l])
        h = memory_cross_attention(rmsnorm(x, pre_mem_g[l]), mem, wq_mem[l], wk_mem[l],
                                   wv_mem[l], wo_mem[l])
        x = x + rmsnorm(h, post_mem_g[l])
        h = conv_ffn(rmsnorm(x, pre_ffn_g[l]), w_up[l], fconv_w[l], fconv_b[l], w_down[l])
        x = x + rmsnorm(h, post_ffn_g[l])
    return x
```

```python
import numpy as np
from contextlib import ExitStack
import concourse.bass as bass
import concourse.mybir as mybir
from concourse.bass_utils import run_bass_kernel_spmd
from concourse.alu_op_type import AluOpType as ALU

AF = mybir.ActivationFunctionType
AX = mybir.AxisListType
F32 = mybir.dt.float32
BF16 = mybir.dt.bfloat16

S = 4096
D = 1024
NL = 4
TT = 512
NTT = S // TT
EPS = 1e-6
IN_COLS = 3592
DFF = 2816
NFF = DFF // 128
NMEM = 256


class Buf:
    __slots__ = ("w", "r")

    def __init__(self):
        self.w = None
        self.r = {}


def bufs(n):
    return [Buf() for _ in range(n)]


class Ctx:
    def __init__(self, nc, es, n_dma_sems=40):
        self.nc = nc
        self.es = es
        self.eng = dict(pe=nc.tensor, dve=nc.vector, act=nc.scalar, pool=nc.gpsimd, sp=nc.sync)
        self.esem = {}
        self.cnt = {}
        self.nsem = 0
        for e in self.eng:
            self._new_esem(e)
        self.waited = {}
        self.dsems = [es.enter_context(nc.semaphore(f"dq{i}")) for i in range(n_dma_sems)]
        self.dval = [0] * n_dma_sems
        self.dnext = 0
        self.n_hw = n_dma_sems - 6
        self.dnext_sw = 0
        self.recent_dma = {}
        self.uid = 0
        self.ninstr = 0

    def _new_esem(self, e):
        self.nsem += 1
        self.esem[e] = self.es.enter_context(self.nc.semaphore(f"e{e}{self.nsem}"))
        self.cnt[e] = 0

    def name(self, p):
        self.uid += 1
        return f"{p}_{self.uid}"

    def sb(self, st, shape, dtype, name="t"):
        return st.enter_context(self.nc.sbuf_tensor(self.name(name), list(shape), dtype))

    def ps(self, st, shape=(128, 512), dtype=F32, name="ps"):
        return st.enter_context(self.nc.psum_tensor(self.name(name), list(shape), dtype))

    def _wait(self, e, tok, raw=False):
        sem, val, owner = tok
        if owner == e and (e == "pe" or e == "sp"):
            return
        key = (e, id(sem))
        if self.waited.get(key, 0) >= val:
            return
        self.waited[key] = val
        self.eng[e].wait_ge(sem, val)
        self.ninstr += 1

    def _deps(self, e, reads, writes):
        for b in reads:
            if b.w is not None:
                self._wait(e, b.w, raw=True)
        for b in writes:
            if b.w is not None:
                self._wait(e, b.w)
            for t in b.r.values():
                self._wait(e, t)

    def _mark(self, tok, reads, writes):
        for b in reads:
            k = id(tok[0])
            o = b.r.get(k)
            if o is None or o[1] < tok[1]:
                b.r[k] = tok
        for b in writes:
            b.w = tok
            b.r = {}

    def op(self, e, fn, reads=(), writes=(), inc=True):
        self._deps(e, reads, writes)
        ins = fn(self.eng[e])
        self.ninstr += 1
        if inc:
            if self.cnt[e] >= 30000:
                self._new_esem(e)
            self.cnt[e] += 1
            ins.then_inc(self.esem[e], 1)
            tok = (self.esem[e], self.cnt[e], e)
        else:
            if self.cnt[e] >= 30000:
                self._new_esem(e)
            tok = (self.esem[e], self.cnt[e] + 1, e)
        self._mark(tok, reads, writes)
        return tok

    def dma(self, q, out, in_, reads=(), writes=()):
        if q == "pool":
            i = self.n_hw + self.dnext_sw
            self.dnext_sw = (self.dnext_sw + 1) % (len(self.dsems) - self.n_hw)
        else:
            i = self.dnext
            self.dnext = (i + 1) % self.n_hw
        sem = self.dsems[i]
        old = self.dval[i]
        self._deps(q, reads, writes)
        if old > 0:
            self._wait(q, (sem, old, None))
        ins = self.eng[q].dma_start(out=out, in_=in_)
        self.ninstr += 1
        self.dval[i] = old + 16
        ins.then_inc(sem, 16)
        tok = (sem, old + 16, "dma")
        self.recent_dma[id(sem)] = tok
        self._mark(tok, reads, writes)
        return tok

    def barrier(self, engines=("pe", "dve", "act", "pool", "sp")):
        toks = [(self.esem[e], self.cnt[e], e) for e in self.eng if self.cnt[e] > 0]
        toks += list(self.recent_dma.values())
        for e in engines:
            for t in toks:
                self._wait(e, t, raw=True)
        self.recent_dma = {}


import math


def norm_pass(c, xT, hT, BhT, cst):
    nc = c.nc
    xv = xT.rearrange("(kc p) t -> p kc t", p=128)
    with ExitStack() as st:
        xt = [c.sb(st, (128, 8, TT), F32, "xt") for _ in range(2)]
        sq = [c.sb(st, (128, 8, TT), BF16, "sq") for _ in range(2)]
        rs = [c.sb(st, (128, TT), F32, "rs") for _ in range(2)]
        pss = [c.ps(st) for _ in range(2)]
        Bxt = bufs(2); Bsq = [bufs(8) for _ in range(2)]; Brs = bufs(2); Bps = bufs(2)
        for tt in range(NTT):
            b = tt % 2
            sl = slice(tt * TT, (tt + 1) * TT)
            c.dma("sp", out=xt[b][:], in_=xv[:, :, sl], writes=[Bxt[b]])
            for kc in range(8):
                c.op("act", lambda e: e.activation(out=sq[b][:, kc, :], in_=xt[b][:, kc, :], func=AF.Square),
                     reads=[Bxt[b]], writes=[Bsq[b][kc]])
            for kc in range(8):
                c.op("pe", lambda e: e.matmul(pss[b][:], lhsT=cst["avgD"][:], rhs=sq[b][:, kc, :], start=(kc == 0), stop=(kc == 7)),
                     reads=[Bsq[b][kc]], writes=[Bps[b]] if kc == 0 else [], inc=(kc == 7))
            Bps[b].w = (c.esem["pe"], c.cnt["pe"], "pe")
            c.op("act", lambda e: e.activation(out=rs[b][:], in_=pss[b][:], func=AF.Sqrt, bias=cst["epsc"][:, 0:1]),
                 reads=[Bps[b]], writes=[Brs[b]])
            c.op("dve", lambda e: e.reciprocal(out=rs[b][:], in_=rs[b][:]),
                 reads=[Brs[b]], writes=[Brs[b]])
            for kc in range(8):
                en = "dve" if kc % 2 == 0 else "pool"
                c.op(en, lambda e: e.tensor_tensor(out=hT[:, kc, sl], in0=xt[b][:, kc, :], in1=rs[b][:], op=ALU.mult),
                     reads=[Bxt[b], Brs[b]], writes=[BhT[tt][kc]])
    c.barrier()


def inproj(c, w_in, gcol, hT, BhT, projT, gatesT, Bproj):
    wv = w_in.rearrange("(kc p) n -> p kc n", p=128)
    with ExitStack() as st:
        ws = [c.sb(st, (128, 8, 128), F32, "ws") for _ in range(2)]
        wb = [c.sb(st, (128, 8, 128), BF16, "wb") for _ in range(2)]
        ob = [c.sb(st, (128, S), BF16, "ob") for _ in range(2)]
        og = c.sb(st, (8, S), F32, "og")
        pss = [c.ps(st) for _ in range(4)]
        Bws = bufs(2); Bwb = [bufs(8) for _ in range(2)]; Bob = [bufs(NTT) for _ in range(2)]; Bps = bufs(4)
        Bog = bufs(NTT)
        k = 0
        for fc in range(29):
            ncol = 128 if fc < 28 else 8
            b = fc % 2
            c.dma("sp", out=ws[b][:, :, :ncol], in_=wv[:, :, fc * 128: fc * 128 + ncol], writes=[Bws[b]])
            for kc in range(8):
                c.op("pool", lambda e: e.tensor_scalar(out=wb[b][:, kc, :ncol], in0=ws[b][:, kc, :ncol], scalar1=gcol[:, kc:kc + 1],
                                                       scalar2=None, op0=ALU.mult),
                     reads=[Bws[b]], writes=[Bwb[b][kc]])
            for tt in range(NTT):
                sl = slice(tt * TT, (tt + 1) * TT)
                p = k % 4
                for kc in range(8):
                    c.op("pe", lambda e: e.matmul(pss[p][:ncol, :], lhsT=wb[b][:, kc, :ncol], rhs=hT[:, kc, sl], start=(kc == 0), stop=(kc == 7)),
                         reads=[Bwb[b][kc], BhT[tt][kc]], writes=[Bps[p]] if kc == 0 else [], inc=(kc == 7))
                Bps[p].w = (c.esem["pe"], c.cnt["pe"], "pe")
                if fc == 28:
                    c.op("dve", lambda e: e.tensor_copy(out=og[:, sl], in_=pss[p][:8, :]), reads=[Bps[p]], writes=[Bog[tt]])
                elif fc < 4:
                    if k % 2 == 0:
                        c.op("act", lambda e: e.activation(out=ob[b][:, sl], in_=pss[p][:], func=AF.Copy, scale=0.125), reads=[Bps[p]], writes=[Bob[b][tt]])
                    else:
                        c.op("dve", lambda e: e.tensor_scalar(out=ob[b][:, sl], in0=pss[p][:], scalar1=0.125, scalar2=None, op0=ALU.mult), reads=[Bps[p]], writes=[Bob[b][tt]])
                else:
                    if k % 2 == 0:
                        c.op("act", lambda e: e.activation(out=ob[b][:, sl], in_=pss[p][:], func=AF.Copy), reads=[Bps[p]], writes=[Bob[b][tt]])
                    else:
                        c.op("dve", lambda e: e.tensor_copy(out=ob[b][:, sl], in_=pss[p][:]), reads=[Bps[p]], writes=[Bob[b][tt]])
                k += 1
            if fc == 28:
                c.dma("sp", out=gatesT[:, :], in_=og[:], reads=Bog, writes=[Bproj[28]])
            else:
                c.dma("sp", out=projT[fc * 128:(fc + 1) * 128, :], in_=ob[b][:], reads=Bob[b], writes=[Bproj[fc]])
    c.barrier()


def load_consts(c, st, cdram):
    cst = {}
    B = Buf()
    def ld(name, shape, dtype):
        t = c.sb(st, shape, dtype, name)
        c.dma("pool" if dtype == BF16 else "sp", out=t[:], in_=cdram[name], writes=[B])
        cst[name] = t
    ld("avgD", (128, 128), BF16)
    t = c.sb(st, (128, 1), F32, "epsc")
    c.op("dve", lambda e: e.memset(t[:], EPS), writes=[B])
    cst["epsc"] = t
    return cst, B


DILS = (1, 4, 16)

def attention(c, projT, mixedT, cst, Bproj, Bmixed):
    with ExitStack() as st:
        qkv = [c.sb(st, (128, S), BF16, "qkv") for _ in range(3)]
        perm = [[c.sb(st, (128, S), BF16, "perm") for _ in range(3)] for _ in range(2)]
        vtok = [c.sb(st, (128, 32, 2, 65), BF16, "vtok") for _ in range(3)]
        acc = [c.sb(st, (65, S), F32, "acc") for _ in range(2)]
        pt = [c.sb(st, (128, 2, 256), BF16, "pt") for _ in range(3)]
        rec = [c.sb(st, (64, TT), F32, "rec") for _ in range(2)]
        ao = [c.sb(st, (64, S), BF16, "ao") for _ in range(2)]
        sp = [c.ps(st, (128, 2, 256), F32, "sp") for _ in range(2)]
        tps = [c.ps(st, (128, 8, 128), BF16, "tp") for _ in range(2)]
        ops = [c.ps(st, (128, 4, 128), F32, "ops") for _ in range(3)]
        dps = ops[2:3]
        Bqkv = bufs(3); Bperm = [bufs(3) for _ in range(2)]; Bvt = [bufs(8) for _ in range(3)]
        Bacc = bufs(2); Bpt = bufs(3); Brec = bufs(2); Bao = [bufs(NTT) for _ in range(2)]
        Bsp = bufs(2); Btp = bufs(2); Bo = bufs(3); Bdps = Bo[2:3]
        ident = cst["ident"]; tab = cst["tab"]; sel = cst["sel65"]
        for pi in range(3):
            c.op("pool", lambda e: e.memset(vtok[pi][:, :, :, 64:65], 1.0), writes=Bvt[pi])
        kq = 0
        for hp in range(4):
            for i in range(3):
                c.dma("sp", out=qkv[i][:], in_=projT[i * 512 + hp * 128: i * 512 + hp * 128 + 128, :], reads=[Bproj[i * 4 + hp]], writes=[Bqkv[i]])
            for pi, dil in enumerate(DILS):
                nbc = 32 // dil
                if dil == 1:
                    src = qkv; Bsrc = Bqkv
                else:
                    src = perm[pi - 1]; Bsrc = Bperm[pi - 1]
                    for i in range(3):
                        c.op("pool", lambda e: e.tensor_copy(out=src[i][:].rearrange("p (c i) -> p c i", c=dil),
                                                             in_=qkv[i][:].rearrange("p (i c) -> p c i", c=dil)),
                             reads=[Bqkv[i]], writes=[Bsrc[i]])
                qP, kP, vP = src
                for g in range(8):
                    tb = g % 2
                    for j in range(4):
                        blk = g * 4 + j
                        c.op("pe", lambda e: e.transpose(out=tps[tb][:, j, :], in_=vP[:, blk * 128:(blk + 1) * 128], identity=ident[:]),
                             reads=[Bsrc[2]], writes=[Btp[tb]] if j == 0 else [], inc=(j == 3))
                    Btp[tb].w = (c.esem["pe"], c.cnt["pe"], "pe")
                    en = "dve" if g % 2 == 0 else "act"
                    o_ap = vtok[pi][:, g * 4:(g + 1) * 4, :, 0:64]
                    i_ap = tps[tb][:, 0:4, :].rearrange("p j (h d) -> p j h d", h=2)
                    if en == "dve":
                        c.op("dve", lambda e: e.tensor_copy(out=o_ap, in_=i_ap), reads=[Btp[tb]], writes=[Bvt[pi][g]])
                    else:
                        c.op("act", lambda e: e.activation(out=o_ap, in_=i_ap, func=AF.Copy), reads=[Btp[tb]], writes=[Bvt[pi][g]])
                for h in range(2):
                    hd = hp * 2 + h
                    rows = slice(h * 64, h * 64 + 64)
                    accv = acc[h][:, :].rearrange("p (i c) -> p c i", c=dil)
                    for cl in range(dil):
                        for n2 in range(0, nbc, 2):
                            s_ = kq % 2; p_ = kq % 3; kq += 1
                            for j in range(2):
                                n = n2 + j; gb = cl * nbc + n
                                nq = 256 if n < nbc - 1 else 128
                                c.op("pe", lambda e: e.matmul(sp[s_][:, j, :nq], lhsT=kP[rows, gb * 128:(gb + 1) * 128], rhs=qP[rows, gb * 128: gb * 128 + nq],
                                                              start=True, stop=False),
                                     reads=[Bsrc[0], Bsrc[1]], writes=[Bsp[s_]] if j == 0 else [], inc=False)
                                c.op("pe", lambda e: e.matmul(sp[s_][:, j, :nq], lhsT=ident[:], rhs=tab[:, hd * 3 + pi, :nq], start=False, stop=True),
                                     inc=(j == 1))
                            Bsp[s_].w = (c.esem["pe"], c.cnt["pe"], "pe")
                            last = (n2 + 1 == nbc - 1)
                            if not last:
                                c.op("act", lambda e: e.activation(out=pt[p_][:], in_=sp[s_][:], func=AF.Exp), reads=[Bsp[s_]], writes=[Bpt[p_]])
                            else:
                                c.op("act", lambda e: e.activation(out=pt[p_][:, 0, :], in_=sp[s_][:, 0, :], func=AF.Exp), reads=[Bsp[s_]], writes=[Bpt[p_]])
                                c.op("act", lambda e: e.activation(out=pt[p_][:, 1, :128], in_=sp[s_][:, 1, :128], func=AF.Exp), reads=[Bsp[s_]], writes=[])
                                Bpt[p_].w = (c.esem["act"], c.cnt["act"], "act")
                            for j in range(2):
                                n = n2 + j; gb = cl * nbc + n
                                oi = gb % 3
                                ot = ops[oi][:65, 0, :]
                                c.op("pe", lambda e: e.matmul(ot, lhsT=vtok[pi][:, gb, h, :], rhs=pt[p_][:, j, 0:128], start=(n == 0), stop=True),
                                     reads=[Bpt[p_], Bvt[pi][gb // 4]], writes=[Bo[oi]] if n == 0 else [], inc=True)
                                Bo[oi].w = (c.esem["pe"], c.cnt["pe"], "pe")
                                av = accv[:, cl, n * 128:(n + 1) * 128]
                                if pi == 0:
                                    c.op("dve", lambda e: e.tensor_copy(out=av, in_=ot), reads=[Bo[oi]], writes=[Bacc[h]])
                                else:
                                    c.op("dve", lambda e: e.tensor_tensor(out=av, in0=av, in1=ot, op=ALU.add), reads=[Bo[oi], Bacc[h]], writes=[Bacc[h]])
                                if n < nbc - 1:
                                    oi2 = (gb + 1) % 3
                                    ot2 = ops[oi2][:65, 0, :]
                                    c.op("pe", lambda e: e.matmul(ot2, lhsT=vtok[pi][:, gb, h, :], rhs=pt[p_][:, j, 128:256], start=True, stop=False),
                                         reads=[Bpt[p_], Bvt[pi][gb // 4]], writes=[Bo[oi2]], inc=False)
            for h in range(2):
                hd = hp * 2 + h
                for tt in range(NTT):
                    sl = slice(tt * TT, (tt + 1) * TT)
                    r_ = tt % 2
                    c.op("pe", lambda e: e.matmul(dps[0][:64, :, :], lhsT=sel[:65, :], rhs=acc[h][:65, sl], start=True, stop=True),
                         reads=[Bacc[h]], writes=[Bdps[0]])
                    c.op("dve", lambda e: e.reciprocal(out=rec[r_][:], in_=dps[0][:64, :, :].rearrange("p a b -> p (a b)")), reads=[Bdps[0]], writes=[Brec[r_]])
                    c.op("pool", lambda e: e.tensor_tensor(out=ao[h][:, sl], in0=acc[h][:64, sl], in1=rec[r_][:], op=ALU.mult),
                         reads=[Bacc[h], Brec[r_]], writes=[Bao[h][tt]])
                c.dma("sp", out=mixedT[hd * 64:(hd + 1) * 64, :], in_=ao[h][:], reads=Bao[h], writes=[Bmixed[hd]])
    c.barrier()


def t5_bucket(dist):
    dist = np.asarray(dist)
    max_exact = 16
    d_f = np.maximum(dist, 1).astype(np.float32)
    large = max_exact + (np.log(d_f / max_exact) / np.log(2048 / max_exact) * (32 - max_exact)).astype(np.int32)
    large = np.minimum(large, 31)
    return np.where(dist < max_exact, dist, large)


def make_tables(rel_bias):
    s = np.arange(128)[:, None]
    t = np.arange(128)[None, :]
    tabs = np.full((128, 24, 256), -30000.0, np.float32)
    for pi, dil in enumerate(DILS):
        bsub = rel_bias[t5_bucket(np.arange(129) * dil)]
        d0 = t - s
        d1 = t + 128 - s
        for h in range(8):
            tabs[:, h * 3 + pi, 0:128] = np.where(d0 >= 0, bsub[np.clip(d0, 0, 128), h], -30000.0)
            tabs[:, h * 3 + pi, 128:256] = np.where(d1 <= 128, bsub[np.clip(d1, 0, 128), h], -30000.0)
    return tabs


def mlstm(c, projT, gatesT, mixedT, Bproj, Bmixed, mcp, gbias, cst):
    ident = cst["ident"]; identf = cst["identf"]; cm = cst["cmask"]
    with ExitStack() as st:
        et = c.sb(st, (128, 32, 8), F32, "et"); Bet = Buf()
        decb = c.sb(st, (128, 128), F32, "decb"); Bdecb = Buf()
        with ExitStack() as s0:
            gi = c.sb(s0, (4, S), F32, "gi"); gf = c.sb(s0, (4, S), F32, "gf"); nb = c.sb(s0, (4, S), F32, "nb")
            dmb = c.sb(s0, (4, S), F32, "dmb"); t1 = c.sb(s0, (4, S), F32, "t1"); t2 = c.sb(s0, (4, S), F32, "t2")
            sm = c.sb(s0, (4, 8, 32), F32, "sm")
            decbd = c.sb(s0, (4, 4, 32), F32, "decbd"); nbf = c.sb(s0, (4, 2), F32, "nbf")
            pg = c.ps(s0, (128, 32, 8), F32, "pg"); pd = c.ps(s0, (128, 128), F32, "pd")
            G = Buf()
            c.dma("sp", out=gi[:], in_=gatesT[0:4, :], reads=[Bproj[28]], writes=[G])
            c.dma("sp", out=gf[:], in_=gatesT[4:8, :], reads=[Bproj[28]], writes=[G])
            def g(en, fn):
                c.op(en, fn, reads=[G], writes=[G])
            g("dve", lambda e: e.tensor_scalar(out=nbf[:, 0:1], in0=gbias[:, 1:2], scalar1=-1.0, scalar2=None, op0=ALU.mult))
            g("dve", lambda e: e.memset(nbf[:, 1:2], -0.5 * math.log(128.0)))
            g("act", lambda e: e.activation(out=t1[:], in_=gf[:], func=AF.Exp, scale=-1.0, bias=nbf[:, 0:1]))
            g("act", lambda e: e.activation(out=t1[:], in_=t1[:], func=AF.Ln, bias=cst["one1"][:4, 0:1]))
            g("dve", lambda e: e.tensor_tensor_scan(out=nb[:], data0=cst["rmask"][:4, :], data1=t1[:], initial=0.0, op0=ALU.mult, op1=ALU.add))
            g("dve", lambda e: e.scalar_tensor_tensor(out=dmb[:], in0=gi[:], scalar=gbias[:, 0:1], in1=nb[:], op0=ALU.add, op1=ALU.add))
            g("dve", lambda e: e.tensor_reduce(out=sm[:, 0, :], in_=dmb[:].rearrange("p (c s) -> p c s", s=128), axis=AX.X, op=ALU.max))
            g("dve", lambda e: e.tensor_scalar(out=sm[:, 1, :], in0=nb[:].rearrange("p (c s) -> p c s", s=128)[:, :, 127], scalar1=-1.0, scalar2=None, op0=ALU.mult))
            g("dve", lambda e: e.tensor_tensor_scan(out=sm[:, 2, :], data0=sm[:, 0, :], data1=sm[:, 1, :], initial=0.0, op0=ALU.max, op1=ALU.add))
            g("dve", lambda e: e.memset(sm[:, 3, 0:1], 0.0))
            g("dve", lambda e: e.tensor_copy(out=sm[:, 3, 1:32], in_=sm[:, 2, 0:31]))
            g("dve", lambda e: e.tensor_tensor(out=sm[:, 4, :], in0=sm[:, 3, :], in1=sm[:, 0, :], op=ALU.max))
            g("dve", lambda e: e.tensor_tensor(out=sm[:, 5, :], in0=sm[:, 3, :], in1=sm[:, 4, :], op=ALU.subtract))
            g("act", lambda e: e.activation(out=sm[:, 5, :], in_=sm[:, 5, :], func=AF.Exp))
            Mb = sm[:, 4, :].unsqueeze(2).to_broadcast([4, 32, 128])
            g("dve", lambda e: e.tensor_tensor(out=t1[:].rearrange("p (c s) -> p c s", s=128), in0=dmb[:].rearrange("p (c s) -> p c s", s=128), in1=Mb, op=ALU.subtract))
            g("act", lambda e: e.activation(out=t1[:], in_=t1[:], func=AF.Exp, bias=nbf[:, 1:2]))
            g("dve", lambda e: e.tensor_tensor(out=t2[:].rearrange("p (c s) -> p c s", s=128), in0=nb[:].rearrange("p (c s) -> p c s", s=128), in1=Mb, op=ALU.subtract))
            g("act", lambda e: e.activation(out=t2[:], in_=t2[:], func=AF.Exp))
            for cc in range(32):
                cs = slice(cc * 128, (cc + 1) * 128)
                c.op("pe", lambda e: e.transpose(out=pg[:, cc, 0:4], in_=t1[:, cs], identity=identf[:4, :4]), reads=[G], writes=[G] if cc == 0 else [], inc=False)
                c.op("pe", lambda e: e.transpose(out=pg[:, cc, 4:8], in_=t2[:, cs], identity=identf[:4, :4]), reads=[G], inc=(cc == 31))
            G.w = (c.esem["pe"], c.cnt["pe"], "pe")
            c.op("dve", lambda e: e.tensor_copy(out=et[:], in_=pg[:]), reads=[G], writes=[Bet])
            g("dve", lambda e: e.tensor_tensor(out=decbd[:], in0=sm[:, 5, :].unsqueeze(1).to_broadcast([4, 4, 32]), in1=cst["bdmask"][:4, :].rearrange("p (a b) -> p a b", a=4), op=ALU.mult))
            c.op("pe", lambda e: e.matmul(pd[:], lhsT=cst["ones4"][:4, :], rhs=decbd[:].rearrange("p a b -> p (a b)"), start=True, stop=True), reads=[G], writes=[G])
            c.op("dve", lambda e: e.tensor_copy(out=decb[:], in_=pd[:]), reads=[G], writes=[Bdecb])
            c.barrier()
        raw = [c.sb(st, (128, 3 + S), BF16, "raw") for _ in range(2)]; Braw = bufs(2)
        cv = [c.sb(st, (128, S), F32, "cv") for _ in range(2)]; Bcv = bufs(2)
        qT = c.sb(st, (128, S), BF16, "qT"); kT = c.sb(st, (128, S), BF16, "kT"); BqT = Buf(); BkT = Buf()
        vT = c.sb(st, (128, S), BF16, "vT"); oT = c.sb(st, (128, S), BF16, "oT"); BvT = Buf(); BoT = Buf()
        hmT = c.sb(st, (128, S), BF16, "hmT"); BhmT = bufs(32)
        ktk = [c.sb(st, (128, 128), BF16, "ktk") for _ in range(2)]; Bktk = bufs(2)
        vaug = [c.sb(st, (128, 129), BF16, "vaug") for _ in range(2)]; Bvaug = bufs(2)
        og = [c.sb(st, (128, 128), F32, "og") for _ in range(2)]; Bog = bufs(2)
        pT = [c.sb(st, (128, 128), BF16, "pT") for _ in range(2)]; BpT = bufs(2)
        hmt = [c.sb(st, (128, 128), BF16, "hmt") for _ in range(2)]; Bhmt = bufs(2)
        C = c.sb(st, (128, 129), F32, "C"); BC = Buf()
        Cbf = [c.sb(st, (128, 129), BF16, "Cbf") for _ in range(2)]; BCbf = bufs(2)
        dd = [c.sb(st, (128, 2), F32, "dd") for _ in range(2)]; Bdd = bufs(2)
        tpk = [c.ps(st, (128, 8, 128), BF16, "tpk") for _ in range(2)]; Btpk = bufs(2)
        sps = c.ps(st, (128, 512), F32, "sps"); Bsps = Buf()
        nps = [c.ps(st, (128, 512), F32, "nps") for _ in range(2)]; Bnps = bufs(2)
        dps = c.ps(st, (128, 512), F32, "dps"); Bdps = Buf()
        tph = c.ps(st, (128, 8, 128), BF16, "tph"); Btph = Buf()
        for r in range(2):
            c.op("pool", lambda e: e.memset(raw[r][:, 0:3], 0.0), writes=[Braw[r]])
            c.op("pool", lambda e: e.memset(vaug[r][:, 128:129], 1.0), writes=[Bvaug[r]])
        for hd in range(4):
            c.dma("sp", out=raw[0][:, 3:], in_=projT[1536 + hd * 128:1536 + (hd + 1) * 128, :], reads=[Bproj[12 + hd]], writes=[Braw[0]])
            c.dma("sp", out=raw[1][:, 3:], in_=projT[2048 + hd * 128:2048 + (hd + 1) * 128, :], reads=[Bproj[16 + hd]], writes=[Braw[1]])
            c.dma("sp", out=vT[:], in_=projT[2560 + hd * 128:2560 + (hd + 1) * 128, :], reads=[Bproj[20 + hd]], writes=[BvT])
            c.dma("sp", out=oT[:], in_=projT[3072 + hd * 128:3072 + (hd + 1) * 128, :], reads=[Bproj[24 + hd]], writes=[BoT])
            for i, en, dst, Bdst in ((0, "dve", qT, BqT), (1, "dve", kT, BkT)):
                ci = i * 4 + hd
                c.op(en, lambda e: e.tensor_scalar(out=cv[i][:], in0=raw[i][:, 3:3 + S], scalar1=mcp[:, ci, 3:4], scalar2=mcp[:, ci, 4:5], op0=ALU.mult, op1=ALU.add),
                     reads=[Braw[i]], writes=[Bcv[i]])
                for j in range(3):
                    c.op(en, lambda e: e.scalar_tensor_tensor(out=cv[i][:], in0=raw[i][:, j:j + S], scalar=mcp[:, ci, j:j + 1], in1=cv[i][:], op0=ALU.mult, op1=ALU.add),
                         reads=[Braw[i], Bcv[i]], writes=[Bcv[i]])
                c.op("act", lambda e: e.activation(out=dst[:], in_=cv[i][:], func=AF.Silu), reads=[Bcv[i]], writes=[Bdst])
            for cc in range(32):
                r = cc % 2
                cs = slice(cc * 128, (cc + 1) * 128)
                ecol = et[:, cc, hd:hd + 1]; fcol = et[:, cc, 4 + hd:5 + hd]
                for j, (src, Bsrc) in enumerate(((kT, BkT), (vT, BvT), (oT, BoT))):
                    c.op("pe", lambda e: e.transpose(out=tpk[r][:, j, :], in_=src[:, cs], identity=ident[:]), reads=[Bsrc], writes=[Btpk[r]] if j == 0 else [], inc=(j == 2))
                Btpk[r].w = (c.esem["pe"], c.cnt["pe"], "pe")
                c.op("dve", lambda e: e.tensor_scalar(out=ktk[r][:], in0=tpk[r][:, 0, :], scalar1=ecol, scalar2=None, op0=ALU.mult), reads=[Btpk[r], Bet], writes=[Bktk[r]])
                c.op("act", lambda e: e.activation(out=vaug[r][:, 0:128], in_=tpk[r][:, 1, :], func=AF.Copy), reads=[Btpk[r]], writes=[Bvaug[r]])
                c.op("act", lambda e: e.activation(out=og[r][:], in_=tpk[r][:, 2, :], func=AF.Sigmoid), reads=[Btpk[r]], writes=[Bog[r]])
                c.op("pe", lambda e: e.matmul(sps[:, 0:128], lhsT=kT[:, cs], rhs=qT[:, cs], start=True, stop=True), reads=[BkT, BqT], writes=[Bsps])
                c.op("dve", lambda e: e.scalar_tensor_tensor(out=pT[r][:], in0=sps[:, 0:128], scalar=ecol, in1=cm[:], op0=ALU.mult, op1=ALU.mult), reads=[Bsps, Bet], writes=[BpT[r]])
                if cc > 0:
                    dcol = decb[:, hd * 32 + cc: hd * 32 + cc + 1]
                    c.op("dve", lambda e: e.tensor_scalar(out=C[:], in0=C[:], scalar1=dcol, scalar2=None, op0=ALU.mult), reads=[BC, Bdecb], writes=[BC])
                    c.op("act", lambda e: e.activation(out=Cbf[r][:], in_=C[:], func=AF.Copy), reads=[BC], writes=[BCbf[r]])
                c.op("pe", lambda e: e.matmul(nps[r][:, 0:129], lhsT=pT[r][:], rhs=vaug[r][:], start=True, stop=(cc == 0)), reads=[BpT[r], Bvaug[r]], writes=[Bnps[r]], inc=(cc == 0))
                if cc > 0:
                    c.op("pe", lambda e: e.matmul(nps[r][:, 0:129], lhsT=qT[:, cs], rhs=Cbf[r][:], start=False, stop=True), reads=[BqT, BCbf[r]], inc=True)
                Bnps[r].w = (c.esem["pe"], c.cnt["pe"], "pe")
                c.op("pe", lambda e: e.matmul(dps[:, 0:129], lhsT=ktk[r][:], rhs=vaug[r][:], start=True, stop=True), reads=[Bktk[r], Bvaug[r]], writes=[Bdps])
                if cc == 0:
                    c.op("dve", lambda e: e.tensor_copy(out=C[:], in_=dps[:, 0:129]), reads=[Bdps], writes=[BC])
                else:
                    c.op("dve", lambda e: e.tensor_tensor(out=C[:], in0=C[:], in1=dps[:, 0:129], op=ALU.add), reads=[Bdps, BC], writes=[BC])
                c.op("dve", lambda e: e.tensor_scalar(out=dd[r][:, 1:2], in0=nps[r][:, 128:129], scalar1=-1.0, scalar2=None, op0=ALU.mult), reads=[Bnps[r]], writes=[Bdd[r]])
                c.op("dve", lambda e: e.scalar_tensor_tensor(out=dd[r][:, 0:1], in0=nps[r][:, 128:129], scalar=fcol, in1=dd[r][:, 1:2], op0=ALU.max, op1=ALU.max), reads=[Bnps[r], Bet, Bdd[r]], writes=[Bdd[r]])
                c.op("dve", lambda e: e.reciprocal(out=dd[r][:, 1:2], in_=dd[r][:, 0:1]), reads=[Bdd[r]], writes=[Bdd[r]])
                c.op("dve", lambda e: e.scalar_tensor_tensor(out=hmt[r][:], in0=nps[r][:, 0:128], scalar=dd[r][:, 1:2], in1=og[r][:], op0=ALU.mult, op1=ALU.mult),
                     reads=[Bnps[r], Bdd[r], Bog[r]], writes=[Bhmt[r]])
                c.op("pe", lambda e: e.transpose(out=tph[:, 0, :], in_=hmt[r][:], identity=ident[:]), reads=[Bhmt[r]], writes=[Btph])
                c.op("act", lambda e: e.activation(out=hmT[:, cs], in_=tph[:, 0, :], func=AF.Copy), reads=[Btph], writes=[BhmT[cc]])
            c.dma("sp", out=mixedT[512 + hd * 128: 512 + (hd + 1) * 128, :], in_=hmT[:], reads=BhmT, writes=[Bmixed[8 + hd]])
    c.barrier()


def mlstm_consts(c, st, cd):
    cst = {}; B = Buf()
    cst["identf"] = c.sb(st, (128, 128), F32, "identf"); c.dma("sp", out=cst["identf"][:], in_=cd["identf"], writes=[B])
    cst["ident"] = c.sb(st, (128, 128), BF16, "ident"); c.dma("pool", out=cst["ident"][:], in_=cd["identf"], writes=[B])
    cst["cmask"] = c.sb(st, (128, 128), F32, "cmask"); c.dma("sp", out=cst["cmask"][:], in_=cd["cmask"], writes=[B])
    cst["rmask"] = c.sb(st, (4, S), F32, "rmask"); c.dma("sp", out=cst["rmask"][:], in_=cd["rmask"], writes=[B])
    cst["bdmask"] = c.sb(st, (4, 128), F32, "bdmask"); c.dma("sp", out=cst["bdmask"][:], in_=cd["bdmask"], writes=[B])
    cst["ones4"] = c.sb(st, (4, 128), F32, "ones4"); c.dma("sp", out=cst["ones4"][:], in_=cd["ones4"], writes=[B])
    cst["one1"] = c.sb(st, (128, 1), F32, "one1"); c.op("dve", lambda e: e.memset(cst["one1"][:], 1.0), writes=[B])
    return cst


def mlstm_const_arrays():
    t = np.arange(128)
    cmask = (t[None, :] >= t[:, None]).astype(np.float32)
    rmask = np.ones((4, S), np.float32); rmask[:, ::128] = 0.0
    bd = np.zeros((4, 4, 32), np.float32)
    for h in range(4):
        bd[h, h] = 1.0
    return {"identf": np.eye(128, dtype=np.float32), "cmask": cmask, "rmask": rmask, "bdmask": bd.reshape(4, 128), "ones4": np.ones((4, 128), np.float32)}


def ffn_up(c, w_up, gcol, fcp, hT, BhT, hffT, Bhff):
    wv = w_up.rearrange("(kc p) n -> p kc n", p=128)
    with ExitStack() as st:
        ws = [c.sb(st, (128, 8, 128), F32, "ws") for _ in range(2)]
        wb = [c.sb(st, (128, 8, 128), BF16, "wb") for _ in range(2)]
        u = [[c.sb(st, (128, 2 + S), BF16, "u") for _ in range(2)] for _ in range(2)]
        cv = [c.sb(st, (128, S), F32, "cv") for _ in range(2)]
        gg = c.sb(st, (128, S), BF16, "gg")
        ho = [c.sb(st, (128, S), BF16, "ho") for _ in range(2)]
        pss = [c.ps(st) for _ in range(4)]
        Bws = bufs(2); Bwb = [bufs(8) for _ in range(2)]; Bu = [[bufs(NTT) for _ in range(2)] for _ in range(2)]
        Bcv = bufs(2); Bgg = Buf(); Bho = bufs(2); Bps = bufs(4)
        for par in range(2):
            for half in range(2):
                c.op("pool", lambda e: e.memset(u[par][half][:, 0:2], 0.0), writes=Bu[par][half])
        k = 0
        for j in range(NFF):
            par = j % 2
            for half in range(2):
                ci = j + NFF * half
                b = (2 * j + half) % 2
                c.dma("sp", out=ws[b][:], in_=wv[:, :, ci * 128:(ci + 1) * 128], writes=[Bws[b]])
                for kc in range(8):
                    c.op("pool", lambda e: e.tensor_scalar(out=wb[b][:, kc, :], in0=ws[b][:, kc, :], scalar1=gcol[:, kc:kc + 1], scalar2=None, op0=ALU.mult),
                         reads=[Bws[b]], writes=[Bwb[b][kc]])
                for tt in range(NTT):
                    sl = slice(tt * TT, (tt + 1) * TT)
                    p = k % 4
                    for kc in range(8):
                        c.op("pe", lambda e: e.matmul(pss[p][:], lhsT=wb[b][:, kc, :], rhs=hT[:, kc, sl], start=(kc == 0), stop=(kc == 7)),
                             reads=[Bwb[b][kc], BhT[tt][kc]], writes=[Bps[p]] if kc == 0 else [], inc=(kc == 7))
                    Bps[p].w = (c.esem["pe"], c.cnt["pe"], "pe")
                    o_ap = u[par][half][:, 2 + tt * TT: 2 + (tt + 1) * TT]
                    c.op("act", lambda e: e.activation(out=o_ap, in_=pss[p][:], func=AF.Copy), reads=[Bps[p]], writes=[Bu[par][half][tt]])
                    k += 1
            for half, en in ((0, "dve"), (1, "dve")):
                ci = j + NFF * half
                uu = u[par][half]
                c.op(en, lambda e: e.tensor_scalar(out=cv[half][:], in0=uu[:, 2:2 + S], scalar1=fcp[:, ci, 2:3], scalar2=fcp[:, ci, 3:4], op0=ALU.mult, op1=ALU.add),
                     reads=Bu[par][half], writes=[Bcv[half]])
                c.op(en, lambda e: e.scalar_tensor_tensor(out=cv[half][:], in0=uu[:, 1:1 + S], scalar=fcp[:, ci, 1:2], in1=cv[half][:], op0=ALU.mult, op1=ALU.add),
                     reads=Bu[par][half] + [Bcv[half]], writes=[Bcv[half]])
                c.op(en, lambda e: e.scalar_tensor_tensor(out=cv[half][:], in0=uu[:, 0:S], scalar=fcp[:, ci, 0:1], in1=cv[half][:], op0=ALU.mult, op1=ALU.add),
                     reads=Bu[par][half] + [Bcv[half]], writes=[Bcv[half]])
            c.op("act", lambda e: e.activation(out=gg[:], in_=cv[1][:], func=AF.Gelu_apprx_tanh), reads=[Bcv[1]], writes=[Bgg])
            c.op("dve", lambda e: e.tensor_tensor(out=ho[par][:], in0=gg[:], in1=cv[0][:], op=ALU.mult), reads=[Bgg, Bcv[0]], writes=[Bho[par]])
            c.dma("sp", out=hffT[j * 128:(j + 1) * 128, :], in_=ho[par][:], reads=[Bho[par]], writes=[Bhff[j]])
    c.barrier()


def postnorm_residual(c, st_bufs, hsrc, Bh, xt, Bx, gcol, cst, tag):
    sq, Bsq, ps, Bps, rs, Brs, tmp, Btmp = st_bufs
    for oc in range(8):
        c.op("act", lambda e: e.activation(out=sq[:, oc, :], in_=hsrc[:, oc, :], func=AF.Square), reads=[Bh[oc]], writes=[Bsq[oc]])
    for oc in range(8):
        c.op("pe", lambda e: e.matmul(ps[:], lhsT=cst["avgD"][:], rhs=sq[:, oc, :], start=(oc == 0), stop=(oc == 7)),
             reads=[Bsq[oc]], writes=[Bps] if oc == 0 else [], inc=(oc == 7))
    Bps.w = (c.esem["pe"], c.cnt["pe"], "pe")
    c.op("act", lambda e: e.activation(out=rs[:], in_=ps[:], func=AF.Sqrt, bias=cst["epsc"][:, 0:1]), reads=[Bps], writes=[Brs])
    c.op("dve", lambda e: e.reciprocal(out=rs[:], in_=rs[:]), reads=[Brs], writes=[Brs])
    for oc in range(8):
        en = "dve" if oc % 2 == 0 else "pool"
        c.op(en, lambda e: e.tensor_tensor(out=tmp[:, oc, :], in0=hsrc[:, oc, :], in1=rs[:], op=ALU.mult), reads=[Bh[oc], Brs], writes=[Btmp[oc]])
        c.op("dve", lambda e: e.scalar_tensor_tensor(out=xt[:, oc, :], in0=tmp[:, oc, :], scalar=gcol[:, oc:oc + 1], in1=xt[:, oc, :], op0=ALU.mult, op1=ALU.add),
             reads=[Btmp[oc], Bx[oc]], writes=[Bx[oc]])


def ffn_down(c, w_down, gpost, hffT, Bhff, xT, cst):
    wv = w_down.rearrange("(c p) n -> p c n", p=128)
    hv = hffT.rearrange("(c p) t -> p c t", p=128)
    xv = xT.rearrange("(kc p) t -> p kc t", p=128)
    with ExitStack() as st:
        wd = c.sb(st, (128, NFF, D), BF16, "wd")
        wst = [c.sb(st, (128, NFF, 128), F32, "wst") for _ in range(1)]
        hf = [c.sb(st, (128, NFF, TT), BF16, "hf") for _ in range(2)]
        xt = [c.sb(st, (128, 8, TT), F32, "xt") for _ in range(2)]
        hd = c.sb(st, (128, 8, TT), F32, "hd")
        sq = c.sb(st, (128, 8, TT), BF16, "sq"); rs = c.sb(st, (128, TT), F32, "rs")
        pss = [c.ps(st) for _ in range(3)]
        psn = c.ps(st)
        Bwd = bufs(8); Bwst = bufs(1); Bhf = bufs(2); Bxt = [bufs(8) for _ in range(2)]; Bhd = bufs(8)
        Bps = bufs(3)
        sb_ = (sq, bufs(8), psn, Buf(), rs, Buf(), hd, Bhd)
        for oc in range(8):
            b = 0
            c.dma("sp", out=wst[b][:], in_=wv[:, :, oc * 128:(oc + 1) * 128], writes=[Bwst[b]])
            c.op("pool", lambda e: e.tensor_copy(out=wd[:, :, oc * 128:(oc + 1) * 128], in_=wst[b][:]), reads=[Bwst[b]], writes=[Bwd[oc]])
        k = 0
        for tt in range(NTT):
            b = tt % 2
            sl = slice(tt * TT, (tt + 1) * TT)
            c.dma("sp", out=hf[b][:], in_=hv[:, :, sl], reads=Bhff, writes=[Bhf[b]])
            c.dma("sp", out=xt[b][:], in_=xv[:, :, sl], writes=Bxt[b])
            for oc in range(8):
                p = k % 3; k += 1
                for fc in range(NFF):
                    c.op("pe", lambda e: e.matmul(pss[p][:], lhsT=wd[:, fc, oc * 128:(oc + 1) * 128], rhs=hf[b][:, fc, :], start=(fc == 0), stop=(fc == NFF - 1)),
                         reads=[Bwd[oc], Bhf[b]], writes=[Bps[p]] if fc == 0 else [], inc=(fc == NFF - 1))
                Bps[p].w = (c.esem["pe"], c.cnt["pe"], "pe")
                c.op("dve", lambda e: e.tensor_copy(out=hd[:, oc, :], in_=pss[p][:]), reads=[Bps[p]], writes=[Bhd[oc]])
            postnorm_residual(c, sb_, hd, Bhd, xt[b], Bxt[b], gpost, cst, "ffn")
            c.dma("sp", out=xv[:, :, sl], in_=xt[b][:], reads=Bxt[b])
    c.barrier()


def token_phase(c, l, W, prm, mixedT, Bmixed, xT, memT, cst):
    mv = mixedT.rearrange("(kc p) t -> p kc t", p=128)
    xv = xT.rearrange("(kc p) t -> p kc t", p=128)
    with ExitStack() as st:
        wres = {n: c.sb(st, (128, 8, D), BF16, n) for n in ("w_out", "wq", "wo")}
        Bwres = {n: bufs(8) for n in wres}
        KT = c.sb(st, (128, 8, NMEM), BF16, "KT"); V = c.sb(st, (128, 2, D), BF16, "V"); mem = c.sb(st, (128, 8, NMEM), BF16, "mem")
        BKT = bufs(8); BV = bufs(8); Bmem = Buf()
        ws = [c.sb(st, (128, 8, 128), F32, "ws") for _ in range(2)]; Bws = bufs(2)
        wtmp = [c.sb(st, (128, 8, 128), BF16, "wtmp") for _ in range(2)]; Bwtmp = bufs(2)
        mx = c.sb(st, (128, 8, TT), BF16, "mx"); Bmx = bufs(8)
        sq = c.sb(st, (128, 8, TT), BF16, "sq"); Bsq = bufs(8)
        hd = c.sb(st, (128, 8, TT), F32, "hd"); Bhd = bufs(8)
        xt = [c.sb(st, (128, 8, TT), F32, "xt") for _ in range(2)]; Bxt = [bufs(8) for _ in range(2)]
        qT = c.sb(st, (128, 8, TT), BF16, "qT"); BqT = bufs(8)
        on = c.sb(st, (128, 8, TT), BF16, "on"); Bon = bufs(8)
        P = [c.sb(st, (128, TT), BF16, "P") for _ in range(2)]; BP = bufs(2)
        rs = c.sb(st, (128, TT), F32, "rs"); Brs = Buf()
        rA = c.sb(st, (128, TT), F32, "rA"); BrA = Buf()
        rM = c.sb(st, (128, TT), F32, "rM"); BrM = Buf()
        rden = c.sb(st, (128, TT), F32, "rden"); Brden = Buf()
        NPS = 7
        pss = [c.ps(st) for _ in range(NPS)]; Bps = bufs(NPS)
        psn = c.ps(st); Bpsn = Buf()
        pk = [0]

        def nextps():
            i = pk[0] % NPS; pk[0] += 1
            return pss[i], Bps[i]

        def mm_group(ps, Bp, parts, reads_list):
            n = len(parts)
            for i, (lh, rh) in enumerate(parts):
                c.op("pe", lambda e: e.matmul(ps, lhsT=lh, rhs=rh, start=(i == 0), stop=(i == n - 1)),
                     reads=reads_list[i], writes=[Bp] if i == 0 else [], inc=(i == n - 1))
            Bp.w = (c.esem["pe"], c.cnt["pe"], "pe")

        with ExitStack() as s0:
            mf = c.sb(s0, (128, 8, NMEM), F32, "mf"); Bmf = Buf()
            c.dma("sp", out=mf[:], in_=memT.rearrange("(kc p) m -> p kc m", p=128), writes=[Bmf])
            c.op("pool", lambda e: e.tensor_copy(out=mem[:], in_=mf[:]), reads=[Bmf], writes=[Bmem])
            c.barrier()
        kw = 0
        for name, g in (("w_out", prm["gmix"]), ("wq", prm["pre_mem"]), ("wo", None)):
            wv_ = W[name].rearrange("(kc p) n -> p kc n", p=128)
            for oc in range(8):
                b = kw % 2; kw += 1
                c.dma("sp", out=ws[b][:], in_=wv_[:, :, oc * 128:(oc + 1) * 128], writes=[Bws[b]])
                if g is None:
                    c.op("pool", lambda e: e.tensor_copy(out=wres[name][:, :, oc * 128:(oc + 1) * 128], in_=ws[b][:]), reads=[Bws[b]], writes=[Bwres[name][oc]])
                else:
                    for kc in range(8):
                        c.op("pool", lambda e: e.tensor_scalar(out=wres[name][:, kc, oc * 128:(oc + 1) * 128], in0=ws[b][:, kc, :], scalar1=g[:, kc:kc + 1], scalar2=None, op0=ALU.mult),
                             reads=[Bws[b]], writes=[Bwres[name][oc]])
        for name in ("wk", "wv"):
            wv_ = W[name].rearrange("(kc p) n -> p kc n", p=128)
            for oc in range(8):
                b = kw % 2; kw += 1
                c.dma("sp", out=ws[b][:], in_=wv_[:, :, oc * 128:(oc + 1) * 128], writes=[Bws[b]])
                c.op("pool", lambda e: e.tensor_copy(out=wtmp[b][:], in_=ws[b][:]), reads=[Bws[b]], writes=[Bwtmp[b]])
                if name == "wk":
                    ps, Bp = nextps()
                    mm_group(ps[:, :NMEM], Bp, [(wtmp[b][:, kc, :], mem[:, kc, :]) for kc in range(8)], [[Bwtmp[b], Bmem]] * 8)
                    c.op("dve", lambda e: e.tensor_copy(out=KT[:, oc, :], in_=ps[:, :NMEM]), reads=[Bp], writes=[BKT[oc]])
                else:
                    for mb in range(2):
                        ps, Bp = nextps()
                        mm_group(ps[:, :128], Bp, [(mem[:, kc, mb * 128:(mb + 1) * 128], wtmp[b][:, kc, :]) for kc in range(8)], [[Bwtmp[b], Bmem]] * 8)
                        c.op("dve", lambda e: e.tensor_copy(out=V[:, mb, oc * 128:(oc + 1) * 128], in_=ps[:, :128]), reads=[Bp], writes=[BV[oc]])
        sb_ = (sq, Bsq, psn, Bpsn, rs, Brs, hd, Bhd)
        ek = 0
        for tt in range(NTT):
            b = tt % 2
            sl = slice(tt * TT, (tt + 1) * TT)
            c.dma("sp", out=mx[:], in_=mv[:, :, sl], reads=Bmixed, writes=Bmx)
            c.dma("sp", out=xt[b][:], in_=xv[:, :, sl], writes=Bxt[b])
            for kc in range(8):
                c.op("act", lambda e: e.activation(out=sq[:, kc, :], in_=mx[:, kc, :], func=AF.Square), reads=[Bmx[kc]], writes=[Bsq[kc]])
            for grp, (rr, Brr) in enumerate(((rA, BrA), (rM, BrM))):
                ps, Bp = nextps()
                mm_group(ps[:], Bp, [(cst["avgH"][:], sq[:, grp * 4 + i, :]) for i in range(4)], [[Bsq[grp * 4 + i]] for i in range(4)])
                c.op("act", lambda e: e.activation(out=rr[:], in_=ps[:], func=AF.Sqrt, bias=cst["epsc"][:, 0:1]), reads=[Bp], writes=[Brr])
                c.op("dve", lambda e: e.reciprocal(out=rr[:], in_=rr[:]), reads=[Brr], writes=[Brr])
            for kc in range(8):
                rr, Brr = (rA, BrA) if kc < 4 else (rM, BrM)
                en = "dve" if kc % 2 == 0 else "pool"
                c.op(en, lambda e: e.tensor_tensor(out=mx[:, kc, :], in0=mx[:, kc, :], in1=rr[:], op=ALU.mult), reads=[Bmx[kc], Brr], writes=[Bmx[kc]])

            def proj(wname, src, Bsrc, evac):
                nonlocal ek
                for oc in range(8):
                    ps, Bp = nextps()
                    mm_group(ps[:], Bp, [(wres[wname][:, kc, oc * 128:(oc + 1) * 128], src[:, kc, :]) for kc in range(8)],
                             [[Bwres[wname][oc], Bsrc[kc]] for kc in range(8)])
                    evac(oc, ps, Bp)

            def evac_hd(oc, ps, Bp):
                nonlocal ek
                ek += 1
                if ek % 2 == 0:
                    c.op("dve", lambda e: e.tensor_copy(out=hd[:, oc, :], in_=ps[:]), reads=[Bp], writes=[Bhd[oc]])
                else:
                    c.op("act", lambda e: e.activation(out=hd[:, oc, :], in_=ps[:], func=AF.Copy), reads=[Bp], writes=[Bhd[oc]])

            proj("w_out", mx, Bmx, evac_hd)
            postnorm_residual(c, sb_, hd, Bhd, xt[b], Bxt[b], prm["post_mix"], cst, "mix")
            for kc in range(8):
                c.op("act", lambda e: e.activation(out=sq[:, kc, :], in_=xt[b][:, kc, :], func=AF.Square), reads=[Bxt[b][kc]], writes=[Bsq[kc]])
            mm_group(psn[:], Bpsn, [(cst["avgD"][:], sq[:, kc, :]) for kc in range(8)], [[Bsq[kc]] for kc in range(8)])
            c.op("act", lambda e: e.activation(out=rs[:], in_=psn[:], func=AF.Sqrt, bias=cst["epsc"][:, 0:1]), reads=[Bpsn], writes=[Brs])
            c.op("dve", lambda e: e.reciprocal(out=rs[:], in_=rs[:]), reads=[Brs], writes=[Brs])
            for kc in range(8):
                en = "dve" if kc % 2 == 0 else "pool"
                c.op(en, lambda e: e.tensor_tensor(out=mx[:, kc, :], in0=xt[b][:, kc, :], in1=rs[:], op=ALU.mult), reads=[Bxt[b][kc], Brs], writes=[Bmx[kc]])

            def evac_q(oc, ps, Bp):
                nonlocal ek
                ek += 1
                if ek % 2 == 0:
                    c.op("dve", lambda e: e.tensor_scalar(out=qT[:, oc, :], in0=ps[:], scalar1=1.0 / 16, scalar2=None, op0=ALU.mult), reads=[Bp], writes=[BqT[oc]])
                else:
                    c.op("act", lambda e: e.activation(out=qT[:, oc, :], in_=ps[:], func=AF.Copy, scale=1.0 / 16), reads=[Bp], writes=[BqT[oc]])

            proj("wq", mx, Bmx, evac_q)
            for xh in range(4):
                for mb in range(2):
                    ps, Bp = nextps()
                    mm_group(ps[:], Bp, [(KT[:, 2 * xh + dc, mb * 128:(mb + 1) * 128], qT[:, 2 * xh + dc, :]) for dc in range(2)],
                             [[BKT[2 * xh + dc], BqT[2 * xh + dc]] for dc in range(2)])
                    c.op("act", lambda e: e.activation(out=P[mb][:], in_=ps[:], func=AF.Exp), reads=[Bp], writes=[BP[mb]])
                ps, Bp = nextps()
                mm_group(ps[:], Bp, [(cst["ones"][:], P[mb][:]) for mb in range(2)], [[BP[mb]] for mb in range(2)])
                c.op("dve", lambda e: e.reciprocal(out=rden[:], in_=ps[:]), reads=[Bp], writes=[Brden])
                for dc in range(2):
                    oc = 2 * xh + dc
                    ps, Bp = nextps()
                    mm_group(ps[:], Bp, [(V[:, mb, oc * 128:(oc + 1) * 128], P[mb][:]) for mb in range(2)], [[BV[oc], BP[mb]] for mb in range(2)])
                    c.op("dve", lambda e: e.tensor_tensor(out=on[:, oc, :], in0=ps[:], in1=rden[:], op=ALU.mult), reads=[Bp, Brden], writes=[Bon[oc]])
            proj("wo", on, Bon, evac_hd)
            postnorm_residual(c, sb_, hd, Bhd, xt[b], Bxt[b], prm["post_mem"], cst, "mem")
            c.dma("sp", out=xv[:, :, sl], in_=xt[b][:], reads=Bxt[b])
    c.barrier()


NPAR = 272


def build_program(nl=NL, debug=False):
    nc = bass.Bass("TRN2", target_bir_lowering=False)
    di = lambda n, shp: nc.dram_tensor(n, list(shp), F32, kind="ExternalInput").ap()
    xin = di("xin", (D, S)); memT = di("memT", (D, NMEM))
    w_in = di("w_in", (NL, D, IN_COLS)); w_out = di("w_out", (NL, D, D))
    wq = di("wq_mem", (NL, D, D)); wk = di("wk_mem", (NL, D, D)); wv = di("wv_mem", (NL, D, D)); wo = di("wo_mem", (NL, D, D))
    w_up = di("w_up", (NL, D, 2 * DFF)); w_down = di("w_down", (NL, DFF, D))
    par = di("par", (128, NL, NPAR)); gb = di("gb", (4, NL, 2))
    cin = di("cin", (128, 3, 128)); tabd = di("tab", (128, 24, 256)); seld = di("sel65", (65, 64))
    ca = mlstm_const_arrays()
    cd = {k: di(k, v.shape) for k, v in ca.items()}
    yT = nc.dram_tensor("yT", [D, S], F32, kind="ExternalOutput").ap()
    sk = "ExternalOutput" if debug else "Internal"
    projT = nc.dram_tensor("projT", [3584, S], BF16, kind=sk).ap()
    gatesT = nc.dram_tensor("gatesT", [8, S], F32, kind=sk).ap()
    mixedT = nc.dram_tensor("mixedT", [D, S], BF16, kind=sk).ap()
    hffT = nc.dram_tensor("hffT", [DFF, S], BF16, kind=sk).ap()
    with ExitStack() as es:
        c = Ctx(nc, es)
        cst = mlstm_consts(c, es, cd)
        B = Buf()
        cc = c.sb(es, (128, 3, 128), BF16, "cc")
        for i in range(3):
            c.dma("pool", out=cc[:, i, :], in_=cin[:, i, :], writes=[B])
        cst["avgD"] = cc[:, 0, :]; cst["avgH"] = cc[:, 1, :]; cst["ones"] = cc[:, 2, :]
        cst["epsc"] = c.sb(es, (128, 1), F32, "epsc"); c.op("dve", lambda e: e.memset(cst["epsc"][:], EPS), writes=[B])
        cst["sel65"] = c.sb(es, (65, 64), F32, "sel65"); c.dma("sp", out=cst["sel65"][:], in_=seld, writes=[B])
        cst["tab"] = c.sb(es, (128, 24, 256), BF16, "tab")
        with ExitStack() as s0:
            tf = c.sb(s0, (128, 24, 256), F32, "tabf"); Bt = Buf()
            c.dma("sp", out=tf[:], in_=tabd, writes=[Bt])
            c.op("pool", lambda e: e.tensor_copy(out=cst["tab"][:], in_=tf[:]), reads=[Bt], writes=[B])
            c.barrier()
        pt = c.sb(es, (128, NL, NPAR), F32, "par"); c.dma("sp", out=pt[:], in_=par, writes=[B])
        gbt = c.sb(es, (4, NL, 2), F32, "gb"); c.dma("sp", out=gbt[:], in_=gb, writes=[B])
        c.dma("sp", out=yT, in_=xin, writes=[B])
        c.barrier()
        for l in range(nl):
            P = lambda a, b_: pt[:, l, a:b_]
            Bproj = bufs(29); Bmixed = bufs(12); Bhff = bufs(NFF)
            with ExitStack() as s1:
                hT = c.sb(s1, (128, 8, S), BF16, "hT")
                BhT = [bufs(8) for _ in range(NTT)]
                norm_pass(c, yT, hT, BhT, cst)
                inproj(c, w_in[l], P(0, 8), hT, BhT, projT, gatesT, Bproj)
            attention(c, projT, mixedT, cst, Bproj, Bmixed)
            mlstm(c, projT, gatesT, mixedT, Bproj, Bmixed, P(56, 96).rearrange("p (c f) -> p c f", f=5), gbt[:, l, :], cst)
            W = {"w_out": w_out[l], "wq": wq[l], "wk": wk[l], "wv": wv[l], "wo": wo[l]}
            prm = {"gmix": P(8, 16), "post_mix": P(16, 24), "pre_mem": P(24, 32), "post_mem": P(32, 40)}
            token_phase(c, l, W, prm, mixedT, Bmixed, yT, memT, cst)
            with ExitStack() as s1:
                hT = c.sb(s1, (128, 8, S), BF16, "hT")
                BhT = [bufs(8) for _ in range(NTT)]
                norm_pass(c, yT, hT, BhT, cst)
                ffn_up(c, w_up[l], P(40, 48), P(96, 272).rearrange("p (c f) -> p c f", f=4), hT, BhT, hffT, Bhff)
            ffn_down(c, w_down[l], P(48, 56), hffT, Bhff, yT, cst)
        c.barrier()
    return nc


def host_inputs(inp):
    col = lambda v: np.asarray(v, np.float32).reshape(-1, 128).T
    par = np.zeros((128, NL, NPAR), np.float32)
    gb = np.zeros((4, NL, 2), np.float32)
    for l in range(NL):
        par[:, l, 0:8] = col(inp["pre_mix_g"][l])
        par[:, l, 8:16] = col(np.concatenate([inp["attn_out_g"][l], inp["mlstm_out_g"][l]]))
        par[:, l, 16:24] = col(inp["post_mix_g"][l]); par[:, l, 24:32] = col(inp["pre_mem_g"][l]); par[:, l, 32:40] = col(inp["post_mem_g"][l])
        par[:, l, 40:48] = col(inp["pre_ffn_g"][l]); par[:, l, 48:56] = col(inp["post_ffn_g"][l])
        mc = np.concatenate([inp["mconv_w"][l], inp["mconv_b"][l][None]], 0)
        par[:, l, 56:96] = mc.reshape(5, 8, 128).transpose(2, 1, 0).reshape(128, 40)
        fc = np.concatenate([inp["fconv_w"][l], inp["fconv_b"][l][None]], 0)
        par[:, l, 96:272] = fc.reshape(4, 44, 128).transpose(2, 1, 0).reshape(128, 176)
        gb[:, l, 0] = inp["b_igate"][l]; gb[:, l, 1] = inp["b_fgate"][l]
    sel = np.zeros((65, 64), np.float32); sel[64] = 1.0
    cin = np.stack([np.full((128, 128), 1 / 1024), np.full((128, 128), 1 / 512), np.ones((128, 128))], 1).astype(np.float32)
    shared = {"par": par, "gb": gb, "cin": cin, "tab": make_tables(np.asarray(inp["rel_bias"], np.float32)), "sel65": sel}
    shared.update(mlstm_const_arrays())
    for k in ("w_in", "w_out", "wq_mem", "wk_mem", "wv_mem", "wo_mem", "w_up", "w_down"):
        shared[k] = np.ascontiguousarray(inp[k], dtype=np.float32)
    maps = []
    for b in range(4):
        m = dict(shared)
        m["xin"] = np.ascontiguousarray(np.asarray(inp["x"][b], np.float32).T)
        m["memT"] = np.ascontiguousarray(np.asarray(inp["mem"][b], np.float32).T)
        maps.append(m)
    return maps


def kernel(**inputs):
    nc = build_program(NL)
    maps = host_inputs(inputs)
    res = run_bass_kernel_spmd(nc, maps, core_ids=[0, 1, 2, 3])
    out = np.stack([np.ascontiguousarray(np.asarray(r["yT"]).T) for r in res.results], 0)
    return out.astype(np.float32)
```

```python
import numpy as np
from contextlib import ExitStack
import concourse.bass as bass
import concourse.mybir as mybir
from concourse.bass_utils import run_bass_kernel_spmd
from concourse.alu_op_type import AluOpType as ALU

AF = mybir.ActivationFunctionType
AX = mybir.AxisListType
F32 = mybir.dt.float32
BF16 = mybir.dt.bfloat16

S = 4096
D = 1024
NL = 4
TT = 512
NTT = S // TT
EPS = 1e-6
IN_COLS = 3592
DFF = 2816
NFF = DFF // 128
NMEM = 256


class Buf:
    __slots__ = ("w", "r")

    def __init__(self):
        self.w = None
        self.r = {}


def bufs(n):
    return [Buf() for _ in range(n)]


class Ctx:
    def __init__(self, nc, es, n_dma_sems=40):
        self.nc = nc
        self.es = es
        self.eng = dict(pe=nc.tensor, dve=nc.vector, act=nc.scalar, pool=nc.gpsimd, sp=nc.sync)
        self.esem = {}
        self.cnt = {}
        self.nsem = 0
        for e in self.eng:
            self._new_esem(e)
        self.waited = {}
        self.dsems = [es.enter_context(nc.semaphore(f"dq{i}")) for i in range(n_dma_sems)]
        self.dval = [0] * n_dma_sems
        self.dnext = 0
        self.n_hw = n_dma_sems - 6
        self.dnext_sw = 0
        self.recent_dma = {}
        self.uid = 0
        self.ninstr = 0

    def _new_esem(self, e):
        self.nsem += 1
        self.esem[e] = self.es.enter_context(self.nc.semaphore(f"e{e}{self.nsem}"))
        self.cnt[e] = 0

    def name(self, p):
        self.uid += 1
        return f"{p}_{self.uid}"

    def sb(self, st, shape, dtype, name="t"):
        return st.enter_context(self.nc.sbuf_tensor(self.name(name), list(shape), dtype))

    def ps(self, st, shape=(128, 512), dtype=F32, name="ps"):
        return st.enter_context(self.nc.psum_tensor(self.name(name), list(shape), dtype))

    def _wait(self, e, tok, raw=False):
        sem, val, owner = tok
        if owner == e and (e == "pe" or e == "sp"):
            return
        key = (e, id(sem))
        if self.waited.get(key, 0) >= val:
            return
        self.waited[key] = val
        self.eng[e].wait_ge(sem, val)
        self.ninstr += 1

    def _deps(self, e, reads, writes):
        for b in reads:
            if b.w is not None:
                self._wait(e, b.w, raw=True)
        for b in writes:
            if b.w is not None:
                self._wait(e, b.w)
            for t in b.r.values():
                self._wait(e, t)

    def _mark(self, tok, reads, writes):
        for b in reads:
            k = id(tok[0])
            o = b.r.get(k)
            if o is None or o[1] < tok[1]:
                b.r[k] = tok
        for b in writes:
            b.w = tok
            b.r = {}

    def op(self, e, fn, reads=(), writes=(), inc=True):
        self._deps(e, reads, writes)
        ins = fn(self.eng[e])
        self.ninstr += 1
        if inc:
            if self.cnt[e] >= 30000:
                self._new_esem(e)
            self.cnt[e] += 1
            ins.then_inc(self.esem[e], 1)
            tok = (self.esem[e], self.cnt[e], e)
        else:
            if self.cnt[e] >= 30000:
                self._new_esem(e)
            tok = (self.esem[e], self.cnt[e] + 1, e)
        self._mark(tok, reads, writes)
        return tok

    def dma(self, q, out, in_, reads=(), writes=()):
        if q == "pool":
            i = self.n_hw + self.dnext_sw
            self.dnext_sw = (self.dnext_sw + 1) % (len(self.dsems) - self.n_hw)
        else:
            i = self.dnext
            self.dnext = (i + 1) % self.n_hw
        sem = self.dsems[i]
        old = self.dval[i]
        self._deps(q, reads, writes)
        if old > 0:
            self._wait(q, (sem, old, None))
        ins = self.eng[q].dma_start(out=out, in_=in_)
        self.ninstr += 1
        self.dval[i] = old + 16
        ins.then_inc(sem, 16)
        tok = (sem, old + 16, "dma")
        self.recent_dma[id(sem)] = tok
        self._mark(tok, reads, writes)
        return tok

    def barrier(self, engines=("pe", "dve", "act", "pool", "sp")):
        toks = [(self.esem[e], self.cnt[e], e) for e in self.eng if self.cnt[e] > 0]
        toks += list(self.recent_dma.values())
        for e in engines:
            for t in toks:
                self._wait(e, t, raw=True)
        self.recent_dma = {}


import math


def norm_pass(c, xT, hT, BhT, cst):
    nc = c.nc
    xv = xT.rearrange("(kc p) t -> p kc t", p=128)
    with ExitStack() as st:
        xt = [c.sb(st, (128, 8, TT), F32, "xt") for _ in range(2)]
        sq = [c.sb(st, (128, 8, TT), BF16, "sq") for _ in range(2)]
        rs = [c.sb(st, (128, TT), F32, "rs") for _ in range(2)]
        pss = [c.ps(st) for _ in range(2)]
        Bxt = bufs(2); Bsq = [bufs(8) for _ in range(2)]; Brs = bufs(2); Bps = bufs(2)
        for tt in range(NTT):
            b = tt % 2
            sl = slice(tt * TT, (tt + 1) * TT)
            c.dma("sp", out=xt[b][:], in_=xv[:, :, sl], writes=[Bxt[b]])
            for kc in range(8):
                c.op("act", lambda e: e.activation(out=sq[b][:, kc, :], in_=xt[b][:, kc, :], func=AF.Square),
                     reads=[Bxt[b]], writes=[Bsq[b][kc]])
            for kc in range(8):
                c.op("pe", lambda e: e.matmul(pss[b][:], lhsT=cst["avgD"][:], rhs=sq[b][:, kc, :], start=(kc == 0), stop=(kc == 7)),
                     reads=[Bsq[b][kc]], writes=[Bps[b]] if kc == 0 else [], inc=(kc == 7))
            Bps[b].w = (c.esem["pe"], c.cnt["pe"], "pe")
            c.op("act", lambda e: e.activation(out=rs[b][:], in_=pss[b][:], func=AF.Sqrt, bias=cst["epsc"][:, 0:1]),
                 reads=[Bps[b]], writes=[Brs[b]])
            c.op("dve", lambda e: e.reciprocal(out=rs[b][:], in_=rs[b][:]),
                 reads=[Brs[b]], writes=[Brs[b]])
            for kc in range(8):
                en = "dve" if kc % 2 == 0 else "pool"
                c.op(en, lambda e: e.tensor_tensor(out=hT[:, kc, sl], in0=xt[b][:, kc, :], in1=rs[b][:], op=ALU.mult),
                     reads=[Bxt[b], Brs[b]], writes=[BhT[tt][kc]])
    c.barrier()


def inproj(c, w_in, gcol, hT, BhT, projT, gatesT, Bproj):
    wv = w_in.rearrange("(kc p) n -> p kc n", p=128)
    with ExitStack() as st:
        ws = [c.sb(st, (128, 8, 128), F32, "ws") for _ in range(2)]
        wb = [c.sb(st, (128, 8, 128), BF16, "wb") for _ in range(2)]
        ob = [c.sb(st, (128, S), BF16, "ob") for _ in range(2)]
        og = c.sb(st, (8, S), F32, "og")
        pss = [c.ps(st) for _ in range(4)]
        Bws = bufs(2); Bwb = [bufs(8) for _ in range(2)]; Bob = [bufs(NTT) for _ in range(2)]; Bps = bufs(4)
        Bog = bufs(NTT)
        k = 0
        for fc in range(29):
            ncol = 128 if fc < 28 else 8
            b = fc % 2
            c.dma("sp", out=ws[b][:, :, :ncol], in_=wv[:, :, fc * 128: fc * 128 + ncol], writes=[Bws[b]])
            c.op("pool", lambda e: e.tensor_tensor(out=wb[b][:, :, :ncol], in0=ws[b][:, :, :ncol], in1=gcol.unsqueeze(2).to_broadcast([128, 8, ncol]), op=ALU.mult),
                 reads=[Bws[b]], writes=Bwb[b])
            for tt in range(NTT):
                sl = slice(tt * TT, (tt + 1) * TT)
                p = k % 4
                for kc in range(8):
                    c.op("pe", lambda e: e.matmul(pss[p][:ncol, :], lhsT=wb[b][:, kc, :ncol], rhs=hT[:, kc, sl], start=(kc == 0), stop=(kc == 7)),
                         reads=[Bwb[b][kc], BhT[tt][kc]], writes=[Bps[p]] if kc == 0 else [], inc=(kc == 7))
                Bps[p].w = (c.esem["pe"], c.cnt["pe"], "pe")
                if fc == 28:
                    c.op("dve", lambda e: e.tensor_copy(out=og[:, sl], in_=pss[p][:8, :]), reads=[Bps[p]], writes=[Bog[tt]])
                elif fc < 4:
                    if k % 2 == 0:
                        c.op("act", lambda e: e.activation(out=ob[b][:, sl], in_=pss[p][:], func=AF.Copy, scale=0.125), reads=[Bps[p]], writes=[Bob[b][tt]])
                    else:
                        c.op("dve", lambda e: e.tensor_scalar(out=ob[b][:, sl], in0=pss[p][:], scalar1=0.125, scalar2=None, op0=ALU.mult), reads=[Bps[p]], writes=[Bob[b][tt]])
                else:
                    if k % 2 == 0:
                        c.op("act", lambda e: e.activation(out=ob[b][:, sl], in_=pss[p][:], func=AF.Copy), reads=[Bps[p]], writes=[Bob[b][tt]])
                    else:
                        c.op("dve", lambda e: e.tensor_copy(out=ob[b][:, sl], in_=pss[p][:]), reads=[Bps[p]], writes=[Bob[b][tt]])
                k += 1
            if fc == 28:
                c.dma("sp", out=gatesT[:, :], in_=og[:], reads=Bog, writes=[Bproj[28]])
            else:
                c.dma("sp", out=projT[fc * 128:(fc + 1) * 128, :], in_=ob[b][:], reads=Bob[b], writes=[Bproj[fc]])
    c.barrier()


def load_consts(c, st, cdram):
    cst = {}
    B = Buf()
    def ld(name, shape, dtype):
        t = c.sb(st, shape, dtype, name)
        c.dma("pool" if dtype == BF16 else "sp", out=t[:], in_=cdram[name], writes=[B])
        cst[name] = t
    ld("avgD", (128, 128), BF16)
    t = c.sb(st, (128, 1), F32, "epsc")
    c.op("dve", lambda e: e.memset(t[:], EPS), writes=[B])
    cst["epsc"] = t
    return cst, B


DILS = (1, 4, 16)

def attention(c, projT, mixedT, cst, Bproj, Bmixed):
    with ExitStack() as st:
        qkv = [c.sb(st, (128, S), BF16, "qkv") for _ in range(3)]
        perm = [[c.sb(st, (128, S), BF16, "perm") for _ in range(3)] for _ in range(2)]
        vtok = [c.sb(st, (128, 32, 2, 65), BF16, "vtok") for _ in range(3)]
        acc = [c.sb(st, (65, S), F32, "acc") for _ in range(2)]
        pt = [c.sb(st, (128, 2, 256), BF16, "pt") for _ in range(3)]
        rec = [c.sb(st, (64, TT), F32, "rec") for _ in range(2)]
        ao = [c.sb(st, (64, S), BF16, "ao") for _ in range(2)]
        sp = [c.ps(st, (128, 2, 256), F32, "sp") for _ in range(2)]
        tps = [c.ps(st, (128, 8, 128), BF16, "tp") for _ in range(2)]
        ops = [c.ps(st, (128, 4, 128), F32, "ops") for _ in range(3)]
        dps = ops[2:3]
        Bqkv = bufs(3); Bperm = [bufs(3) for _ in range(2)]; Bvt = [bufs(8) for _ in range(3)]
        Bacc = bufs(2); Bpt = bufs(3); Brec = bufs(2); Bao = [bufs(NTT) for _ in range(2)]
        Bsp = bufs(2); Btp = bufs(2); Bo = bufs(3); Bdps = Bo[2:3]
        ident = cst["ident"]; tab = cst["tab"]; sel = cst["sel65"]
        for pi in range(3):
            c.op("pool", lambda e: e.memset(vtok[pi][:, :, :, 64:65], 1.0), writes=Bvt[pi])
        kq = 0
        for hp in range(4):
            for i in range(3):
                c.dma("sp", out=qkv[i][:], in_=projT[i * 512 + hp * 128: i * 512 + hp * 128 + 128, :], reads=[Bproj[i * 4 + hp]], writes=[Bqkv[i]])
            for pi, dil in enumerate(DILS):
                nbc = 32 // dil
                if dil == 1:
                    src = qkv; Bsrc = Bqkv
                else:
                    src = perm[pi - 1]; Bsrc = Bperm[pi - 1]
                    for i in range(3):
                        c.op("pool", lambda e: e.tensor_copy(out=src[i][:].rearrange("p (c i) -> p c i", c=dil),
                                                             in_=qkv[i][:].rearrange("p (i c) -> p c i", c=dil)),
                             reads=[Bqkv[i]], writes=[Bsrc[i]])
                qP, kP, vP = src
                for g in range(8):
                    tb = g % 2
                    for j in range(4):
                        blk = g * 4 + j
                        c.op("pe", lambda e: e.transpose(out=tps[tb][:, j, :], in_=vP[:, blk * 128:(blk + 1) * 128], identity=ident[:]),
                             reads=[Bsrc[2]], writes=[Btp[tb]] if j == 0 else [], inc=(j == 3))
                    Btp[tb].w = (c.esem["pe"], c.cnt["pe"], "pe")
                    en = "dve" if g % 2 == 0 else "act"
                    o_ap = vtok[pi][:, g * 4:(g + 1) * 4, :, 0:64]
                    i_ap = tps[tb][:, 0:4, :].rearrange("p j (h d) -> p j h d", h=2)
                    if en == "dve":
                        c.op("dve", lambda e: e.tensor_copy(out=o_ap, in_=i_ap), reads=[Btp[tb]], writes=[Bvt[pi][g]])
                    else:
                        c.op("act", lambda e: e.activation(out=o_ap, in_=i_ap, func=AF.Copy), reads=[Btp[tb]], writes=[Bvt[pi][g]])
                for h in range(2):
                    hd = hp * 2 + h
                    rows = slice(h * 64, h * 64 + 64)
                    accv = acc[h][:, :].rearrange("p (i c) -> p c i", c=dil)
                    for cl in range(dil):
                        for n2 in range(0, nbc, 2):
                            s_ = kq % 2; p_ = kq % 3; kq += 1
                            for j in range(2):
                                n = n2 + j; gb = cl * nbc + n
                                nq = 256 if n < nbc - 1 else 128
                                c.op("pe", lambda e: e.matmul(sp[s_][:, j, :nq], lhsT=kP[rows, gb * 128:(gb + 1) * 128], rhs=qP[rows, gb * 128: gb * 128 + nq],
                                                              start=True, stop=False),
                                     reads=[Bsrc[0], Bsrc[1]], writes=[Bsp[s_]] if j == 0 else [], inc=False)
                                c.op("pe", lambda e: e.matmul(sp[s_][:, j, :nq], lhsT=ident[:], rhs=tab[:, hd * 3 + pi, :nq], start=False, stop=True),
                                     inc=(j == 1))
                            Bsp[s_].w = (c.esem["pe"], c.cnt["pe"], "pe")
                            last = (n2 + 1 == nbc - 1)
                            if not last:
                                c.op("act", lambda e: e.activation(out=pt[p_][:], in_=sp[s_][:], func=AF.Exp), reads=[Bsp[s_]], writes=[Bpt[p_]])
                            else:
                                c.op("act", lambda e: e.activation(out=pt[p_][:, 0, :], in_=sp[s_][:, 0, :], func=AF.Exp), reads=[Bsp[s_]], writes=[Bpt[p_]])
                                c.op("act", lambda e: e.activation(out=pt[p_][:, 1, :128], in_=sp[s_][:, 1, :128], func=AF.Exp), reads=[Bsp[s_]], writes=[])
                                Bpt[p_].w = (c.esem["act"], c.cnt["act"], "act")
                            for j in range(2):
                                n = n2 + j; gb = cl * nbc + n
                                oi = gb % 3
                                ot = ops[oi][:65, 0, :]
                                c.op("pe", lambda e: e.matmul(ot, lhsT=vtok[pi][:, gb, h, :], rhs=pt[p_][:, j, 0:128], start=(n == 0), stop=True),
                                     reads=[Bpt[p_], Bvt[pi][gb // 4]], writes=[Bo[oi]] if n == 0 else [], inc=True)
                                Bo[oi].w = (c.esem["pe"], c.cnt["pe"], "pe")
                                av = accv[:, cl, n * 128:(n + 1) * 128]
                                if pi == 0:
                                    c.op("dve", lambda e: e.tensor_copy(out=av, in_=ot), reads=[Bo[oi]], writes=[Bacc[h]])
                                else:
                                    c.op("dve", lambda e: e.tensor_tensor(out=av, in0=av, in1=ot, op=ALU.add), reads=[Bo[oi], Bacc[h]], writes=[Bacc[h]])
                                if n < nbc - 1:
                                    oi2 = (gb + 1) % 3
                                    ot2 = ops[oi2][:65, 0, :]
                                    c.op("pe", lambda e: e.matmul(ot2, lhsT=vtok[pi][:, gb, h, :], rhs=pt[p_][:, j, 128:256], start=True, stop=False),
                                         reads=[Bpt[p_], Bvt[pi][gb // 4]], writes=[Bo[oi2]], inc=False)
            for h in range(2):
                hd = hp * 2 + h
                for tt in range(NTT):
                    sl = slice(tt * TT, (tt + 1) * TT)
                    r_ = tt % 2
                    c.op("pe", lambda e: e.matmul(dps[0][:64, :, :], lhsT=sel[:65, :], rhs=acc[h][:65, sl], start=True, stop=True),
                         reads=[Bacc[h]], writes=[Bdps[0]])
                    c.op("dve", lambda e: e.reciprocal(out=rec[r_][:], in_=dps[0][:64, :, :].rearrange("p a b -> p (a b)")), reads=[Bdps[0]], writes=[Brec[r_]])
                    c.op("pool", lambda e: e.tensor_tensor(out=ao[h][:, sl], in0=acc[h][:64, sl], in1=rec[r_][:], op=ALU.mult),
                         reads=[Bacc[h], Brec[r_]], writes=[Bao[h][tt]])
                c.dma("sp", out=mixedT[hd * 64:(hd + 1) * 64, :], in_=ao[h][:], reads=Bao[h], writes=[Bmixed[hd]])
    c.barrier()


def t5_bucket(dist):
    dist = np.asarray(dist)
    max_exact = 16
    d_f = np.maximum(dist, 1).astype(np.float32)
    large = max_exact + (np.log(d_f / max_exact) / np.log(2048 / max_exact) * (32 - max_exact)).astype(np.int32)
    large = np.minimum(large, 31)
    return np.where(dist < max_exact, dist, large)


def make_tables(rel_bias):
    s = np.arange(128)[:, None]
    t = np.arange(128)[None, :]
    tabs = np.full((128, 24, 256), -30000.0, np.float32)
    for pi, dil in enumerate(DILS):
        bsub = rel_bias[t5_bucket(np.arange(129) * dil)]
        d0 = t - s
        d1 = t + 128 - s
        for h in range(8):
            tabs[:, h * 3 + pi, 0:128] = np.where(d0 >= 0, bsub[np.clip(d0, 0, 128), h], -30000.0)
            tabs[:, h * 3 + pi, 128:256] = np.where(d1 <= 128, bsub[np.clip(d1, 0, 128), h], -30000.0)
    return tabs


def mlstm(c, projT, gatesT, mixedT, Bproj, Bmixed, mcp, gbias, cst):
    ident = cst["ident"]; identf = cst["identf"]; cm = cst["cmask"]
    with ExitStack() as st:
        et = c.sb(st, (128, 32, 8), F32, "et"); Bet = Buf()
        decb = c.sb(st, (128, 128), F32, "decb"); Bdecb = Buf()
        with ExitStack() as s0:
            gi = c.sb(s0, (4, S), F32, "gi"); gf = c.sb(s0, (4, S), F32, "gf"); nb = c.sb(s0, (4, S), F32, "nb")
            dmb = c.sb(s0, (4, S), F32, "dmb"); t1 = c.sb(s0, (4, S), F32, "t1"); t2 = c.sb(s0, (4, S), F32, "t2")
            sm = c.sb(s0, (4, 8, 32), F32, "sm")
            decbd = c.sb(s0, (4, 4, 32), F32, "decbd"); nbf = c.sb(s0, (4, 2), F32, "nbf")
            pg = c.ps(s0, (128, 32, 8), F32, "pg"); pd = c.ps(s0, (128, 128), F32, "pd")
            G = Buf()
            c.dma("sp", out=gi[:], in_=gatesT[0:4, :], reads=[Bproj[28]], writes=[G])
            c.dma("sp", out=gf[:], in_=gatesT[4:8, :], reads=[Bproj[28]], writes=[G])
            def g(en, fn):
                c.op(en, fn, reads=[G], writes=[G])
            g("dve", lambda e: e.tensor_scalar(out=nbf[:, 0:1], in0=gbias[:, 1:2], scalar1=-1.0, scalar2=None, op0=ALU.mult))
            g("dve", lambda e: e.memset(nbf[:, 1:2], -0.5 * math.log(128.0)))
            g("act", lambda e: e.activation(out=t1[:], in_=gf[:], func=AF.Exp, scale=-1.0, bias=nbf[:, 0:1]))
            g("act", lambda e: e.activation(out=t1[:], in_=t1[:], func=AF.Ln, bias=cst["one1"][:4, 0:1]))
            g("dve", lambda e: e.tensor_tensor_scan(out=nb[:], data0=cst["rmask"][:4, :], data1=t1[:], initial=0.0, op0=ALU.mult, op1=ALU.add))
            g("dve", lambda e: e.scalar_tensor_tensor(out=dmb[:], in0=gi[:], scalar=gbias[:, 0:1], in1=nb[:], op0=ALU.add, op1=ALU.add))
            g("dve", lambda e: e.tensor_reduce(out=sm[:, 0, :], in_=dmb[:].rearrange("p (c s) -> p c s", s=128), axis=AX.X, op=ALU.max))
            g("dve", lambda e: e.tensor_scalar(out=sm[:, 1, :], in0=nb[:].rearrange("p (c s) -> p c s", s=128)[:, :, 127], scalar1=-1.0, scalar2=None, op0=ALU.mult))
            g("dve", lambda e: e.tensor_tensor_scan(out=sm[:, 2, :], data0=sm[:, 0, :], data1=sm[:, 1, :], initial=0.0, op0=ALU.max, op1=ALU.add))
            g("dve", lambda e: e.memset(sm[:, 3, 0:1], 0.0))
            g("dve", lambda e: e.tensor_copy(out=sm[:, 3, 1:32], in_=sm[:, 2, 0:31]))
            g("dve", lambda e: e.tensor_tensor(out=sm[:, 4, :], in0=sm[:, 3, :], in1=sm[:, 0, :], op=ALU.max))
            g("dve", lambda e: e.tensor_tensor(out=sm[:, 5, :], in0=sm[:, 3, :], in1=sm[:, 4, :], op=ALU.subtract))
            g("act", lambda e: e.activation(out=sm[:, 5, :], in_=sm[:, 5, :], func=AF.Exp))
            Mb = sm[:, 4, :].unsqueeze(2).to_broadcast([4, 32, 128])
            g("dve", lambda e: e.tensor_tensor(out=t1[:].rearrange("p (c s) -> p c s", s=128), in0=dmb[:].rearrange("p (c s) -> p c s", s=128), in1=Mb, op=ALU.subtract))
            g("act", lambda e: e.activation(out=t1[:], in_=t1[:], func=AF.Exp, bias=nbf[:, 1:2]))
            g("dve", lambda e: e.tensor_tensor(out=t2[:].rearrange("p (c s) -> p c s", s=128), in0=nb[:].rearrange("p (c s) -> p c s", s=128), in1=Mb, op=ALU.subtract))
            g("act", lambda e: e.activation(out=t2[:], in_=t2[:], func=AF.Exp))
            for cc in range(32):
                cs = slice(cc * 128, (cc + 1) * 128)
                c.op("pe", lambda e: e.transpose(out=pg[:, cc, 0:4], in_=t1[:, cs], identity=identf[:4, :4]), reads=[G], writes=[G] if cc == 0 else [], inc=False)
                c.op("pe", lambda e: e.transpose(out=pg[:, cc, 4:8], in_=t2[:, cs], identity=identf[:4, :4]), reads=[G], inc=(cc == 31))
            G.w = (c.esem["pe"], c.cnt["pe"], "pe")
            c.op("dve", lambda e: e.tensor_copy(out=et[:], in_=pg[:]), reads=[G], writes=[Bet])
            g("dve", lambda e: e.tensor_tensor(out=decbd[:], in0=sm[:, 5, :].unsqueeze(1).to_broadcast([4, 4, 32]), in1=cst["bdmask"][:4, :].rearrange("p (a b) -> p a b", a=4), op=ALU.mult))
            c.op("pe", lambda e: e.matmul(pd[:], lhsT=cst["ones4"][:4, :], rhs=decbd[:].rearrange("p a b -> p (a b)"), start=True, stop=True), reads=[G], writes=[G])
            c.op("dve", lambda e: e.tensor_copy(out=decb[:], in_=pd[:]), reads=[G], writes=[Bdecb])
            c.barrier()
        raw = [c.sb(st, (128, 3 + S), BF16, "raw") for _ in range(2)]; Braw = bufs(2)
        cv = [c.sb(st, (128, S), F32, "cv") for _ in range(2)]; Bcv = bufs(2)
        qT = c.sb(st, (128, S), BF16, "qT"); kT = c.sb(st, (128, S), BF16, "kT"); BqT = Buf(); BkT = Buf()
        vT = c.sb(st, (128, S), BF16, "vT"); oT = c.sb(st, (128, S), BF16, "oT"); BvT = Buf(); BoT = Buf()
        hmT = c.sb(st, (128, S), BF16, "hmT"); BhmT = bufs(32)
        ktk = [c.sb(st, (128, 128), BF16, "ktk") for _ in range(2)]; Bktk = bufs(2)
        vaug = [c.sb(st, (128, 129), BF16, "vaug") for _ in range(2)]; Bvaug = bufs(2)
        og = [c.sb(st, (128, 128), F32, "og") for _ in range(2)]; Bog = bufs(2)
        pT = [c.sb(st, (128, 128), BF16, "pT") for _ in range(2)]; BpT = bufs(2)
        hmt = [c.sb(st, (128, 128), BF16, "hmt") for _ in range(2)]; Bhmt = bufs(2)
        C = c.sb(st, (128, 129), F32, "C"); BC = Buf()
        Cbf = [c.sb(st, (128, 129), BF16, "Cbf") for _ in range(2)]; BCbf = bufs(2)
        dd = [c.sb(st, (128, 2), F32, "dd") for _ in range(2)]; Bdd = bufs(2)
        tpk = [c.ps(st, (128, 8, 128), BF16, "tpk") for _ in range(2)]; Btpk = bufs(2)
        sps = c.ps(st, (128, 512), F32, "sps"); Bsps = Buf()
        nps = [c.ps(st, (128, 512), F32, "nps") for _ in range(2)]; Bnps = bufs(2)
        dps = c.ps(st, (128, 512), F32, "dps"); Bdps = Buf()
        tph = c.ps(st, (128, 8, 128), BF16, "tph"); Btph = Buf()
        for r in range(2):
            c.op("pool", lambda e: e.memset(raw[r][:, 0:3], 0.0), writes=[Braw[r]])
            c.op("pool", lambda e: e.memset(vaug[r][:, 128:129], 1.0), writes=[Bvaug[r]])
        for hd in range(4):
            c.dma("sp", out=raw[0][:, 3:], in_=projT[1536 + hd * 128:1536 + (hd + 1) * 128, :], reads=[Bproj[12 + hd]], writes=[Braw[0]])
            c.dma("sp", out=raw[1][:, 3:], in_=projT[2048 + hd * 128:2048 + (hd + 1) * 128, :], reads=[Bproj[16 + hd]], writes=[Braw[1]])
            c.dma("sp", out=vT[:], in_=projT[2560 + hd * 128:2560 + (hd + 1) * 128, :], reads=[Bproj[20 + hd]], writes=[BvT])
            c.dma("sp", out=oT[:], in_=projT[3072 + hd * 128:3072 + (hd + 1) * 128, :], reads=[Bproj[24 + hd]], writes=[BoT])
            for i, en, dst, Bdst in ((0, "dve", qT, BqT), (1, "dve", kT, BkT)):
                ci = i * 4 + hd
                c.op(en, lambda e: e.tensor_scalar(out=cv[i][:], in0=raw[i][:, 3:3 + S], scalar1=mcp[:, ci, 3:4], scalar2=mcp[:, ci, 4:5], op0=ALU.mult, op1=ALU.add),
                     reads=[Braw[i]], writes=[Bcv[i]])
                for j in range(3):
                    c.op(en, lambda e: e.scalar_tensor_tensor(out=cv[i][:], in0=raw[i][:, j:j + S], scalar=mcp[:, ci, j:j + 1], in1=cv[i][:], op0=ALU.mult, op1=ALU.add),
                         reads=[Braw[i], Bcv[i]], writes=[Bcv[i]])
                c.op("act", lambda e: e.activation(out=dst[:], in_=cv[i][:], func=AF.Silu), reads=[Bcv[i]], writes=[Bdst])
            for cc in range(32):
                r = cc % 2
                cs = slice(cc * 128, (cc + 1) * 128)
                ecol = et[:, cc, hd:hd + 1]; fcol = et[:, cc, 4 + hd:5 + hd]
                for j, (src, Bsrc) in enumerate(((kT, BkT), (vT, BvT), (oT, BoT))):
                    c.op("pe", lambda e: e.transpose(out=tpk[r][:, j, :], in_=src[:, cs], identity=ident[:]), reads=[Bsrc], writes=[Btpk[r]] if j == 0 else [], inc=(j == 2))
                Btpk[r].w = (c.esem["pe"], c.cnt["pe"], "pe")
                c.op("dve", lambda e: e.tensor_scalar(out=ktk[r][:], in0=tpk[r][:, 0, :], scalar1=ecol, scalar2=None, op0=ALU.mult), reads=[Btpk[r], Bet], writes=[Bktk[r]])
                c.op("act", lambda e: e.activation(out=vaug[r][:, 0:128], in_=tpk[r][:, 1, :], func=AF.Copy), reads=[Btpk[r]], writes=[Bvaug[r]])
                c.op("act", lambda e: e.activation(out=og[r][:], in_=tpk[r][:, 2, :], func=AF.Sigmoid), reads=[Btpk[r]], writes=[Bog[r]])
                c.op("pe", lambda e: e.matmul(sps[:, 0:128], lhsT=kT[:, cs], rhs=qT[:, cs], start=True, stop=True), reads=[BkT, BqT], writes=[Bsps])
                c.op("dve", lambda e: e.scalar_tensor_tensor(out=pT[r][:], in0=sps[:, 0:128], scalar=ecol, in1=cm[:], op0=ALU.mult, op1=ALU.mult), reads=[Bsps, Bet], writes=[BpT[r]])
                if cc > 0:
                    dcol = decb[:, hd * 32 + cc: hd * 32 + cc + 1]
                    c.op("dve", lambda e: e.tensor_scalar(out=C[:], in0=C[:], scalar1=dcol, scalar2=None, op0=ALU.mult), reads=[BC, Bdecb], writes=[BC])
                    c.op("act", lambda e: e.activation(out=Cbf[r][:], in_=C[:], func=AF.Copy), reads=[BC], writes=[BCbf[r]])
                c.op("pe", lambda e: e.matmul(nps[r][:, 0:129], lhsT=pT[r][:], rhs=vaug[r][:], start=True, stop=(cc == 0)), reads=[BpT[r], Bvaug[r]], writes=[Bnps[r]], inc=(cc == 0))
                if cc > 0:
                    c.op("pe", lambda e: e.matmul(nps[r][:, 0:129], lhsT=qT[:, cs], rhs=Cbf[r][:], start=False, stop=True), reads=[BqT, BCbf[r]], inc=True)
                Bnps[r].w = (c.esem["pe"], c.cnt["pe"], "pe")
                c.op("pe", lambda e: e.matmul(dps[:, 0:129], lhsT=ktk[r][:], rhs=vaug[r][:], start=True, stop=True), reads=[Bktk[r], Bvaug[r]], writes=[Bdps])
                if cc == 0:
                    c.op("dve", lambda e: e.tensor_copy(out=C[:], in_=dps[:, 0:129]), reads=[Bdps], writes=[BC])
                else:
                    c.op("dve", lambda e: e.tensor_tensor(out=C[:], in0=C[:], in1=dps[:, 0:129], op=ALU.add), reads=[Bdps, BC], writes=[BC])
                c.op("dve", lambda e: e.tensor_scalar(out=dd[r][:, 1:2], in0=nps[r][:, 128:129], scalar1=-1.0, scalar2=None, op0=ALU.mult), reads=[Bnps[r]], writes=[Bdd[r]])
                c.op("dve", lambda e: e.scalar_tensor_tensor(out=dd[r][:, 0:1], in0=nps[r][:, 128:129], scalar=fcol, in1=dd[r][:, 1:2], op0=ALU.max, op1=ALU.max), reads=[Bnps[r], Bet, Bdd[r]], writes=[Bdd[r]])
                c.op("dve", lambda e: e.reciprocal(out=dd[r][:, 1:2], in_=dd[r][:, 0:1]), reads=[Bdd[r]], writes=[Bdd[r]])
                c.op("dve", lambda e: e.scalar_tensor_tensor(out=hmt[r][:], in0=nps[r][:, 0:128], scalar=dd[r][:, 1:2], in1=og[r][:], op0=ALU.mult, op1=ALU.mult),
                     reads=[Bnps[r], Bdd[r], Bog[r]], writes=[Bhmt[r]])
                c.op("pe", lambda e: e.transpose(out=tph[:, 0, :], in_=hmt[r][:], identity=ident[:]), reads=[Bhmt[r]], writes=[Btph])
                c.op("act", lambda e: e.activation(out=hmT[:, cs], in_=tph[:, 0, :], func=AF.Copy), reads=[Btph], writes=[BhmT[cc]])
            c.dma("sp", out=mixedT[512 + hd * 128: 512 + (hd + 1) * 128, :], in_=hmT[:], reads=BhmT, writes=[Bmixed[8 + hd]])
    c.barrier()


def mlstm_consts(c, st, cd):
    cst = {}; B = Buf()
    cst["identf"] = c.sb(st, (128, 128), F32, "identf"); c.dma("sp", out=cst["identf"][:], in_=cd["identf"], writes=[B])
    cst["ident"] = c.sb(st, (128, 128), BF16, "ident"); c.dma("pool", out=cst["ident"][:], in_=cd["identf"], writes=[B])
    cst["cmask"] = c.sb(st, (128, 128), F32, "cmask"); c.dma("sp", out=cst["cmask"][:], in_=cd["cmask"], writes=[B])
    cst["rmask"] = c.sb(st, (4, S), F32, "rmask"); c.dma("sp", out=cst["rmask"][:], in_=cd["rmask"], writes=[B])
    cst["bdmask"] = c.sb(st, (4, 128), F32, "bdmask"); c.dma("sp", out=cst["bdmask"][:], in_=cd["bdmask"], writes=[B])
    cst["ones4"] = c.sb(st, (4, 128), F32, "ones4"); c.dma("sp", out=cst["ones4"][:], in_=cd["ones4"], writes=[B])
    cst["one1"] = c.sb(st, (128, 1), F32, "one1"); c.op("dve", lambda e: e.memset(cst["one1"][:], 1.0), writes=[B])
    return cst


def mlstm_const_arrays():
    t = np.arange(128)
    cmask = (t[None, :] >= t[:, None]).astype(np.float32)
    rmask = np.ones((4, S), np.float32); rmask[:, ::128] = 0.0
    bd = np.zeros((4, 4, 32), np.float32)
    for h in range(4):
        bd[h, h] = 1.0
    return {"identf": np.eye(128, dtype=np.float32), "cmask": cmask, "rmask": rmask, "bdmask": bd.reshape(4, 128), "ones4": np.ones((4, 128), np.float32)}


def ffn_up(c, w_up, gcol, fcp, hT, BhT, hffT, Bhff):
    wv = w_up.rearrange("(kc p) n -> p kc n", p=128)
    with ExitStack() as st:
        ws = [c.sb(st, (128, 8, 128), F32, "ws") for _ in range(2)]
        wb = [c.sb(st, (128, 8, 128), BF16, "wb") for _ in range(2)]
        u = [[c.sb(st, (128, 2 + S), BF16, "u") for _ in range(2)] for _ in range(2)]
        cv = [c.sb(st, (128, S), F32, "cv") for _ in range(2)]
        gg = c.sb(st, (128, S), BF16, "gg")
        ho = [c.sb(st, (128, S), BF16, "ho") for _ in range(2)]
        pss = [c.ps(st) for _ in range(4)]
        Bws = bufs(2); Bwb = [bufs(8) for _ in range(2)]; Bu = [[bufs(NTT) for _ in range(2)] for _ in range(2)]
        Bcv = bufs(2); Bgg = Buf(); Bho = bufs(2); Bps = bufs(4)
        for par in range(2):
            for half in range(2):
                c.op("pool", lambda e: e.memset(u[par][half][:, 0:2], 0.0), writes=Bu[par][half])
        k = 0

        def ld_w(jj, half):
            ci_ = jj + NFF * half
            c.dma("sp", out=ws[half][:], in_=wv[:, :, ci_ * 128:(ci_ + 1) * 128], writes=[Bws[half]])

        ld_w(0, 0); ld_w(0, 1)
        for j in range(NFF):
            par = j % 2
            for half in range(2):
                ci = j + NFF * half
                b = half
                c.op("pool", lambda e: e.tensor_tensor(out=wb[b][:], in0=ws[b][:], in1=gcol.unsqueeze(2).to_broadcast([128, 8, 128]), op=ALU.mult),
                     reads=[Bws[b]], writes=Bwb[b])
                for tt in range(NTT):
                    sl = slice(tt * TT, (tt + 1) * TT)
                    p = k % 4
                    for kc in range(8):
                        c.op("pe", lambda e: e.matmul(pss[p][:], lhsT=wb[b][:, kc, :], rhs=hT[:, kc, sl], start=(kc == 0), stop=(kc == 7)),
                             reads=[Bwb[b][kc], BhT[tt][kc]], writes=[Bps[p]] if kc == 0 else [], inc=(kc == 7))
                    Bps[p].w = (c.esem["pe"], c.cnt["pe"], "pe")
                    o_ap = u[par][half][:, 2 + tt * TT: 2 + (tt + 1) * TT]
                    c.op("act", lambda e: e.activation(out=o_ap, in_=pss[p][:], func=AF.Copy), reads=[Bps[p]], writes=[Bu[par][half][tt]])
                    k += 1
                if j + 1 < NFF:
                    ld_w(j + 1, half)
            for half, en in ((0, "dve"), (1, "dve")):
                ci = j + NFF * half
                uu = u[par][half]
                c.op(en, lambda e: e.tensor_scalar(out=cv[half][:], in0=uu[:, 2:2 + S], scalar1=fcp[:, ci, 2:3], scalar2=fcp[:, ci, 3:4], op0=ALU.mult, op1=ALU.add),
                     reads=Bu[par][half], writes=[Bcv[half]])
                c.op(en, lambda e: e.scalar_tensor_tensor(out=cv[half][:], in0=uu[:, 1:1 + S], scalar=fcp[:, ci, 1:2], in1=cv[half][:], op0=ALU.mult, op1=ALU.add),
                     reads=Bu[par][half] + [Bcv[half]], writes=[Bcv[half]])
                c.op(en, lambda e: e.scalar_tensor_tensor(out=cv[half][:], in0=uu[:, 0:S], scalar=fcp[:, ci, 0:1], in1=cv[half][:], op0=ALU.mult, op1=ALU.add),
                     reads=Bu[par][half] + [Bcv[half]], writes=[Bcv[half]])
            c.op("act", lambda e: e.activation(out=gg[:], in_=cv[1][:], func=AF.Gelu_apprx_tanh), reads=[Bcv[1]], writes=[Bgg])
            c.op("dve", lambda e: e.tensor_tensor(out=ho[par][:], in0=gg[:], in1=cv[0][:], op=ALU.mult), reads=[Bgg, Bcv[0]], writes=[Bho[par]])
            c.dma("sp", out=hffT[j * 128:(j + 1) * 128, :], in_=ho[par][:], reads=[Bho[par]], writes=[Bhff[j]])
    c.barrier()


def postnorm_residual(c, st_bufs, hsrc, Bh, xt, Bx, gcol, cst, tag):
    sq, Bsq, ps, Bps, rs, Brs, tmp, Btmp = st_bufs
    for oc in range(8):
        c.op("act", lambda e: e.activation(out=sq[:, oc, :], in_=hsrc[:, oc, :], func=AF.Square), reads=[Bh[oc]], writes=[Bsq[oc]])
    for oc in range(8):
        c.op("pe", lambda e: e.matmul(ps[:], lhsT=cst["avgD"][:], rhs=sq[:, oc, :], start=(oc == 0), stop=(oc == 7)),
             reads=[Bsq[oc]], writes=[Bps] if oc == 0 else [], inc=(oc == 7))
    Bps.w = (c.esem["pe"], c.cnt["pe"], "pe")
    c.op("act", lambda e: e.activation(out=rs[:], in_=ps[:], func=AF.Sqrt, bias=cst["epsc"][:, 0:1]), reads=[Bps], writes=[Brs])
    c.op("dve", lambda e: e.reciprocal(out=rs[:], in_=rs[:]), reads=[Brs], writes=[Brs])
    for oc in range(8):
        en = "dve" if oc % 2 == 0 else "pool"
        c.op(en, lambda e: e.tensor_tensor(out=tmp[:, oc, :], in0=hsrc[:, oc, :], in1=rs[:], op=ALU.mult), reads=[Bh[oc], Brs], writes=[Btmp[oc]])
        c.op("dve", lambda e: e.scalar_tensor_tensor(out=xt[:, oc, :], in0=tmp[:, oc, :], scalar=gcol[:, oc:oc + 1], in1=xt[:, oc, :], op0=ALU.mult, op1=ALU.add),
             reads=[Btmp[oc], Bx[oc]], writes=[Bx[oc]])


def ffn_down(c, w_down, gpost, hffT, Bhff, xT, cst):
    wv = w_down.rearrange("(c p) n -> p c n", p=128)
    hv = hffT.rearrange("(c p) t -> p c t", p=128)
    xv = xT.rearrange("(kc p) t -> p kc t", p=128)
    with ExitStack() as st:
        wd = c.sb(st, (128, NFF, D), BF16, "wd")
        wst = [c.sb(st, (128, NFF, 128), F32, "wst") for _ in range(1)]
        hf = [c.sb(st, (128, NFF, TT), BF16, "hf") for _ in range(2)]
        xt = [c.sb(st, (128, 8, TT), F32, "xt") for _ in range(2)]
        hd = c.sb(st, (128, 8, TT), F32, "hd")
        sq = c.sb(st, (128, 8, TT), BF16, "sq"); rs = c.sb(st, (128, TT), F32, "rs")
        pss = [c.ps(st) for _ in range(3)]
        psn = c.ps(st)
        Bwd = bufs(8); Bwst = bufs(1); Bhf = bufs(2); Bxt = [bufs(8) for _ in range(2)]; Bhd = bufs(8)
        Bps = bufs(3)
        sb_ = (sq, bufs(8), psn, Buf(), rs, Buf(), hd, Bhd)
        for oc in range(8):
            b = 0
            c.dma("sp", out=wst[b][:], in_=wv[:, :, oc * 128:(oc + 1) * 128], writes=[Bwst[b]])
            c.op("pool", lambda e: e.tensor_copy(out=wd[:, :, oc * 128:(oc + 1) * 128], in_=wst[b][:]), reads=[Bwst[b]], writes=[Bwd[oc]])
        k = 0
        for tt in range(NTT):
            b = tt % 2
            sl = slice(tt * TT, (tt + 1) * TT)
            c.dma("sp", out=hf[b][:], in_=hv[:, :, sl], reads=Bhff, writes=[Bhf[b]])
            c.dma("sp", out=xt[b][:], in_=xv[:, :, sl], writes=Bxt[b])
            for oc in range(8):
                p = k % 3; k += 1
                for fc in range(NFF):
                    c.op("pe", lambda e: e.matmul(pss[p][:], lhsT=wd[:, fc, oc * 128:(oc + 1) * 128], rhs=hf[b][:, fc, :], start=(fc == 0), stop=(fc == NFF - 1)),
                         reads=[Bwd[oc], Bhf[b]], writes=[Bps[p]] if fc == 0 else [], inc=(fc == NFF - 1))
                Bps[p].w = (c.esem["pe"], c.cnt["pe"], "pe")
                c.op("dve", lambda e: e.tensor_copy(out=hd[:, oc, :], in_=pss[p][:]), reads=[Bps[p]], writes=[Bhd[oc]])
            postnorm_residual(c, sb_, hd, Bhd, xt[b], Bxt[b], gpost, cst, "ffn")
            c.dma("sp", out=xv[:, :, sl], in_=xt[b][:], reads=Bxt[b])
    c.barrier()


def token_phase(c, l, W, prm, mixedT, Bmixed, xT, memT, cst):
    mv = mixedT.rearrange("(kc p) t -> p kc t", p=128)
    xv = xT.rearrange("(kc p) t -> p kc t", p=128)
    with ExitStack() as st:
        wres = {n: c.sb(st, (128, 8, D), BF16, n) for n in ("w_out", "wq", "wo")}
        Bwres = {n: bufs(8) for n in wres}
        KT = c.sb(st, (128, 8, NMEM), BF16, "KT"); V = c.sb(st, (128, 2, D), BF16, "V"); mem = c.sb(st, (128, 8, NMEM), BF16, "mem")
        BKT = bufs(8); BV = bufs(8); Bmem = Buf()
        ws = [c.sb(st, (128, 8, 128), F32, "ws") for _ in range(2)]; Bws = bufs(2)
        wtmp = [c.sb(st, (128, 8, 128), BF16, "wtmp") for _ in range(2)]; Bwtmp = bufs(2)
        mx = c.sb(st, (128, 8, TT), BF16, "mx"); Bmx = bufs(8)
        sq = c.sb(st, (128, 8, TT), BF16, "sq"); Bsq = bufs(8)
        hd = c.sb(st, (128, 8, TT), F32, "hd"); Bhd = bufs(8)
        xt = [c.sb(st, (128, 8, TT), F32, "xt") for _ in range(2)]; Bxt = [bufs(8) for _ in range(2)]
        qT = c.sb(st, (128, 8, TT), BF16, "qT"); BqT = bufs(8)
        on = c.sb(st, (128, 8, TT), BF16, "on"); Bon = bufs(8)
        P = [c.sb(st, (128, TT), BF16, "P") for _ in range(2)]; BP = bufs(2)
        rs = c.sb(st, (128, TT), F32, "rs"); Brs = Buf()
        rA = c.sb(st, (128, TT), F32, "rA"); BrA = Buf()
        rM = c.sb(st, (128, TT), F32, "rM"); BrM = Buf()
        rden = c.sb(st, (128, TT), F32, "rden"); Brden = Buf()
        NPS = 7
        pss = [c.ps(st) for _ in range(NPS)]; Bps = bufs(NPS)
        psn = c.ps(st); Bpsn = Buf()
        pk = [0]

        def nextps():
            i = pk[0] % NPS; pk[0] += 1
            return pss[i], Bps[i]

        def mm_group(ps, Bp, parts, reads_list):
            n = len(parts)
            for i, (lh, rh) in enumerate(parts):
                c.op("pe", lambda e: e.matmul(ps, lhsT=lh, rhs=rh, start=(i == 0), stop=(i == n - 1)),
                     reads=reads_list[i], writes=[Bp] if i == 0 else [], inc=(i == n - 1))
            Bp.w = (c.esem["pe"], c.cnt["pe"], "pe")

        with ExitStack() as s0:
            mf = c.sb(s0, (128, 8, NMEM), F32, "mf"); Bmf = Buf()
            c.dma("sp", out=mf[:], in_=memT.rearrange("(kc p) m -> p kc m", p=128), writes=[Bmf])
            c.op("pool", lambda e: e.tensor_copy(out=mem[:], in_=mf[:]), reads=[Bmf], writes=[Bmem])
            c.barrier()
        kw = 0
        for name, g in (("w_out", prm["gmix"]), ("wq", prm["pre_mem"]), ("wo", None)):
            wv_ = W[name].rearrange("(kc p) n -> p kc n", p=128)
            for oc in range(8):
                b = kw % 2; kw += 1
                c.dma("sp", out=ws[b][:], in_=wv_[:, :, oc * 128:(oc + 1) * 128], writes=[Bws[b]])
                if g is None:
                    c.op("pool", lambda e: e.tensor_copy(out=wres[name][:, :, oc * 128:(oc + 1) * 128], in_=ws[b][:]), reads=[Bws[b]], writes=[Bwres[name][oc]])
                else:
                    c.op("pool", lambda e: e.tensor_tensor(out=wres[name][:, :, oc * 128:(oc + 1) * 128], in0=ws[b][:], in1=g.unsqueeze(2).to_broadcast([128, 8, 128]), op=ALU.mult),
                         reads=[Bws[b]], writes=[Bwres[name][oc]])
        for name in ("wk", "wv"):
            wv_ = W[name].rearrange("(kc p) n -> p kc n", p=128)
            for oc in range(8):
                b = kw % 2; kw += 1
                c.dma("sp", out=ws[b][:], in_=wv_[:, :, oc * 128:(oc + 1) * 128], writes=[Bws[b]])
                c.op("pool", lambda e: e.tensor_copy(out=wtmp[b][:], in_=ws[b][:]), reads=[Bws[b]], writes=[Bwtmp[b]])
                if name == "wk":
                    ps, Bp = nextps()
                    mm_group(ps[:, :NMEM], Bp, [(wtmp[b][:, kc, :], mem[:, kc, :]) for kc in range(8)], [[Bwtmp[b], Bmem]] * 8)
                    c.op("dve", lambda e: e.tensor_copy(out=KT[:, oc, :], in_=ps[:, :NMEM]), reads=[Bp], writes=[BKT[oc]])
                else:
                    for mb in range(2):
                        ps, Bp = nextps()
                        mm_group(ps[:, :128], Bp, [(mem[:, kc, mb * 128:(mb + 1) * 128], wtmp[b][:, kc, :]) for kc in range(8)], [[Bwtmp[b], Bmem]] * 8)
                        c.op("dve", lambda e: e.tensor_copy(out=V[:, mb, oc * 128:(oc + 1) * 128], in_=ps[:, :128]), reads=[Bp], writes=[BV[oc]])
        sb_ = (sq, Bsq, psn, Bpsn, rs, Brs, hd, Bhd)
        ek = 0
        for tt in range(NTT):
            b = tt % 2
            sl = slice(tt * TT, (tt + 1) * TT)
            c.dma("sp", out=mx[:], in_=mv[:, :, sl], reads=Bmixed, writes=Bmx)
            c.dma("sp", out=xt[b][:], in_=xv[:, :, sl], writes=Bxt[b])
            for kc in range(8):
                c.op("act", lambda e: e.activation(out=sq[:, kc, :], in_=mx[:, kc, :], func=AF.Square), reads=[Bmx[kc]], writes=[Bsq[kc]])
            for grp, (rr, Brr) in enumerate(((rA, BrA), (rM, BrM))):
                ps, Bp = nextps()
                mm_group(ps[:], Bp, [(cst["avgH"][:], sq[:, grp * 4 + i, :]) for i in range(4)], [[Bsq[grp * 4 + i]] for i in range(4)])
                c.op("act", lambda e: e.activation(out=rr[:], in_=ps[:], func=AF.Sqrt, bias=cst["epsc"][:, 0:1]), reads=[Bp], writes=[Brr])
                c.op("dve", lambda e: e.reciprocal(out=rr[:], in_=rr[:]), reads=[Brr], writes=[Brr])
            for kc in range(8):
                rr, Brr = (rA, BrA) if kc < 4 else (rM, BrM)
                en = "dve" if kc % 2 == 0 else "pool"
                c.op(en, lambda e: e.tensor_tensor(out=mx[:, kc, :], in0=mx[:, kc, :], in1=rr[:], op=ALU.mult), reads=[Bmx[kc], Brr], writes=[Bmx[kc]])

            def proj(wname, src, Bsrc, evac):
                nonlocal ek
                for oc in range(8):
                    ps, Bp = nextps()
                    mm_group(ps[:], Bp, [(wres[wname][:, kc, oc * 128:(oc + 1) * 128], src[:, kc, :]) for kc in range(8)],
                             [[Bwres[wname][oc], Bsrc[kc]] for kc in range(8)])
                    evac(oc, ps, Bp)

            def evac_hd(oc, ps, Bp):
                nonlocal ek
                ek += 1
                if ek % 2 == 0:
                    c.op("dve", lambda e: e.tensor_copy(out=hd[:, oc, :], in_=ps[:]), reads=[Bp], writes=[Bhd[oc]])
                else:
                    c.op("act", lambda e: e.activation(out=hd[:, oc, :], in_=ps[:], func=AF.Copy), reads=[Bp], writes=[Bhd[oc]])

            proj("w_out", mx, Bmx, evac_hd)
            postnorm_residual(c, sb_, hd, Bhd, xt[b], Bxt[b], prm["post_mix"], cst, "mix")
            for kc in range(8):
                c.op("act", lambda e: e.activation(out=sq[:, kc, :], in_=xt[b][:, kc, :], func=AF.Square), reads=[Bxt[b][kc]], writes=[Bsq[kc]])
            mm_group(psn[:], Bpsn, [(cst["avgD"][:], sq[:, kc, :]) for kc in range(8)], [[Bsq[kc]] for kc in range(8)])
            c.op("act", lambda e: e.activation(out=rs[:], in_=psn[:], func=AF.Sqrt, bias=cst["epsc"][:, 0:1]), reads=[Bpsn], writes=[Brs])
            c.op("dve", lambda e: e.reciprocal(out=rs[:], in_=rs[:]), reads=[Brs], writes=[Brs])
            for kc in range(8):
                en = "dve" if kc % 2 == 0 else "pool"
                c.op(en, lambda e: e.tensor_tensor(out=mx[:, kc, :], in0=xt[b][:, kc, :], in1=rs[:], op=ALU.mult), reads=[Bxt[b][kc], Brs], writes=[Bmx[kc]])

            def evac_q(oc, ps, Bp):
                nonlocal ek
                ek += 1
                if ek % 2 == 0:
                    c.op("dve", lambda e: e.tensor_scalar(out=qT[:, oc, :], in0=ps[:], scalar1=1.0 / 16, scalar2=None, op0=ALU.mult), reads=[Bp], writes=[BqT[oc]])
                else:
                    c.op("act", lambda e: e.activation(out=qT[:, oc, :], in_=ps[:], func=AF.Copy, scale=1.0 / 16), reads=[Bp], writes=[BqT[oc]])

            proj("wq", mx, Bmx, evac_q)
            for xh in range(4):
                for mb in range(2):
                    ps, Bp = nextps()
                    mm_group(ps[:], Bp, [(KT[:, 2 * xh + dc, mb * 128:(mb + 1) * 128], qT[:, 2 * xh + dc, :]) for dc in range(2)],
                             [[BKT[2 * xh + dc], BqT[2 * xh + dc]] for dc in range(2)])
                    c.op("act", lambda e: e.activation(out=P[mb][:], in_=ps[:], func=AF.Exp), reads=[Bp], writes=[BP[mb]])
                ps, Bp = nextps()
                mm_group(ps[:], Bp, [(cst["ones"][:], P[mb][:]) for mb in range(2)], [[BP[mb]] for mb in range(2)])
                c.op("dve", lambda e: e.reciprocal(out=rden[:], in_=ps[:]), reads=[Bp], writes=[Brden])
                for dc in range(2):
                    oc = 2 * xh + dc
                    ps, Bp = nextps()
                    mm_group(ps[:], Bp, [(V[:, mb, oc * 128:(oc + 1) * 128], P[mb][:]) for mb in range(2)], [[BV[oc], BP[mb]] for mb in range(2)])
                    c.op("dve", lambda e: e.tensor_tensor(out=on[:, oc, :], in0=ps[:], in1=rden[:], op=ALU.mult), reads=[Bp, Brden], writes=[Bon[oc]])
            proj("wo", on, Bon, evac_hd)
            postnorm_residual(c, sb_, hd, Bhd, xt[b], Bxt[b], prm["post_mem"], cst, "mem")
            c.dma("sp", out=xv[:, :, sl], in_=xt[b][:], reads=Bxt[b])
    c.barrier()


NPAR = 272


def build_program(nl=NL, debug=False):
    nc = bass.Bass("TRN2", target_bir_lowering=False)
    di = lambda n, shp: nc.dram_tensor(n, list(shp), F32, kind="ExternalInput").ap()
    xin = di("xin", (D, S)); memT = di("memT", (D, NMEM))
    w_in = di("w_in", (NL, D, IN_COLS)); w_out = di("w_out", (NL, D, D))
    wq = di("wq_mem", (NL, D, D)); wk = di("wk_mem", (NL, D, D)); wv = di("wv_mem", (NL, D, D)); wo = di("wo_mem", (NL, D, D))
    w_up = di("w_up", (NL, D, 2 * DFF)); w_down = di("w_down", (NL, DFF, D))
    par = di("par", (128, NL, NPAR)); gb = di("gb", (4, NL, 2))
    cin = di("cin", (128, 3, 128)); tabd = di("tab", (128, 24, 256)); seld = di("sel65", (65, 64))
    ca = mlstm_const_arrays()
    cd = {k: di(k, v.shape) for k, v in ca.items()}
    yT = nc.dram_tensor("yT", [D, S], F32, kind="ExternalOutput").ap()
    sk = "ExternalOutput" if debug else "Internal"
    projT = nc.dram_tensor("projT", [3584, S], BF16, kind=sk).ap()
    gatesT = nc.dram_tensor("gatesT", [8, S], F32, kind=sk).ap()
    mixedT = nc.dram_tensor("mixedT", [D, S], BF16, kind=sk).ap()
    hffT = nc.dram_tensor("hffT", [DFF, S], BF16, kind=sk).ap()
    with ExitStack() as es:
        c = Ctx(nc, es)
        cst = mlstm_consts(c, es, cd)
        B = Buf()
        cc = c.sb(es, (128, 3, 128), BF16, "cc")
        for i in range(3):
            c.dma("pool", out=cc[:, i, :], in_=cin[:, i, :], writes=[B])
        cst["avgD"] = cc[:, 0, :]; cst["avgH"] = cc[:, 1, :]; cst["ones"] = cc[:, 2, :]
        cst["epsc"] = c.sb(es, (128, 1), F32, "epsc"); c.op("dve", lambda e: e.memset(cst["epsc"][:], EPS), writes=[B])
        cst["sel65"] = c.sb(es, (65, 64), F32, "sel65"); c.dma("sp", out=cst["sel65"][:], in_=seld, writes=[B])
        cst["tab"] = c.sb(es, (128, 24, 256), BF16, "tab")
        with ExitStack() as s0:
            tf = c.sb(s0, (128, 24, 256), F32, "tabf"); Bt = Buf()
            c.dma("sp", out=tf[:], in_=tabd, writes=[Bt])
            c.op("pool", lambda e: e.tensor_copy(out=cst["tab"][:], in_=tf[:]), reads=[Bt], writes=[B])
            c.barrier()
        pt = c.sb(es, (128, NL, NPAR), F32, "par"); c.dma("sp", out=pt[:], in_=par, writes=[B])
        gbt = c.sb(es, (4, NL, 2), F32, "gb"); c.dma("sp", out=gbt[:], in_=gb, writes=[B])
        c.dma("sp", out=yT, in_=xin, writes=[B])
        c.barrier()
        for l in range(nl):
            P = lambda a, b_: pt[:, l, a:b_]
            Bproj = bufs(29); Bmixed = bufs(12); Bhff = bufs(NFF)
            with ExitStack() as s1:
                hT = c.sb(s1, (128, 8, S), BF16, "hT")
                BhT = [bufs(8) for _ in range(NTT)]
                norm_pass(c, yT, hT, BhT, cst)
                inproj(c, w_in[l], P(0, 8), hT, BhT, projT, gatesT, Bproj)
            attention(c, projT, mixedT, cst, Bproj, Bmixed)
            mlstm(c, projT, gatesT, mixedT, Bproj, Bmixed, P(56, 96).rearrange("p (c f) -> p c f", f=5), gbt[:, l, :], cst)
            W = {"w_out": w_out[l], "wq": wq[l], "wk": wk[l], "wv": wv[l], "wo": wo[l]}
            prm = {"gmix": P(8, 16), "post_mix": P(16, 24), "pre_mem": P(24, 32), "post_mem": P(32, 40)}
            token_phase(c, l, W, prm, mixedT, Bmixed, yT, memT, cst)
            with ExitStack() as s1:
                hT = c.sb(s1, (128, 8, S), BF16, "hT")
                BhT = [bufs(8) for _ in range(NTT)]
                norm_pass(c, yT, hT, BhT, cst)
                ffn_up(c, w_up[l], P(40, 48), P(96, 272).rearrange("p (c f) -> p c f", f=4), hT, BhT, hffT, Bhff)
            ffn_down(c, w_down[l], P(48, 56), hffT, Bhff, yT, cst)
        c.barrier()
    return nc


def host_inputs(inp):
    col = lambda v: np.asarray(v, np.float32).reshape(-1, 128).T
    par = np.zeros((128, NL, NPAR), np.float32)
    gb = np.zeros((4, NL, 2), np.float32)
    for l in range(NL):
        par[:, l, 0:8] = col(inp["pre_mix_g"][l])
        par[:, l, 8:16] = col(np.concatenate([inp["attn_out_g"][l], inp["mlstm_out_g"][l]]))
        par[:, l, 16:24] = col(inp["post_mix_g"][l]); par[:, l, 24:32] = col(inp["pre_mem_g"][l]); par[:, l, 32:40] = col(inp["post_mem_g"][l])
        par[:, l, 40:48] = col(inp["pre_ffn_g"][l]); par[:, l, 48:56] = col(inp["post_ffn_g"][l])
        mc = np.concatenate([inp["mconv_w"][l], inp["mconv_b"][l][None]], 0)
        par[:, l, 56:96] = mc.reshape(5, 8, 128).transpose(2, 1, 0).reshape(128, 40)
        fc = np.concatenate([inp["fconv_w"][l], inp["fconv_b"][l][None]], 0)
        par[:, l, 96:272] = fc.reshape(4, 44, 128).transpose(2, 1, 0).reshape(128, 176)
        gb[:, l, 0] = inp["b_igate"][l]; gb[:, l, 1] = inp["b_fgate"][l]
    sel = np.zeros((65, 64), np.float32); sel[64] = 1.0
    cin = np.stack([np.full((128, 128), 1 / 1024), np.full((128, 128), 1 / 512), np.ones((128, 128))], 1).astype(np.float32)
    shared = {"par": par, "gb": gb, "cin": cin, "tab": make_tables(np.asarray(inp["rel_bias"], np.float32)), "sel65": sel}
    shared.update(mlstm_const_arrays())
    for k in ("w_in", "w_out", "wq_mem", "wk_mem", "wv_mem", "wo_mem", "w_up", "w_down"):
        shared[k] = np.ascontiguousarray(inp[k], dtype=np.float32)
    maps = []
    for b in range(4):
        m = dict(shared)
        m["xin"] = np.ascontiguousarray(np.asarray(inp["x"][b], np.float32).T)
        m["memT"] = np.ascontiguousarray(np.asarray(inp["mem"][b], np.float32).T)
        maps.append(m)
    return maps


def kernel(**inputs):
    nc = build_program(NL)
    maps = host_inputs(inputs)
    res = run_bass_kernel_spmd(nc, maps, core_ids=[0, 1, 2, 3])
    out = np.stack([np.ascontiguousarray(np.asarray(r["yT"]).T) for r in res.results], 0)
    return out.astype(np.float32)
```

```python
import numpy as np
from contextlib import ExitStack
import concourse.bass as bass
import concourse.mybir as mybir
from concourse.bass_utils import run_bass_kernel_spmd
from concourse.alu_op_type import AluOpType as ALU

AF = mybir.ActivationFunctionType
AX = mybir.AxisListType
F32 = mybir.dt.float32
BF16 = mybir.dt.bfloat16

S = 4096
D = 1024
NL = 4
TT = 512
NTT = S // TT
EPS = 1e-6
IN_COLS = 3592
DFF = 2816
NFF = DFF // 128
NMEM = 256


class Buf:
    __slots__ = ("w", "r")

    def __init__(self):
        self.w = None
        self.r = {}


def bufs(n):
    return [Buf() for _ in range(n)]


class Ctx:
    def __init__(self, nc, es, n_dma_sems=40):
        self.nc = nc
        self.es = es
        self.eng = dict(pe=nc.tensor, dve=nc.vector, act=nc.scalar, pool=nc.gpsimd, sp=nc.sync)
        self.esem = {}
        self.cnt = {}
        self.nsem = 0
        for e in self.eng:
            self._new_esem(e)
        self.waited = {}
        self.dsems = [es.enter_context(nc.semaphore(f"dq{i}")) for i in range(n_dma_sems)]
        self.dval = [0] * n_dma_sems
        self.dnext = 0
        self.n_hw = n_dma_sems - 6
        self.dnext_sw = 0
        self.recent_dma = {}
        self.uid = 0
        self.ninstr = 0

    def _new_esem(self, e):
        self.nsem += 1
        self.esem[e] = self.es.enter_context(self.nc.semaphore(f"e{e}{self.nsem}"))
        self.cnt[e] = 0

    def name(self, p):
        self.uid += 1
        return f"{p}_{self.uid}"

    def sb(self, st, shape, dtype, name="t"):
        return st.enter_context(self.nc.sbuf_tensor(self.name(name), list(shape), dtype))

    def ps(self, st, shape=(128, 512), dtype=F32, name="ps"):
        return st.enter_context(self.nc.psum_tensor(self.name(name), list(shape), dtype))

    def _wait(self, e, tok, raw=False):
        sem, val, owner = tok
        if owner == e and (e == "pe" or e == "sp"):
            return
        key = (e, id(sem))
        if self.waited.get(key, 0) >= val:
            return
        self.waited[key] = val
        self.eng[e].wait_ge(sem, val)
        self.ninstr += 1

    def _deps(self, e, reads, writes):
        for b in reads:
            if b.w is not None:
                self._wait(e, b.w, raw=True)
        for b in writes:
            if b.w is not None:
                self._wait(e, b.w)
            for t in b.r.values():
                self._wait(e, t)

    def _mark(self, tok, reads, writes):
        for b in reads:
            k = id(tok[0])
            o = b.r.get(k)
            if o is None or o[1] < tok[1]:
                b.r[k] = tok
        for b in writes:
            b.w = tok
            b.r = {}

    def op(self, e, fn, reads=(), writes=(), inc=True):
        self._deps(e, reads, writes)
        ins = fn(self.eng[e])
        self.ninstr += 1
        if inc:
            if self.cnt[e] >= 30000:
                self._new_esem(e)
            self.cnt[e] += 1
            ins.then_inc(self.esem[e], 1)
            tok = (self.esem[e], self.cnt[e], e)
        else:
            if self.cnt[e] >= 30000:
                self._new_esem(e)
            tok = (self.esem[e], self.cnt[e] + 1, e)
        self._mark(tok, reads, writes)
        return tok

    def dma(self, q, out, in_, reads=(), writes=()):
        if q == "pool":
            i = self.n_hw + self.dnext_sw
            self.dnext_sw = (self.dnext_sw + 1) % (len(self.dsems) - self.n_hw)
        else:
            i = self.dnext
            self.dnext = (i + 1) % self.n_hw
        sem = self.dsems[i]
        old = self.dval[i]
        self._deps(q, reads, writes)
        if old > 0:
            self._wait(q, (sem, old, None))
        ins = self.eng[q].dma_start(out=out, in_=in_)
        self.ninstr += 1
        self.dval[i] = old + 16
        ins.then_inc(sem, 16)
        tok = (sem, old + 16, "dma")
        self.recent_dma[id(sem)] = tok
        self._mark(tok, reads, writes)
        return tok

    def barrier(self, engines=("pe", "dve", "act", "pool", "sp")):
        toks = [(self.esem[e], self.cnt[e], e) for e in self.eng if self.cnt[e] > 0]
        toks += list(self.recent_dma.values())
        for e in engines:
            for t in toks:
                self._wait(e, t, raw=True)
        self.recent_dma = {}


import math


def norm_pass(c, xT, hT, BhT, cst):
    nc = c.nc
    xv = xT.rearrange("(kc p) t -> p kc t", p=128)
    with ExitStack() as st:
        xt = [c.sb(st, (128, 8, TT), F32, "xt") for _ in range(2)]
        sq = [c.sb(st, (128, 8, TT), BF16, "sq") for _ in range(2)]
        rs = [c.sb(st, (128, TT), F32, "rs") for _ in range(2)]
        pss = [c.ps(st) for _ in range(2)]
        Bxt = bufs(2); Bsq = [bufs(8) for _ in range(2)]; Brs = bufs(2); Bps = bufs(2)
        for tt in range(NTT):
            b = tt % 2
            sl = slice(tt * TT, (tt + 1) * TT)
            c.dma("sp", out=xt[b][:], in_=xv[:, :, sl], writes=[Bxt[b]])
            for kc in range(8):
                c.op("act", lambda e: e.activation(out=sq[b][:, kc, :], in_=xt[b][:, kc, :], func=AF.Square),
                     reads=[Bxt[b]], writes=[Bsq[b][kc]])
            for kc in range(8):
                c.op("pe", lambda e: e.matmul(pss[b][:], lhsT=cst["avgD"][:], rhs=sq[b][:, kc, :], start=(kc == 0), stop=(kc == 7)),
                     reads=[Bsq[b][kc]], writes=[Bps[b]] if kc == 0 else [], inc=(kc == 7))
            Bps[b].w = (c.esem["pe"], c.cnt["pe"], "pe")
            c.op("act", lambda e: e.activation(out=rs[b][:], in_=pss[b][:], func=AF.Sqrt, bias=cst["epsc"][:, 0:1]),
                 reads=[Bps[b]], writes=[Brs[b]])
            c.op("dve", lambda e: e.reciprocal(out=rs[b][:], in_=rs[b][:]),
                 reads=[Brs[b]], writes=[Brs[b]])
            for kc in range(8):
                en = "dve" if kc % 2 == 0 else "pool"
                c.op(en, lambda e: e.tensor_tensor(out=hT[:, kc, sl], in0=xt[b][:, kc, :], in1=rs[b][:], op=ALU.mult),
                     reads=[Bxt[b], Brs[b]], writes=[BhT[tt][kc]])
    c.barrier()


def inproj(c, w_in, gcol, hT, BhT, projT, gatesT, Bproj):
    wv = w_in.rearrange("(kc p) n -> p kc n", p=128)
    with ExitStack() as st:
        ws = [c.sb(st, (128, 8, 128), F32, "ws") for _ in range(2)]
        wb = [c.sb(st, (128, 8, 128), BF16, "wb") for _ in range(2)]
        ob = [c.sb(st, (128, S), BF16, "ob") for _ in range(2)]
        og = c.sb(st, (8, S), F32, "og")
        pss = [c.ps(st) for _ in range(4)]
        Bws = bufs(2); Bwb = [bufs(8) for _ in range(2)]; Bob = [bufs(NTT) for _ in range(2)]; Bps = bufs(4)
        Bog = bufs(NTT)
        k = 0
        for fc in range(29):
            ncol = 128 if fc < 28 else 8
            b = fc % 2
            c.dma("sp", out=ws[b][:, :, :ncol], in_=wv[:, :, fc * 128: fc * 128 + ncol], writes=[Bws[b]])
            c.op("pool", lambda e: e.tensor_tensor(out=wb[b][:, :, :ncol], in0=ws[b][:, :, :ncol], in1=gcol.unsqueeze(2).to_broadcast([128, 8, ncol]), op=ALU.mult),
                 reads=[Bws[b]], writes=Bwb[b])
            for tt in range(NTT):
                sl = slice(tt * TT, (tt + 1) * TT)
                p = k % 4
                for kc in range(8):
                    c.op("pe", lambda e: e.matmul(pss[p][:ncol, :], lhsT=wb[b][:, kc, :ncol], rhs=hT[:, kc, sl], start=(kc == 0), stop=(kc == 7)),
                         reads=[Bwb[b][kc], BhT[tt][kc]], writes=[Bps[p]] if kc == 0 else [], inc=(kc == 7))
                Bps[p].w = (c.esem["pe"], c.cnt["pe"], "pe")
                if fc == 28:
                    c.op("dve", lambda e: e.tensor_copy(out=og[:, sl], in_=pss[p][:8, :]), reads=[Bps[p]], writes=[Bog[tt]])
                elif fc < 4:
                    if k % 2 == 0:
                        c.op("act", lambda e: e.activation(out=ob[b][:, sl], in_=pss[p][:], func=AF.Copy, scale=0.125), reads=[Bps[p]], writes=[Bob[b][tt]])
                    else:
                        c.op("dve", lambda e: e.tensor_scalar(out=ob[b][:, sl], in0=pss[p][:], scalar1=0.125, scalar2=None, op0=ALU.mult), reads=[Bps[p]], writes=[Bob[b][tt]])
                else:
                    if k % 2 == 0:
                        c.op("act", lambda e: e.activation(out=ob[b][:, sl], in_=pss[p][:], func=AF.Copy), reads=[Bps[p]], writes=[Bob[b][tt]])
                    else:
                        c.op("dve", lambda e: e.tensor_copy(out=ob[b][:, sl], in_=pss[p][:]), reads=[Bps[p]], writes=[Bob[b][tt]])
                k += 1
            if fc == 28:
                c.dma("sp", out=gatesT[:, :], in_=og[:], reads=Bog, writes=[Bproj[28]])
            else:
                c.dma("sp", out=projT[fc * 128:(fc + 1) * 128, :], in_=ob[b][:], reads=Bob[b], writes=[Bproj[fc]])
    c.barrier()


def load_consts(c, st, cdram):
    cst = {}
    B = Buf()
    def ld(name, shape, dtype):
        t = c.sb(st, shape, dtype, name)
        c.dma("pool" if dtype == BF16 else "sp", out=t[:], in_=cdram[name], writes=[B])
        cst[name] = t
    ld("avgD", (128, 128), BF16)
    t = c.sb(st, (128, 1), F32, "epsc")
    c.op("dve", lambda e: e.memset(t[:], EPS), writes=[B])
    cst["epsc"] = t
    return cst, B


DILS = (1, 4, 16)

def attention(c, projT, mixedT, cst, Bproj, Bmixed):
    with ExitStack() as st:
        qkv = [c.sb(st, (128, S), BF16, "qkv") for _ in range(3)]
        perm = [[c.sb(st, (128, S), BF16, "perm") for _ in range(3)] for _ in range(2)]
        vtok = [c.sb(st, (128, 32, 2, 65), BF16, "vtok") for _ in range(3)]
        acc = [c.sb(st, (65, S), F32, "acc") for _ in range(2)]
        pt = [c.sb(st, (128, 2, 256), BF16, "pt") for _ in range(3)]
        rec = [c.sb(st, (64, TT), F32, "rec") for _ in range(2)]
        ao = [c.sb(st, (64, S), BF16, "ao") for _ in range(2)]
        sp = [c.ps(st, (128, 2, 256), F32, "sp") for _ in range(2)]
        tps = [c.ps(st, (128, 8, 128), BF16, "tp") for _ in range(2)]
        ops = [c.ps(st, (128, 4, 128), F32, "ops") for _ in range(3)]
        dps = ops[2:3]
        Bqkv = bufs(3); Bperm = [bufs(3) for _ in range(2)]; Bvt = [bufs(8) for _ in range(3)]
        Bacc = bufs(2); Bpt = bufs(3); Brec = bufs(2); Bao = [bufs(NTT) for _ in range(2)]
        Bsp = bufs(2); Btp = bufs(2); Bo = bufs(3); Bdps = Bo[2:3]
        ident = cst["ident"]; tab = cst["tab"]; sel = cst["sel65"]
        for pi in range(3):
            c.op("pool", lambda e: e.memset(vtok[pi][:, :, :, 64:65], 1.0), writes=Bvt[pi])
        kq = 0
        for hp in range(4):
            for i in range(3):
                c.dma("sp", out=qkv[i][:], in_=projT[i * 512 + hp * 128: i * 512 + hp * 128 + 128, :], reads=[Bproj[i * 4 + hp]], writes=[Bqkv[i]])
            for pi, dil in enumerate(DILS):
                nbc = 32 // dil
                if dil == 1:
                    src = qkv; Bsrc = Bqkv
                else:
                    src = perm[pi - 1]; Bsrc = Bperm[pi - 1]
                    for i in range(3):
                        c.op("pool", lambda e: e.tensor_copy(out=src[i][:].rearrange("p (c i) -> p c i", c=dil),
                                                             in_=qkv[i][:].rearrange("p (i c) -> p c i", c=dil)),
                             reads=[Bqkv[i]], writes=[Bsrc[i]])
                qP, kP, vP = src
                for g in range(8):
                    tb = g % 2
                    for j in range(4):
                        blk = g * 4 + j
                        c.op("pe", lambda e: e.transpose(out=tps[tb][:, j, :], in_=vP[:, blk * 128:(blk + 1) * 128], identity=ident[:]),
                             reads=[Bsrc[2]], writes=[Btp[tb]] if j == 0 else [], inc=(j == 3))
                    Btp[tb].w = (c.esem["pe"], c.cnt["pe"], "pe")
                    en = "dve" if g % 2 == 0 else "act"
                    o_ap = vtok[pi][:, g * 4:(g + 1) * 4, :, 0:64]
                    i_ap = tps[tb][:, 0:4, :].rearrange("p j (h d) -> p j h d", h=2)
                    if en == "dve":
                        c.op("dve", lambda e: e.tensor_copy(out=o_ap, in_=i_ap), reads=[Btp[tb]], writes=[Bvt[pi][g]])
                    else:
                        c.op("act", lambda e: e.activation(out=o_ap, in_=i_ap, func=AF.Copy), reads=[Btp[tb]], writes=[Bvt[pi][g]])
                for h in range(2):
                    hd = hp * 2 + h
                    rows = slice(h * 64, h * 64 + 64)
                    accv = acc[h][:, :].rearrange("p (i c) -> p c i", c=dil)
                    for cl in range(dil):
                        for n2 in range(0, nbc, 2):
                            s_ = kq % 2; p_ = kq % 3; kq += 1
                            for j in range(2):
                                n = n2 + j; gb = cl * nbc + n
                                nq = 256 if n < nbc - 1 else 128
                                c.op("pe", lambda e: e.matmul(sp[s_][:, j, :nq], lhsT=kP[rows, gb * 128:(gb + 1) * 128], rhs=qP[rows, gb * 128: gb * 128 + nq],
                                                              start=True, stop=False),
                                     reads=[Bsrc[0], Bsrc[1]], writes=[Bsp[s_]] if j == 0 else [], inc=False)
                                c.op("pe", lambda e: e.matmul(sp[s_][:, j, :nq], lhsT=ident[:], rhs=tab[:, hd * 3 + pi, :nq], start=False, stop=True),
                                     inc=(j == 1))
                            Bsp[s_].w = (c.esem["pe"], c.cnt["pe"], "pe")
                            last = (n2 + 1 == nbc - 1)
                            if not last:
                                c.op("act", lambda e: e.activation(out=pt[p_][:], in_=sp[s_][:], func=AF.Exp), reads=[Bsp[s_]], writes=[Bpt[p_]])
                            else:
                                c.op("act", lambda e: e.activation(out=pt[p_][:, 0, :], in_=sp[s_][:, 0, :], func=AF.Exp), reads=[Bsp[s_]], writes=[Bpt[p_]])
                                c.op("act", lambda e: e.activation(out=pt[p_][:, 1, :128], in_=sp[s_][:, 1, :128], func=AF.Exp), reads=[Bsp[s_]], writes=[])
                                Bpt[p_].w = (c.esem["act"], c.cnt["act"], "act")
                            for j in range(2):
                                n = n2 + j; gb = cl * nbc + n
                                oi = gb % 3
                                ot = ops[oi][:65, 0, :]
                                c.op("pe", lambda e: e.matmul(ot, lhsT=vtok[pi][:, gb, h, :], rhs=pt[p_][:, j, 0:128], start=(n == 0), stop=True),
                                     reads=[Bpt[p_], Bvt[pi][gb // 4]], writes=[Bo[oi]] if n == 0 else [], inc=True)
                                Bo[oi].w = (c.esem["pe"], c.cnt["pe"], "pe")
                                av = accv[:, cl, n * 128:(n + 1) * 128]
                                if pi == 0:
                                    c.op("dve", lambda e: e.tensor_copy(out=av, in_=ot), reads=[Bo[oi]], writes=[Bacc[h]])
                                else:
                                    c.op("dve", lambda e: e.tensor_tensor(out=av, in0=av, in1=ot, op=ALU.add), reads=[Bo[oi], Bacc[h]], writes=[Bacc[h]])
                                if n < nbc - 1:
                                    oi2 = (gb + 1) % 3
                                    ot2 = ops[oi2][:65, 0, :]
                                    c.op("pe", lambda e: e.matmul(ot2, lhsT=vtok[pi][:, gb, h, :], rhs=pt[p_][:, j, 128:256], start=True, stop=False),
                                         reads=[Bpt[p_], Bvt[pi][gb // 4]], writes=[Bo[oi2]], inc=False)
            for h in range(2):
                hd = hp * 2 + h
                for tt in range(NTT):
                    sl = slice(tt * TT, (tt + 1) * TT)
                    r_ = tt % 2
                    c.op("pe", lambda e: e.matmul(dps[0][:64, :, :], lhsT=sel[:65, :], rhs=acc[h][:65, sl], start=True, stop=True),
                         reads=[Bacc[h]], writes=[Bdps[0]])
                    c.op("dve", lambda e: e.reciprocal(out=rec[r_][:], in_=dps[0][:64, :, :].rearrange("p a b -> p (a b)")), reads=[Bdps[0]], writes=[Brec[r_]])
                    c.op("pool", lambda e: e.tensor_tensor(out=ao[h][:, sl], in0=acc[h][:64, sl], in1=rec[r_][:], op=ALU.mult),
                         reads=[Bacc[h], Brec[r_]], writes=[Bao[h][tt]])
                c.dma("sp", out=mixedT[hd * 64:(hd + 1) * 64, :], in_=ao[h][:], reads=Bao[h], writes=[Bmixed[hd]])
    c.barrier()


def t5_bucket(dist):
    dist = np.asarray(dist)
    max_exact = 16
    d_f = np.maximum(dist, 1).astype(np.float32)
    large = max_exact + (np.log(d_f / max_exact) / np.log(2048 / max_exact) * (32 - max_exact)).astype(np.int32)
    large = np.minimum(large, 31)
    return np.where(dist < max_exact, dist, large)


def make_tables(rel_bias):
    s = np.arange(128)[:, None]
    t = np.arange(128)[None, :]
    tabs = np.full((128, 24, 256), -30000.0, np.float32)
    for pi, dil in enumerate(DILS):
        bsub = rel_bias[t5_bucket(np.arange(129) * dil)]
        d0 = t - s
        d1 = t + 128 - s
        for h in range(8):
            tabs[:, h * 3 + pi, 0:128] = np.where(d0 >= 0, bsub[np.clip(d0, 0, 128), h], -30000.0)
            tabs[:, h * 3 + pi, 128:256] = np.where(d1 <= 128, bsub[np.clip(d1, 0, 128), h], -30000.0)
    return tabs


def mlstm(c, projT, gatesT, mixedT, Bproj, Bmixed, mcp, gbias, cst):
    ident = cst["ident"]; identf = cst["identf"]; cm = cst["cmask"]
    with ExitStack() as st:
        et = c.sb(st, (128, 32, 8), F32, "et"); Bet = Buf()
        decb = c.sb(st, (128, 128), F32, "decb"); Bdecb = Buf()
        with ExitStack() as s0:
            gi = c.sb(s0, (4, S), F32, "gi"); gf = c.sb(s0, (4, S), F32, "gf"); nb = c.sb(s0, (4, S), F32, "nb")
            dmb = c.sb(s0, (4, S), F32, "dmb"); t1 = c.sb(s0, (4, S), F32, "t1"); t2 = c.sb(s0, (4, S), F32, "t2")
            sm = c.sb(s0, (4, 8, 32), F32, "sm")
            decbd = c.sb(s0, (4, 4, 32), F32, "decbd"); nbf = c.sb(s0, (4, 2), F32, "nbf")
            pg = c.ps(s0, (128, 32, 8), F32, "pg"); pd = c.ps(s0, (128, 128), F32, "pd")
            G = Buf()
            c.dma("sp", out=gi[:], in_=gatesT[0:4, :], reads=[Bproj[28]], writes=[G])
            c.dma("sp", out=gf[:], in_=gatesT[4:8, :], reads=[Bproj[28]], writes=[G])
            def g(en, fn):
                c.op(en, fn, reads=[G], writes=[G])
            g("dve", lambda e: e.tensor_scalar(out=nbf[:, 0:1], in0=gbias[:, 1:2], scalar1=-1.0, scalar2=None, op0=ALU.mult))
            g("dve", lambda e: e.memset(nbf[:, 1:2], -0.5 * math.log(128.0)))
            g("act", lambda e: e.activation(out=t1[:], in_=gf[:], func=AF.Exp, scale=-1.0, bias=nbf[:, 0:1]))
            g("act", lambda e: e.activation(out=t1[:], in_=t1[:], func=AF.Ln, bias=cst["one1"][:4, 0:1]))
            g("dve", lambda e: e.tensor_tensor_scan(out=nb[:], data0=cst["rmask"][:4, :], data1=t1[:], initial=0.0, op0=ALU.mult, op1=ALU.add))
            g("dve", lambda e: e.scalar_tensor_tensor(out=dmb[:], in0=gi[:], scalar=gbias[:, 0:1], in1=nb[:], op0=ALU.add, op1=ALU.add))
            g("dve", lambda e: e.tensor_reduce(out=sm[:, 0, :], in_=dmb[:].rearrange("p (c s) -> p c s", s=128), axis=AX.X, op=ALU.max))
            g("dve", lambda e: e.tensor_scalar(out=sm[:, 1, :], in0=nb[:].rearrange("p (c s) -> p c s", s=128)[:, :, 127], scalar1=-1.0, scalar2=None, op0=ALU.mult))
            g("dve", lambda e: e.tensor_tensor_scan(out=sm[:, 2, :], data0=sm[:, 0, :], data1=sm[:, 1, :], initial=0.0, op0=ALU.max, op1=ALU.add))
            g("dve", lambda e: e.memset(sm[:, 3, 0:1], 0.0))
            g("dve", lambda e: e.tensor_copy(out=sm[:, 3, 1:32], in_=sm[:, 2, 0:31]))
            g("dve", lambda e: e.tensor_tensor(out=sm[:, 4, :], in0=sm[:, 3, :], in1=sm[:, 0, :], op=ALU.max))
            g("dve", lambda e: e.tensor_tensor(out=sm[:, 5, :], in0=sm[:, 3, :], in1=sm[:, 4, :], op=ALU.subtract))
            g("act", lambda e: e.activation(out=sm[:, 5, :], in_=sm[:, 5, :], func=AF.Exp))
            Mb = sm[:, 4, :].unsqueeze(2).to_broadcast([4, 32, 128])
            g("dve", lambda e: e.tensor_tensor(out=t1[:].rearrange("p (c s) -> p c s", s=128), in0=dmb[:].rearrange("p (c s) -> p c s", s=128), in1=Mb, op=ALU.subtract))
            g("act", lambda e: e.activation(out=t1[:], in_=t1[:], func=AF.Exp, bias=nbf[:, 1:2]))
            g("dve", lambda e: e.tensor_tensor(out=t2[:].rearrange("p (c s) -> p c s", s=128), in0=nb[:].rearrange("p (c s) -> p c s", s=128), in1=Mb, op=ALU.subtract))
            g("act", lambda e: e.activation(out=t2[:], in_=t2[:], func=AF.Exp))
            for cc in range(32):
                cs = slice(cc * 128, (cc + 1) * 128)
                c.op("pe", lambda e: e.transpose(out=pg[:, cc, 0:4], in_=t1[:, cs], identity=identf[:4, :4]), reads=[G], writes=[G] if cc == 0 else [], inc=False)
                c.op("pe", lambda e: e.transpose(out=pg[:, cc, 4:8], in_=t2[:, cs], identity=identf[:4, :4]), reads=[G], inc=(cc == 31))
            G.w = (c.esem["pe"], c.cnt["pe"], "pe")
            c.op("dve", lambda e: e.tensor_copy(out=et[:], in_=pg[:]), reads=[G], writes=[Bet])
            g("dve", lambda e: e.tensor_tensor(out=decbd[:], in0=sm[:, 5, :].unsqueeze(1).to_broadcast([4, 4, 32]), in1=cst["bdmask"][:4, :].rearrange("p (a b) -> p a b", a=4), op=ALU.mult))
            c.op("pe", lambda e: e.matmul(pd[:], lhsT=cst["ones4"][:4, :], rhs=decbd[:].rearrange("p a b -> p (a b)"), start=True, stop=True), reads=[G], writes=[G])
            c.op("dve", lambda e: e.tensor_copy(out=decb[:], in_=pd[:]), reads=[G], writes=[Bdecb])
            c.barrier()
        raw = [c.sb(st, (128, 3 + S), BF16, "raw") for _ in range(2)]; Braw = bufs(2)
        cv = [c.sb(st, (128, S), F32, "cv") for _ in range(2)]; Bcv = bufs(2)
        qT = c.sb(st, (128, S), BF16, "qT"); kT = c.sb(st, (128, S), BF16, "kT"); BqT = Buf(); BkT = Buf()
        vT = c.sb(st, (128, S), BF16, "vT"); oT = c.sb(st, (128, S), BF16, "oT"); BvT = Buf(); BoT = Buf()
        hmT = c.sb(st, (128, S), BF16, "hmT"); BhmT = bufs(32)
        ktk = [c.sb(st, (128, 128), BF16, "ktk") for _ in range(2)]; Bktk = bufs(2)
        vaug = [c.sb(st, (128, 129), BF16, "vaug") for _ in range(2)]; Bvaug = bufs(2)
        og = [c.sb(st, (128, 128), F32, "og") for _ in range(2)]; Bog = bufs(2)
        pT = [c.sb(st, (128, 128), BF16, "pT") for _ in range(2)]; BpT = bufs(2)
        hmt = [c.sb(st, (128, 128), BF16, "hmt") for _ in range(2)]; Bhmt = bufs(2)
        C = c.sb(st, (128, 129), F32, "C"); BC = Buf()
        Cbf = [c.sb(st, (128, 129), BF16, "Cbf") for _ in range(2)]; BCbf = bufs(2)
        dd = [c.sb(st, (128, 2), F32, "dd") for _ in range(2)]; Bdd = bufs(2)
        tpk = [c.ps(st, (128, 8, 128), BF16, "tpk") for _ in range(2)]; Btpk = bufs(2)
        sps = c.ps(st, (128, 512), F32, "sps"); Bsps = Buf()
        nps = [c.ps(st, (128, 512), F32, "nps") for _ in range(2)]; Bnps = bufs(2)
        dps = c.ps(st, (128, 512), F32, "dps"); Bdps = Buf()
        tph = c.ps(st, (128, 8, 128), BF16, "tph"); Btph = Buf()
        for r in range(2):
            c.op("pool", lambda e: e.memset(raw[r][:, 0:3], 0.0), writes=[Braw[r]])
            c.op("pool", lambda e: e.memset(vaug[r][:, 128:129], 1.0), writes=[Bvaug[r]])
        for hd in range(4):
            c.dma("sp", out=raw[0][:, 3:], in_=projT[1536 + hd * 128:1536 + (hd + 1) * 128, :], reads=[Bproj[12 + hd]], writes=[Braw[0]])
            c.dma("sp", out=raw[1][:, 3:], in_=projT[2048 + hd * 128:2048 + (hd + 1) * 128, :], reads=[Bproj[16 + hd]], writes=[Braw[1]])
            c.dma("sp", out=vT[:], in_=projT[2560 + hd * 128:2560 + (hd + 1) * 128, :], reads=[Bproj[20 + hd]], writes=[BvT])
            c.dma("sp", out=oT[:], in_=projT[3072 + hd * 128:3072 + (hd + 1) * 128, :], reads=[Bproj[24 + hd]], writes=[BoT])
            for i, en, dst, Bdst in ((0, "dve", qT, BqT), (1, "dve", kT, BkT)):
                ci = i * 4 + hd
                c.op(en, lambda e: e.tensor_scalar(out=cv[i][:], in0=raw[i][:, 3:3 + S], scalar1=mcp[:, ci, 3:4], scalar2=mcp[:, ci, 4:5], op0=ALU.mult, op1=ALU.add),
                     reads=[Braw[i]], writes=[Bcv[i]])
                for j in range(3):
                    c.op(en, lambda e: e.scalar_tensor_tensor(out=cv[i][:], in0=raw[i][:, j:j + S], scalar=mcp[:, ci, j:j + 1], in1=cv[i][:], op0=ALU.mult, op1=ALU.add),
                         reads=[Braw[i], Bcv[i]], writes=[Bcv[i]])
                c.op("act", lambda e: e.activation(out=dst[:], in_=cv[i][:], func=AF.Silu), reads=[Bcv[i]], writes=[Bdst])
            for cc in range(32):
                r = cc % 2
                cs = slice(cc * 128, (cc + 1) * 128)
                ecol = et[:, cc, hd:hd + 1]; fcol = et[:, cc, 4 + hd:5 + hd]
                for j, (src, Bsrc) in enumerate(((kT, BkT), (vT, BvT), (oT, BoT))):
                    c.op("pe", lambda e: e.transpose(out=tpk[r][:, j, :], in_=src[:, cs], identity=ident[:]), reads=[Bsrc], writes=[Btpk[r]] if j == 0 else [], inc=(j == 2))
                Btpk[r].w = (c.esem["pe"], c.cnt["pe"], "pe")
                c.op("dve", lambda e: e.tensor_scalar(out=ktk[r][:], in0=tpk[r][:, 0, :], scalar1=ecol, scalar2=None, op0=ALU.mult), reads=[Btpk[r], Bet], writes=[Bktk[r]])
                c.op("act", lambda e: e.activation(out=vaug[r][:, 0:128], in_=tpk[r][:, 1, :], func=AF.Copy), reads=[Btpk[r]], writes=[Bvaug[r]])
                c.op("act", lambda e: e.activation(out=og[r][:], in_=tpk[r][:, 2, :], func=AF.Sigmoid), reads=[Btpk[r]], writes=[Bog[r]])
                c.op("pe", lambda e: e.matmul(sps[:, 0:128], lhsT=kT[:, cs], rhs=qT[:, cs], start=True, stop=True), reads=[BkT, BqT], writes=[Bsps])
                c.op("dve", lambda e: e.scalar_tensor_tensor(out=pT[r][:], in0=sps[:, 0:128], scalar=ecol, in1=cm[:], op0=ALU.mult, op1=ALU.mult), reads=[Bsps, Bet], writes=[BpT[r]])
                if cc > 0:
                    dcol = decb[:, hd * 32 + cc: hd * 32 + cc + 1]
                    c.op("dve", lambda e: e.tensor_scalar(out=C[:], in0=C[:], scalar1=dcol, scalar2=None, op0=ALU.mult), reads=[BC, Bdecb], writes=[BC])
                    c.op("act", lambda e: e.activation(out=Cbf[r][:], in_=C[:], func=AF.Copy), reads=[BC], writes=[BCbf[r]])
                c.op("pe", lambda e: e.matmul(nps[r][:, 0:129], lhsT=pT[r][:], rhs=vaug[r][:], start=True, stop=(cc == 0)), reads=[BpT[r], Bvaug[r]], writes=[Bnps[r]], inc=(cc == 0))
                if cc > 0:
                    c.op("pe", lambda e: e.matmul(nps[r][:, 0:129], lhsT=qT[:, cs], rhs=Cbf[r][:], start=False, stop=True), reads=[BqT, BCbf[r]], inc=True)
                Bnps[r].w = (c.esem["pe"], c.cnt["pe"], "pe")
                c.op("pe", lambda e: e.matmul(dps[:, 0:129], lhsT=ktk[r][:], rhs=vaug[r][:], start=True, stop=True), reads=[Bktk[r], Bvaug[r]], writes=[Bdps])
                if cc == 0:
                    c.op("dve", lambda e: e.tensor_copy(out=C[:], in_=dps[:, 0:129]), reads=[Bdps], writes=[BC])
                else:
                    c.op("dve", lambda e: e.tensor_tensor(out=C[:], in0=C[:], in1=dps[:, 0:129], op=ALU.add), reads=[Bdps, BC], writes=[BC])
                c.op("dve", lambda e: e.tensor_scalar(out=dd[r][:, 1:2], in0=nps[r][:, 128:129], scalar1=-1.0, scalar2=None, op0=ALU.mult), reads=[Bnps[r]], writes=[Bdd[r]])
                c.op("dve", lambda e: e.scalar_tensor_tensor(out=dd[r][:, 0:1], in0=nps[r][:, 128:129], scalar=fcol, in1=dd[r][:, 1:2], op0=ALU.max, op1=ALU.max), reads=[Bnps[r], Bet, Bdd[r]], writes=[Bdd[r]])
                c.op("dve", lambda e: e.reciprocal(out=dd[r][:, 1:2], in_=dd[r][:, 0:1]), reads=[Bdd[r]], writes=[Bdd[r]])
                c.op("dve", lambda e: e.scalar_tensor_tensor(out=hmt[r][:], in0=nps[r][:, 0:128], scalar=dd[r][:, 1:2], in1=og[r][:], op0=ALU.mult, op1=ALU.mult),
                     reads=[Bnps[r], Bdd[r], Bog[r]], writes=[Bhmt[r]])
                c.op("pe", lambda e: e.transpose(out=tph[:, 0, :], in_=hmt[r][:], identity=ident[:]), reads=[Bhmt[r]], writes=[Btph])
                c.op("act", lambda e: e.activation(out=hmT[:, cs], in_=tph[:, 0, :], func=AF.Copy), reads=[Btph], writes=[BhmT[cc]])
            c.dma("sp", out=mixedT[512 + hd * 128: 512 + (hd + 1) * 128, :], in_=hmT[:], reads=BhmT, writes=[Bmixed[8 + hd]])
    c.barrier()


def mlstm_consts(c, st, cd):
    cst = {}; B = Buf()
    cst["identf"] = c.sb(st, (128, 128), F32, "identf"); c.dma("sp", out=cst["identf"][:], in_=cd["identf"], writes=[B])
    cst["ident"] = c.sb(st, (128, 128), BF16, "ident"); c.dma("pool", out=cst["ident"][:], in_=cd["identf"], writes=[B])
    cst["cmask"] = c.sb(st, (128, 128), F32, "cmask"); c.dma("sp", out=cst["cmask"][:], in_=cd["cmask"], writes=[B])
    cst["rmask"] = c.sb(st, (4, S), F32, "rmask"); c.dma("sp", out=cst["rmask"][:], in_=cd["rmask"], writes=[B])
    cst["bdmask"] = c.sb(st, (4, 128), F32, "bdmask"); c.dma("sp", out=cst["bdmask"][:], in_=cd["bdmask"], writes=[B])
    cst["ones4"] = c.sb(st, (4, 128), F32, "ones4"); c.dma("sp", out=cst["ones4"][:], in_=cd["ones4"], writes=[B])
    cst["one1"] = c.sb(st, (128, 1), F32, "one1"); c.op("dve", lambda e: e.memset(cst["one1"][:], 1.0), writes=[B])
    return cst


def mlstm_const_arrays():
    t = np.arange(128)
    cmask = (t[None, :] >= t[:, None]).astype(np.float32)
    rmask = np.ones((4, S), np.float32); rmask[:, ::128] = 0.0
    bd = np.zeros((4, 4, 32), np.float32)
    for h in range(4):
        bd[h, h] = 1.0
    return {"identf": np.eye(128, dtype=np.float32), "cmask": cmask, "rmask": rmask, "bdmask": bd.reshape(4, 128), "ones4": np.ones((4, 128), np.float32)}


def ffn_up(c, w_up, gcol, fcp, hT, BhT, hffT, Bhff):
    wv = w_up.rearrange("(kc p) n -> p kc n", p=128)
    with ExitStack() as st:
        ws = [c.sb(st, (128, 8, 128), F32, "ws") for _ in range(2)]
        wb = [c.sb(st, (128, 8, 128), BF16, "wb") for _ in range(2)]
        u = [[c.sb(st, (128, 2 + S), BF16, "u") for _ in range(2)] for _ in range(2)]
        cv = [c.sb(st, (128, S), F32, "cv") for _ in range(2)]
        gg = c.sb(st, (128, S), BF16, "gg")
        ho = [c.sb(st, (128, S), BF16, "ho") for _ in range(2)]
        pss = [c.ps(st) for _ in range(4)]
        Bws = bufs(2); Bwb = [bufs(8) for _ in range(2)]; Bu = [[bufs(NTT) for _ in range(2)] for _ in range(2)]
        Bcv = bufs(2); Bgg = Buf(); Bho = bufs(2); Bps = bufs(4)
        for par in range(2):
            for half in range(2):
                c.op("pool", lambda e: e.memset(u[par][half][:, 0:2], 0.0), writes=Bu[par][half])
        k = 0

        def ld_w(jj, half):
            ci_ = jj + NFF * half
            c.dma("sp", out=ws[half][:], in_=wv[:, :, ci_ * 128:(ci_ + 1) * 128], writes=[Bws[half]])

        ld_w(0, 0); ld_w(0, 1)
        for j in range(NFF):
            par = j % 2
            for half in range(2):
                ci = j + NFF * half
                b = half
                c.op("pool", lambda e: e.tensor_tensor(out=wb[b][:], in0=ws[b][:], in1=gcol.unsqueeze(2).to_broadcast([128, 8, 128]), op=ALU.mult),
                     reads=[Bws[b]], writes=Bwb[b])
                for tt in range(NTT):
                    sl = slice(tt * TT, (tt + 1) * TT)
                    p = k % 4
                    for kc in range(8):
                        c.op("pe", lambda e: e.matmul(pss[p][:], lhsT=wb[b][:, kc, :], rhs=hT[:, kc, sl], start=(kc == 0), stop=(kc == 7)),
                             reads=[Bwb[b][kc], BhT[tt][kc]], writes=[Bps[p]] if kc == 0 else [], inc=(kc == 7))
                    Bps[p].w = (c.esem["pe"], c.cnt["pe"], "pe")
                    o_ap = u[par][half][:, 2 + tt * TT: 2 + (tt + 1) * TT]
                    c.op("act", lambda e: e.activation(out=o_ap, in_=pss[p][:], func=AF.Copy), reads=[Bps[p]], writes=[Bu[par][half][tt]])
                    k += 1
                if j + 1 < NFF:
                    ld_w(j + 1, half)
            for half, en in ((0, "dve"), (1, "dve")):
                ci = j + NFF * half
                uu = u[par][half]
                c.op(en, lambda e: e.tensor_scalar(out=cv[half][:], in0=uu[:, 2:2 + S], scalar1=fcp[:, ci, 2:3], scalar2=fcp[:, ci, 3:4], op0=ALU.mult, op1=ALU.add),
                     reads=Bu[par][half], writes=[Bcv[half]])
                c.op(en, lambda e: e.scalar_tensor_tensor(out=cv[half][:], in0=uu[:, 1:1 + S], scalar=fcp[:, ci, 1:2], in1=cv[half][:], op0=ALU.mult, op1=ALU.add),
                     reads=Bu[par][half] + [Bcv[half]], writes=[Bcv[half]])
                c.op(en, lambda e: e.scalar_tensor_tensor(out=cv[half][:], in0=uu[:, 0:S], scalar=fcp[:, ci, 0:1], in1=cv[half][:], op0=ALU.mult, op1=ALU.add),
                     reads=Bu[par][half] + [Bcv[half]], writes=[Bcv[half]])
            c.op("act", lambda e: e.activation(out=gg[:], in_=cv[1][:], func=AF.Gelu_apprx_tanh), reads=[Bcv[1]], writes=[Bgg])
            c.op("dve", lambda e: e.tensor_tensor(out=ho[par][:], in0=gg[:], in1=cv[0][:], op=ALU.mult), reads=[Bgg, Bcv[0]], writes=[Bho[par]])
            c.dma("sp", out=hffT[j * 128:(j + 1) * 128, :], in_=ho[par][:], reads=[Bho[par]], writes=[Bhff[j]])
    c.barrier()


def postnorm_residual(c, st_bufs, hsrc, Bh, xt, Bx, gcol, cst, tag):
    sq, Bsq, ps, Bps, rs, Brs, tmp, Btmp = st_bufs
    for oc in range(8):
        c.op("act", lambda e: e.activation(out=sq[:, oc, :], in_=hsrc[:, oc, :], func=AF.Square), reads=[Bh[oc]], writes=[Bsq[oc]])
    for oc in range(8):
        c.op("pe", lambda e: e.matmul(ps[:], lhsT=cst["avgD"][:], rhs=sq[:, oc, :], start=(oc == 0), stop=(oc == 7)),
             reads=[Bsq[oc]], writes=[Bps] if oc == 0 else [], inc=(oc == 7))
    Bps.w = (c.esem["pe"], c.cnt["pe"], "pe")
    c.op("act", lambda e: e.activation(out=rs[:], in_=ps[:], func=AF.Sqrt, bias=cst["epsc"][:, 0:1]), reads=[Bps], writes=[Brs])
    c.op("dve", lambda e: e.reciprocal(out=rs[:], in_=rs[:]), reads=[Brs], writes=[Brs])
    for oc in range(8):
        en = "dve" if oc % 2 == 0 else "pool"
        c.op(en, lambda e: e.tensor_tensor(out=tmp[:, oc, :], in0=hsrc[:, oc, :], in1=rs[:], op=ALU.mult), reads=[Bh[oc], Brs], writes=[Btmp[oc]])
        c.op("dve", lambda e: e.scalar_tensor_tensor(out=xt[:, oc, :], in0=tmp[:, oc, :], scalar=gcol[:, oc:oc + 1], in1=xt[:, oc, :], op0=ALU.mult, op1=ALU.add),
             reads=[Btmp[oc], Bx[oc]], writes=[Bx[oc]])


def ffn_down(c, w_down, gpost, hffT, Bhff, xT, cst):
    wv = w_down.rearrange("(c p) n -> p c n", p=128)
    hv = hffT.rearrange("(c p) t -> p c t", p=128)
    xv = xT.rearrange("(kc p) t -> p kc t", p=128)
    with ExitStack() as st:
        wd = c.sb(st, (128, NFF, D), BF16, "wd")
        wst = [c.sb(st, (128, NFF, 128), F32, "wst") for _ in range(1)]
        hf = [c.sb(st, (128, NFF, TT), BF16, "hf") for _ in range(2)]
        xt = [c.sb(st, (128, 8, TT), F32, "xt") for _ in range(2)]
        hd = c.sb(st, (128, 8, TT), F32, "hd")
        sq = c.sb(st, (128, 8, TT), BF16, "sq"); rs = c.sb(st, (128, TT), F32, "rs")
        pss = [c.ps(st) for _ in range(3)]
        psn = c.ps(st)
        Bwd = bufs(8); Bwst = bufs(1); Bhf = bufs(2); Bxt = [bufs(8) for _ in range(2)]; Bhd = bufs(8)
        Bps = bufs(3)
        sb_ = (sq, bufs(8), psn, Buf(), rs, Buf(), hd, Bhd)
        for oc in range(8):
            b = 0
            c.dma("sp", out=wst[b][:], in_=wv[:, :, oc * 128:(oc + 1) * 128], writes=[Bwst[b]])
            c.op("pool", lambda e: e.tensor_copy(out=wd[:, :, oc * 128:(oc + 1) * 128], in_=wst[b][:]), reads=[Bwst[b]], writes=[Bwd[oc]])
        k = 0

        def ld_t(t_):
            b_ = t_ % 2
            sl_ = slice(t_ * TT, (t_ + 1) * TT)
            c.dma("sp", out=hf[b_][:], in_=hv[:, :, sl_], reads=Bhff, writes=[Bhf[b_]])
            c.dma("sp", out=xt[b_][:], in_=xv[:, :, sl_], writes=Bxt[b_])

        ld_t(0)
        for tt in range(NTT):
            b = tt % 2
            sl = slice(tt * TT, (tt + 1) * TT)
            for oc in range(8):
                p = k % 3; k += 1
                for fc in range(NFF):
                    c.op("pe", lambda e: e.matmul(pss[p][:], lhsT=wd[:, fc, oc * 128:(oc + 1) * 128], rhs=hf[b][:, fc, :], start=(fc == 0), stop=(fc == NFF - 1)),
                         reads=[Bwd[oc], Bhf[b]], writes=[Bps[p]] if fc == 0 else [], inc=(fc == NFF - 1))
                Bps[p].w = (c.esem["pe"], c.cnt["pe"], "pe")
                c.op("dve", lambda e: e.tensor_copy(out=hd[:, oc, :], in_=pss[p][:]), reads=[Bps[p]], writes=[Bhd[oc]])
            postnorm_residual(c, sb_, hd, Bhd, xt[b], Bxt[b], gpost, cst, "ffn")
            if tt + 1 < NTT:
                ld_t(tt + 1)
            c.dma("sp", out=xv[:, :, sl], in_=xt[b][:], reads=Bxt[b])
    c.barrier()


def token_phase(c, l, W, prm, mixedT, Bmixed, xT, memT, cst):
    mv = mixedT.rearrange("(kc p) t -> p kc t", p=128)
    xv = xT.rearrange("(kc p) t -> p kc t", p=128)
    with ExitStack() as st:
        wres = {n: c.sb(st, (128, 8, D), BF16, n) for n in ("w_out", "wq", "wo")}
        Bwres = {n: bufs(8) for n in wres}
        KT = c.sb(st, (128, 8, NMEM), BF16, "KT"); V = c.sb(st, (128, 2, D), BF16, "V"); mem = c.sb(st, (128, 8, NMEM), BF16, "mem")
        BKT = bufs(8); BV = bufs(8); Bmem = Buf()
        ws = [c.sb(st, (128, 8, 128), F32, "ws") for _ in range(2)]; Bws = bufs(2)
        wtmp = [c.sb(st, (128, 8, 128), BF16, "wtmp") for _ in range(2)]; Bwtmp = bufs(2)
        mx = c.sb(st, (128, 8, TT), BF16, "mx"); Bmx = bufs(8)
        sq = c.sb(st, (128, 8, TT), BF16, "sq"); Bsq = bufs(8)
        hd = c.sb(st, (128, 8, TT), F32, "hd"); Bhd = bufs(8)
        xt = [c.sb(st, (128, 8, TT), F32, "xt") for _ in range(2)]; Bxt = [bufs(8) for _ in range(2)]
        qT = c.sb(st, (128, 8, TT), BF16, "qT"); BqT = bufs(8)
        on = c.sb(st, (128, 8, TT), BF16, "on"); Bon = bufs(8)
        P = [c.sb(st, (128, TT), BF16, "P") for _ in range(2)]; BP = bufs(2)
        rs = c.sb(st, (128, TT), F32, "rs"); Brs = Buf()
        rA = c.sb(st, (128, TT), F32, "rA"); BrA = Buf()
        rM = c.sb(st, (128, TT), F32, "rM"); BrM = Buf()
        rden = c.sb(st, (128, TT), F32, "rden"); Brden = Buf()
        NPS = 7
        pss = [c.ps(st) for _ in range(NPS)]; Bps = bufs(NPS)
        psn = c.ps(st); Bpsn = Buf()
        pk = [0]

        def nextps():
            i = pk[0] % NPS; pk[0] += 1
            return pss[i], Bps[i]

        def mm_group(ps, Bp, parts, reads_list):
            n = len(parts)
            for i, (lh, rh) in enumerate(parts):
                c.op("pe", lambda e: e.matmul(ps, lhsT=lh, rhs=rh, start=(i == 0), stop=(i == n - 1)),
                     reads=reads_list[i], writes=[Bp] if i == 0 else [], inc=(i == n - 1))
            Bp.w = (c.esem["pe"], c.cnt["pe"], "pe")

        with ExitStack() as s0:
            mf = c.sb(s0, (128, 8, NMEM), F32, "mf"); Bmf = Buf()
            c.dma("sp", out=mf[:], in_=memT.rearrange("(kc p) m -> p kc m", p=128), writes=[Bmf])
            c.op("pool", lambda e: e.tensor_copy(out=mem[:], in_=mf[:]), reads=[Bmf], writes=[Bmem])
            c.barrier()
        kw = 0
        for name, g in (("w_out", prm["gmix"]), ("wq", prm["pre_mem"]), ("wo", None)):
            wv_ = W[name].rearrange("(kc p) n -> p kc n", p=128)
            for oc in range(8):
                b = kw % 2; kw += 1
                c.dma("sp", out=ws[b][:], in_=wv_[:, :, oc * 128:(oc + 1) * 128], writes=[Bws[b]])
                if g is None:
                    c.op("pool", lambda e: e.tensor_copy(out=wres[name][:, :, oc * 128:(oc + 1) * 128], in_=ws[b][:]), reads=[Bws[b]], writes=[Bwres[name][oc]])
                else:
                    c.op("pool", lambda e: e.tensor_tensor(out=wres[name][:, :, oc * 128:(oc + 1) * 128], in0=ws[b][:], in1=g.unsqueeze(2).to_broadcast([128, 8, 128]), op=ALU.mult),
                         reads=[Bws[b]], writes=[Bwres[name][oc]])
        for name in ("wk", "wv"):
            wv_ = W[name].rearrange("(kc p) n -> p kc n", p=128)
            for oc in range(8):
                b = kw % 2; kw += 1
                c.dma("sp", out=ws[b][:], in_=wv_[:, :, oc * 128:(oc + 1) * 128], writes=[Bws[b]])
                c.op("pool", lambda e: e.tensor_copy(out=wtmp[b][:], in_=ws[b][:]), reads=[Bws[b]], writes=[Bwtmp[b]])
                if name == "wk":
                    ps, Bp = nextps()
                    mm_group(ps[:, :NMEM], Bp, [(wtmp[b][:, kc, :], mem[:, kc, :]) for kc in range(8)], [[Bwtmp[b], Bmem]] * 8)
                    c.op("dve", lambda e: e.tensor_copy(out=KT[:, oc, :], in_=ps[:, :NMEM]), reads=[Bp], writes=[BKT[oc]])
                else:
                    for mb in range(2):
                        ps, Bp = nextps()
                        mm_group(ps[:, :128], Bp, [(mem[:, kc, mb * 128:(mb + 1) * 128], wtmp[b][:, kc, :]) for kc in range(8)], [[Bwtmp[b], Bmem]] * 8)
                        c.op("dve", lambda e: e.tensor_copy(out=V[:, mb, oc * 128:(oc + 1) * 128], in_=ps[:, :128]), reads=[Bp], writes=[BV[oc]])
        sb_ = (sq, Bsq, psn, Bpsn, rs, Brs, hd, Bhd)
        ek = 0
        for tt in range(NTT):
            b = tt % 2
            sl = slice(tt * TT, (tt + 1) * TT)
            c.dma("sp", out=mx[:], in_=mv[:, :, sl], reads=Bmixed, writes=Bmx)
            c.dma("sp", out=xt[b][:], in_=xv[:, :, sl], writes=Bxt[b])
            for kc in range(8):
                c.op("act", lambda e: e.activation(out=sq[:, kc, :], in_=mx[:, kc, :], func=AF.Square), reads=[Bmx[kc]], writes=[Bsq[kc]])
            for grp, (rr, Brr) in enumerate(((rA, BrA), (rM, BrM))):
                ps, Bp = nextps()
                mm_group(ps[:], Bp, [(cst["avgH"][:], sq[:, grp * 4 + i, :]) for i in range(4)], [[Bsq[grp * 4 + i]] for i in range(4)])
                c.op("act", lambda e: e.activation(out=rr[:], in_=ps[:], func=AF.Sqrt, bias=cst["epsc"][:, 0:1]), reads=[Bp], writes=[Brr])
                c.op("dve", lambda e: e.reciprocal(out=rr[:], in_=rr[:]), reads=[Brr], writes=[Brr])
            for kc in range(8):
                rr, Brr = (rA, BrA) if kc < 4 else (rM, BrM)
                en = "dve" if kc % 2 == 0 else "pool"
                c.op(en, lambda e: e.tensor_tensor(out=mx[:, kc, :], in0=mx[:, kc, :], in1=rr[:], op=ALU.mult), reads=[Bmx[kc], Brr], writes=[Bmx[kc]])

            def proj(wname, src, Bsrc, evac):
                nonlocal ek
                for oc in range(8):
                    ps, Bp = nextps()
                    mm_group(ps[:], Bp, [(wres[wname][:, kc, oc * 128:(oc + 1) * 128], src[:, kc, :]) for kc in range(8)],
                             [[Bwres[wname][oc], Bsrc[kc]] for kc in range(8)])
                    evac(oc, ps, Bp)

            def evac_hd(oc, ps, Bp):
                nonlocal ek
                ek += 1
                if ek % 2 == 0:
                    c.op("dve", lambda e: e.tensor_copy(out=hd[:, oc, :], in_=ps[:]), reads=[Bp], writes=[Bhd[oc]])
                else:
                    c.op("act", lambda e: e.activation(out=hd[:, oc, :], in_=ps[:], func=AF.Copy), reads=[Bp], writes=[Bhd[oc]])

            proj("w_out", mx, Bmx, evac_hd)
            postnorm_residual(c, sb_, hd, Bhd, xt[b], Bxt[b], prm["post_mix"], cst, "mix")
            for kc in range(8):
                c.op("act", lambda e: e.activation(out=sq[:, kc, :], in_=xt[b][:, kc, :], func=AF.Square), reads=[Bxt[b][kc]], writes=[Bsq[kc]])
            mm_group(psn[:], Bpsn, [(cst["avgD"][:], sq[:, kc, :]) for kc in range(8)], [[Bsq[kc]] for kc in range(8)])
            c.op("act", lambda e: e.activation(out=rs[:], in_=psn[:], func=AF.Sqrt, bias=cst["epsc"][:, 0:1]), reads=[Bpsn], writes=[Brs])
            c.op("dve", lambda e: e.reciprocal(out=rs[:], in_=rs[:]), reads=[Brs], writes=[Brs])
            for kc in range(8):
                en = "dve" if kc % 2 == 0 else "pool"
                c.op(en, lambda e: e.tensor_tensor(out=mx[:, kc, :], in0=xt[b][:, kc, :], in1=rs[:], op=ALU.mult), reads=[Bxt[b][kc], Brs], writes=[Bmx[kc]])

            def evac_q(oc, ps, Bp):
                nonlocal ek
                ek += 1
                if ek % 2 == 0:
                    c.op("dve", lambda e: e.tensor_scalar(out=qT[:, oc, :], in0=ps[:], scalar1=1.0 / 16, scalar2=None, op0=ALU.mult), reads=[Bp], writes=[BqT[oc]])
                else:
                    c.op("act", lambda e: e.activation(out=qT[:, oc, :], in_=ps[:], func=AF.Copy, scale=1.0 / 16), reads=[Bp], writes=[BqT[oc]])

            proj("wq", mx, Bmx, evac_q)
            for xh in range(4):
                for mb in range(2):
                    ps, Bp = nextps()
                    mm_group(ps[:], Bp, [(KT[:, 2 * xh + dc, mb * 128:(mb + 1) * 128], qT[:, 2 * xh + dc, :]) for dc in range(2)],
                             [[BKT[2 * xh + dc], BqT[2 * xh + dc]] for dc in range(2)])
                    c.op("act", lambda e: e.activation(out=P[mb][:], in_=ps[:], func=AF.Exp), reads=[Bp], writes=[BP[mb]])
                ps, Bp = nextps()
                mm_group(ps[:], Bp, [(cst["ones"][:], P[mb][:]) for mb in range(2)], [[BP[mb]] for mb in range(2)])
                c.op("dve", lambda e: e.reciprocal(out=rden[:], in_=ps[:]), reads=[Bp], writes=[Brden])
                for dc in range(2):
                    oc = 2 * xh + dc
                    ps, Bp = nextps()
                    mm_group(ps[:], Bp, [(V[:, mb, oc * 128:(oc + 1) * 128], P[mb][:]) for mb in range(2)], [[BV[oc], BP[mb]] for mb in range(2)])
                    c.op("dve", lambda e: e.tensor_tensor(out=on[:, oc, :], in0=ps[:], in1=rden[:], op=ALU.mult), reads=[Bp, Brden], writes=[Bon[oc]])
            proj("wo", on, Bon, evac_hd)
            postnorm_residual(c, sb_, hd, Bhd, xt[b], Bxt[b], prm["post_mem"], cst, "mem")
            c.dma("sp", out=xv[:, :, sl], in_=xt[b][:], reads=Bxt[b])
    c.barrier()


NPAR = 272


def build_program(nl=NL, debug=False):
    nc = bass.Bass("TRN2", target_bir_lowering=False)
    di = lambda n, shp: nc.dram_tensor(n, list(shp), F32, kind="ExternalInput").ap()
    xin = di("xin", (D, S)); memT = di("memT", (D, NMEM))
    w_in = di("w_in", (NL, D, IN_COLS)); w_out = di("w_out", (NL, D, D))
    wq = di("wq_mem", (NL, D, D)); wk = di("wk_mem", (NL, D, D)); wv = di("wv_mem", (NL, D, D)); wo = di("wo_mem", (NL, D, D))
    w_up = di("w_up", (NL, D, 2 * DFF)); w_down = di("w_down", (NL, DFF, D))
    par = di("par", (128, NL, NPAR)); gb = di("gb", (4, NL, 2))
    cin = di("cin", (128, 3, 128)); tabd = di("tab", (128, 24, 256)); seld = di("sel65", (65, 64))
    ca = mlstm_const_arrays()
    cd = {k: di(k, v.shape) for k, v in ca.items()}
    yT = nc.dram_tensor("yT", [D, S], F32, kind="ExternalOutput").ap()
    sk = "ExternalOutput" if debug else "Internal"
    projT = nc.dram_tensor("projT", [3584, S], BF16, kind=sk).ap()
    gatesT = nc.dram_tensor("gatesT", [8, S], F32, kind=sk).ap()
    mixedT = nc.dram_tensor("mixedT", [D, S], BF16, kind=sk).ap()
    hffT = nc.dram_tensor("hffT", [DFF, S], BF16, kind=sk).ap()
    with ExitStack() as es:
        c = Ctx(nc, es)
        cst = mlstm_consts(c, es, cd)
        B = Buf()
        cc = c.sb(es, (128, 3, 128), BF16, "cc")
        for i in range(3):
            c.dma("pool", out=cc[:, i, :], in_=cin[:, i, :], writes=[B])
        cst["avgD"] = cc[:, 0, :]; cst["avgH"] = cc[:, 1, :]; cst["ones"] = cc[:, 2, :]
        cst["epsc"] = c.sb(es, (128, 1), F32, "epsc"); c.op("dve", lambda e: e.memset(cst["epsc"][:], EPS), writes=[B])
        cst["sel65"] = c.sb(es, (65, 64), F32, "sel65"); c.dma("sp", out=cst["sel65"][:], in_=seld, writes=[B])
        cst["tab"] = c.sb(es, (128, 24, 256), BF16, "tab")
        with ExitStack() as s0:
            tf = c.sb(s0, (128, 24, 256), F32, "tabf"); Bt = Buf()
            c.dma("sp", out=tf[:], in_=tabd, writes=[Bt])
            c.op("pool", lambda e: e.tensor_copy(out=cst["tab"][:], in_=tf[:]), reads=[Bt], writes=[B])
            c.barrier()
        pt = c.sb(es, (128, NL, NPAR), F32, "par"); c.dma("sp", out=pt[:], in_=par, writes=[B])
        gbt = c.sb(es, (4, NL, 2), F32, "gb"); c.dma("sp", out=gbt[:], in_=gb, writes=[B])
        c.dma("sp", out=yT, in_=xin, writes=[B])
        c.barrier()
        for l in range(nl):
            P = lambda a, b_: pt[:, l, a:b_]
            Bproj = bufs(29); Bmixed = bufs(12); Bhff = bufs(NFF)
            with ExitStack() as s1:
                hT = c.sb(s1, (128, 8, S), BF16, "hT")
                BhT = [bufs(8) for _ in range(NTT)]
                norm_pass(c, yT, hT, BhT, cst)
                inproj(c, w_in[l], P(0, 8), hT, BhT, projT, gatesT, Bproj)
            attention(c, projT, mixedT, cst, Bproj, Bmixed)
            mlstm(c, projT, gatesT, mixedT, Bproj, Bmixed, P(56, 96).rearrange("p (c f) -> p c f", f=5), gbt[:, l, :], cst)
            W = {"w_out": w_out[l], "wq": wq[l], "wk": wk[l], "wv": wv[l], "wo": wo[l]}
            prm = {"gmix": P(8, 16), "post_mix": P(16, 24), "pre_mem": P(24, 32), "post_mem": P(32, 40)}
            token_phase(c, l, W, prm, mixedT, Bmixed, yT, memT, cst)
            with ExitStack() as s1:
                hT = c.sb(s1, (128, 8, S), BF16, "hT")
                BhT = [bufs(8) for _ in range(NTT)]
                norm_pass(c, yT, hT, BhT, cst)
                ffn_up(c, w_up[l], P(40, 48), P(96, 272).rearrange("p (c f) -> p c f", f=4), hT, BhT, hffT, Bhff)
            ffn_down(c, w_down[l], P(48, 56), hffT, Bhff, yT, cst)
        c.barrier()
    return nc


def host_inputs(inp):
    col = lambda v: np.asarray(v, np.float32).reshape(-1, 128).T
    par = np.zeros((128, NL, NPAR), np.float32)
    gb = np.zeros((4, NL, 2), np.float32)
    for l in range(NL):
        par[:, l, 0:8] = col(inp["pre_mix_g"][l])
        par[:, l, 8:16] = col(np.concatenate([inp["attn_out_g"][l], inp["mlstm_out_g"][l]]))
        par[:, l, 16:24] = col(inp["post_mix_g"][l]); par[:, l, 24:32] = col(inp["pre_mem_g"][l]); par[:, l, 32:40] = col(inp["post_mem_g"][l])
        par[:, l, 40:48] = col(inp["pre_ffn_g"][l]); par[:, l, 48:56] = col(inp["post_ffn_g"][l])
        mc = np.concatenate([inp["mconv_w"][l], inp["mconv_b"][l][None]], 0)
        par[:, l, 56:96] = mc.reshape(5, 8, 128).transpose(2, 1, 0).reshape(128, 40)
        fc = np.concatenate([inp["fconv_w"][l], inp["fconv_b"][l][None]], 0)
        par[:, l, 96:272] = fc.reshape(4, 44, 128).transpose(2, 1, 0).reshape(128, 176)
        gb[:, l, 0] = inp["b_igate"][l]; gb[:, l, 1] = inp["b_fgate"][l]
    sel = np.zeros((65, 64), np.float32); sel[64] = 1.0
    cin = np.stack([np.full((128, 128), 1 / 1024), np.full((128, 128), 1 / 512), np.ones((128, 128))], 1).astype(np.float32)
    shared = {"par": par, "gb": gb, "cin": cin, "tab": make_tables(np.asarray(inp["rel_bias"], np.float32)), "sel65": sel}
    shared.update(mlstm_const_arrays())
    for k in ("w_in", "w_out", "wq_mem", "wk_mem", "wv_mem", "wo_mem", "w_up", "w_down"):
        shared[k] = np.ascontiguousarray(inp[k], dtype=np.float32)
    maps = []
    for b in range(4):
        m = dict(shared)
        m["xin"] = np.ascontiguousarray(np.asarray(inp["x"][b], np.float32).T)
        m["memT"] = np.ascontiguousarray(np.asarray(inp["mem"][b], np.float32).T)
        maps.append(m)
    return maps


def kernel(**inputs):
    nc = build_program(NL)
    maps = host_inputs(inputs)
    res = run_bass_kernel_spmd(nc, maps, core_ids=[0, 1, 2, 3])
    out = np.stack([np.ascontiguousarray(np.asarray(r["yT"]).T) for r in res.results], 0)
    return out.astype(np.float32)
```

```python
import numpy as np
from contextlib import ExitStack
import concourse.bass as bass
import concourse.mybir as mybir
from concourse.bass_utils import run_bass_kernel_spmd
from concourse.alu_op_type import AluOpType as ALU

AF = mybir.ActivationFunctionType
AX = mybir.AxisListType
F32 = mybir.dt.float32
BF16 = mybir.dt.bfloat16

S = 4096
D = 1024
NL = 4
TT = 512
NTT = S // TT
EPS = 1e-6
IN_COLS = 3592
DFF = 2816
NFF = DFF // 128
NMEM = 256


class Buf:
    __slots__ = ("w", "r")

    def __init__(self):
        self.w = None
        self.r = {}


def bufs(n):
    return [Buf() for _ in range(n)]


class Ctx:
    def __init__(self, nc, es, n_dma_sems=40):
        self.nc = nc
        self.es = es
        self.eng = dict(pe=nc.tensor, dve=nc.vector, act=nc.scalar, pool=nc.gpsimd, sp=nc.sync)
        self.esem = {}
        self.cnt = {}
        self.nsem = 0
        for e in self.eng:
            self._new_esem(e)
        self.waited = {}
        self.dsems = [es.enter_context(nc.semaphore(f"dq{i}")) for i in range(n_dma_sems)]
        self.dval = [0] * n_dma_sems
        self.dnext = 0
        self.n_hw = n_dma_sems - 6
        self.dnext_sw = 0
        self.recent_dma = {}
        self.uid = 0
        self.ninstr = 0

    def _new_esem(self, e):
        self.nsem += 1
        self.esem[e] = self.es.enter_context(self.nc.semaphore(f"e{e}{self.nsem}"))
        self.cnt[e] = 0

    def name(self, p):
        self.uid += 1
        return f"{p}_{self.uid}"

    def sb(self, st, shape, dtype, name="t"):
        return st.enter_context(self.nc.sbuf_tensor(self.name(name), list(shape), dtype))

    def ps(self, st, shape=(128, 512), dtype=F32, name="ps"):
        return st.enter_context(self.nc.psum_tensor(self.name(name), list(shape), dtype))

    def _wait(self, e, tok, raw=False):
        sem, val, owner = tok
        if owner == e and (e == "pe" or e == "sp"):
            return
        key = (e, id(sem))
        if self.waited.get(key, 0) >= val:
            return
        self.waited[key] = val
        self.eng[e].wait_ge(sem, val)
        self.ninstr += 1

    def _deps(self, e, reads, writes):
        for b in reads:
            if b.w is not None:
                self._wait(e, b.w, raw=True)
        for b in writes:
            if b.w is not None:
                self._wait(e, b.w)
            for t in b.r.values():
                self._wait(e, t)

    def _mark(self, tok, reads, writes):
        for b in reads:
            k = id(tok[0])
            o = b.r.get(k)
            if o is None or o[1] < tok[1]:
                b.r[k] = tok
        for b in writes:
            b.w = tok
            b.r = {}

    def op(self, e, fn, reads=(), writes=(), inc=True):
        self._deps(e, reads, writes)
        ins = fn(self.eng[e])
        self.ninstr += 1
        if inc:
            if self.cnt[e] >= 30000:
                self._new_esem(e)
            self.cnt[e] += 1
            ins.then_inc(self.esem[e], 1)
            tok = (self.esem[e], self.cnt[e], e)
        else:
            if self.cnt[e] >= 30000:
                self._new_esem(e)
            tok = (self.esem[e], self.cnt[e] + 1, e)
        self._mark(tok, reads, writes)
        return tok

    def dma(self, q, out, in_, reads=(), writes=()):
        if q == "pool":
            i = self.n_hw + self.dnext_sw
            self.dnext_sw = (self.dnext_sw + 1) % (len(self.dsems) - self.n_hw)
        else:
            i = self.dnext
            self.dnext = (i + 1) % self.n_hw
        sem = self.dsems[i]
        old = self.dval[i]
        self._deps(q, reads, writes)
        if old > 0:
            self._wait(q, (sem, old, None))
        ins = self.eng[q].dma_start(out=out, in_=in_)
        self.ninstr += 1
        self.dval[i] = old + 16
        ins.then_inc(sem, 16)
        tok = (sem, old + 16, "dma")
        self.recent_dma[id(sem)] = tok
        self._mark(tok, reads, writes)
        return tok

    def barrier(self, engines=("pe", "dve", "act", "pool", "sp")):
        toks = [(self.esem[e], self.cnt[e], e) for e in self.eng if self.cnt[e] > 0]
        toks += list(self.recent_dma.values())
        for e in engines:
            for t in toks:
                self._wait(e, t, raw=True)
        self.recent_dma = {}


import math


def norm_pass(c, xT, hT, BhT, cst):
    nc = c.nc
    xv = xT.rearrange("(kc p) t -> p kc t", p=128)
    with ExitStack() as st:
        xt = [c.sb(st, (128, 8, TT), F32, "xt") for _ in range(2)]
        sq = [c.sb(st, (128, 8, TT), BF16, "sq") for _ in range(2)]
        rs = [c.sb(st, (128, TT), F32, "rs") for _ in range(2)]
        pss = [c.ps(st) for _ in range(2)]
        Bxt = bufs(2); Bsq = [bufs(8) for _ in range(2)]; Brs = bufs(2); Bps = bufs(2)
        for tt in range(NTT):
            b = tt % 2
            sl = slice(tt * TT, (tt + 1) * TT)
            c.dma("sp", out=xt[b][:], in_=xv[:, :, sl], writes=[Bxt[b]])
            for kc in range(8):
                c.op("act", lambda e: e.activation(out=sq[b][:, kc, :], in_=xt[b][:, kc, :], func=AF.Square),
                     reads=[Bxt[b]], writes=[Bsq[b][kc]])
            for kc in range(8):
                c.op("pe", lambda e: e.matmul(pss[b][:], lhsT=cst["avgD"][:], rhs=sq[b][:, kc, :], start=(kc == 0), stop=(kc == 7)),
                     reads=[Bsq[b][kc]], writes=[Bps[b]] if kc == 0 else [], inc=(kc == 7))
            Bps[b].w = (c.esem["pe"], c.cnt["pe"], "pe")
            c.op("act", lambda e: e.activation(out=rs[b][:], in_=pss[b][:], func=AF.Sqrt, bias=cst["epsc"][:, 0:1]),
                 reads=[Bps[b]], writes=[Brs[b]])
            c.op("dve", lambda e: e.reciprocal(out=rs[b][:], in_=rs[b][:]),
                 reads=[Brs[b]], writes=[Brs[b]])
            for kc in range(8):
                en = "dve" if kc % 2 == 0 else "pool"
                c.op(en, lambda e: e.tensor_tensor(out=hT[:, kc, sl], in0=xt[b][:, kc, :], in1=rs[b][:], op=ALU.mult),
                     reads=[Bxt[b], Brs[b]], writes=[BhT[tt][kc]])
    c.barrier()


def inproj(c, w_in, gcol, hT, BhT, projT, gatesT, Bproj):
    wv = w_in.rearrange("(kc p) n -> p kc n", p=128)
    with ExitStack() as st:
        ws = [c.sb(st, (128, 8, 128), F32, "ws") for _ in range(2)]
        wb = [c.sb(st, (128, 8, 128), BF16, "wb") for _ in range(2)]
        ob = [c.sb(st, (128, S), BF16, "ob") for _ in range(2)]
        og = c.sb(st, (8, S), F32, "og")
        pss = [c.ps(st) for _ in range(4)]
        Bws = bufs(2); Bwb = [bufs(8) for _ in range(2)]; Bob = [bufs(NTT) for _ in range(2)]; Bps = bufs(4)
        Bog = bufs(NTT)
        k = 0
        for fc in range(29):
            ncol = 128 if fc < 28 else 8
            b = fc % 2
            c.dma("sp", out=ws[b][:, :, :ncol], in_=wv[:, :, fc * 128: fc * 128 + ncol], writes=[Bws[b]])
            c.op("pool", lambda e: e.tensor_tensor(out=wb[b][:, :, :ncol], in0=ws[b][:, :, :ncol], in1=gcol.unsqueeze(2).to_broadcast([128, 8, ncol]), op=ALU.mult),
                 reads=[Bws[b]], writes=Bwb[b])
            for tt in range(NTT):
                sl = slice(tt * TT, (tt + 1) * TT)
                p = k % 4
                for kc in range(8):
                    c.op("pe", lambda e: e.matmul(pss[p][:ncol, :], lhsT=wb[b][:, kc, :ncol], rhs=hT[:, kc, sl], start=(kc == 0), stop=(kc == 7)),
                         reads=[Bwb[b][kc], BhT[tt][kc]], writes=[Bps[p]] if kc == 0 else [], inc=(kc == 7))
                Bps[p].w = (c.esem["pe"], c.cnt["pe"], "pe")
                if fc == 28:
                    c.op("dve", lambda e: e.tensor_copy(out=og[:, sl], in_=pss[p][:8, :]), reads=[Bps[p]], writes=[Bog[tt]])
                elif fc < 4:
                    if k % 2 == 0:
                        c.op("act", lambda e: e.activation(out=ob[b][:, sl], in_=pss[p][:], func=AF.Copy, scale=0.125), reads=[Bps[p]], writes=[Bob[b][tt]])
                    else:
                        c.op("dve", lambda e: e.tensor_scalar(out=ob[b][:, sl], in0=pss[p][:], scalar1=0.125, scalar2=None, op0=ALU.mult), reads=[Bps[p]], writes=[Bob[b][tt]])
                else:
                    if k % 2 == 0:
                        c.op("act", lambda e: e.activation(out=ob[b][:, sl], in_=pss[p][:], func=AF.Copy), reads=[Bps[p]], writes=[Bob[b][tt]])
                    else:
                        c.op("dve", lambda e: e.tensor_copy(out=ob[b][:, sl], in_=pss[p][:]), reads=[Bps[p]], writes=[Bob[b][tt]])
                k += 1
            if fc == 28:
                c.dma("sp", out=gatesT[:, :], in_=og[:], reads=Bog, writes=[Bproj[28]])
            else:
                c.dma("sp", out=projT[fc * 128:(fc + 1) * 128, :], in_=ob[b][:], reads=Bob[b], writes=[Bproj[fc]])
    c.barrier()


def load_consts(c, st, cdram):
    cst = {}
    B = Buf()
    def ld(name, shape, dtype):
        t = c.sb(st, shape, dtype, name)
        c.dma("pool" if dtype == BF16 else "sp", out=t[:], in_=cdram[name], writes=[B])
        cst[name] = t
    ld("avgD", (128, 128), BF16)
    t = c.sb(st, (128, 1), F32, "epsc")
    c.op("dve", lambda e: e.memset(t[:], EPS), writes=[B])
    cst["epsc"] = t
    return cst, B


DILS = (1, 4, 16)

def attention(c, projT, mixedT, cst, Bproj, Bmixed):
    with ExitStack() as st:
        qkv = [c.sb(st, (128, S), BF16, "qkv") for _ in range(3)]
        perm = [[c.sb(st, (128, S), BF16, "perm") for _ in range(3)] for _ in range(2)]
        vtok = [c.sb(st, (128, 32, 2, 65), BF16, "vtok") for _ in range(3)]
        acc = [c.sb(st, (65, S), F32, "acc") for _ in range(2)]
        pt = [c.sb(st, (128, 2, 256), BF16, "pt") for _ in range(3)]
        rec = [c.sb(st, (64, TT), F32, "rec") for _ in range(2)]
        ao = [c.sb(st, (64, S), BF16, "ao") for _ in range(2)]
        sp = [c.ps(st, (128, 2, 256), F32, "sp") for _ in range(2)]
        tps = [c.ps(st, (128, 8, 128), BF16, "tp") for _ in range(2)]
        ops = [c.ps(st, (128, 4, 128), F32, "ops") for _ in range(3)]
        dps = ops[2:3]
        Bqkv = bufs(3); Bperm = [bufs(3) for _ in range(2)]; Bvt = [bufs(8) for _ in range(3)]
        Bacc = bufs(2); Bpt = bufs(3); Brec = bufs(2); Bao = [bufs(NTT) for _ in range(2)]
        Bsp = bufs(2); Btp = bufs(2); Bo = bufs(3); Bdps = Bo[2:3]
        ident = cst["ident"]; tab = cst["tab"]; sel = cst["sel65"]
        for pi in range(3):
            c.op("pool", lambda e: e.memset(vtok[pi][:, :, :, 64:65], 1.0), writes=Bvt[pi])
        kq = 0
        for hp in range(4):
            for i in range(3):
                c.dma("sp", out=qkv[i][:], in_=projT[i * 512 + hp * 128: i * 512 + hp * 128 + 128, :], reads=[Bproj[i * 4 + hp]], writes=[Bqkv[i]])
            for pi, dil in enumerate(DILS):
                nbc = 32 // dil
                if dil == 1:
                    src = qkv; Bsrc = Bqkv
                else:
                    src = perm[pi - 1]; Bsrc = Bperm[pi - 1]
                    for i in range(3):
                        c.op("pool", lambda e: e.tensor_copy(out=src[i][:].rearrange("p (c i) -> p c i", c=dil),
                                                             in_=qkv[i][:].rearrange("p (i c) -> p c i", c=dil)),
                             reads=[Bqkv[i]], writes=[Bsrc[i]])
                qP, kP, vP = src
                for g in range(8):
                    tb = g % 2
                    for j in range(4):
                        blk = g * 4 + j
                        c.op("pe", lambda e: e.transpose(out=tps[tb][:, j, :], in_=vP[:, blk * 128:(blk + 1) * 128], identity=ident[:]),
                             reads=[Bsrc[2]], writes=[Btp[tb]] if j == 0 else [], inc=(j == 3))
                    Btp[tb].w = (c.esem["pe"], c.cnt["pe"], "pe")
                    en = "dve" if g % 2 == 0 else "act"
                    o_ap = vtok[pi][:, g * 4:(g + 1) * 4, :, 0:64]
                    i_ap = tps[tb][:, 0:4, :].rearrange("p j (h d) -> p j h d", h=2)
                    if en == "dve":
                        c.op("dve", lambda e: e.tensor_copy(out=o_ap, in_=i_ap), reads=[Btp[tb]], writes=[Bvt[pi][g]])
                    else:
                        c.op("act", lambda e: e.activation(out=o_ap, in_=i_ap, func=AF.Copy), reads=[Btp[tb]], writes=[Bvt[pi][g]])
                for h in range(2):
                    hd = hp * 2 + h
                    rows = slice(h * 64, h * 64 + 64)
                    accv = acc[h][:, :].rearrange("p (i c) -> p c i", c=dil)
                    for cl in range(dil):
                        for n2 in range(0, nbc, 2):
                            s_ = kq % 2; p_ = kq % 3; kq += 1
                            for j in range(2):
                                n = n2 + j; gb = cl * nbc + n
                                nq = 256 if n < nbc - 1 else 128
                                c.op("pe", lambda e: e.matmul(sp[s_][:, j, :nq], lhsT=kP[rows, gb * 128:(gb + 1) * 128], rhs=qP[rows, gb * 128: gb * 128 + nq],
                                                              start=True, stop=False),
                                     reads=[Bsrc[0], Bsrc[1]], writes=[Bsp[s_]] if j == 0 else [], inc=False)
                                c.op("pe", lambda e: e.matmul(sp[s_][:, j, :nq], lhsT=ident[:], rhs=tab[:, hd * 3 + pi, :nq], start=False, stop=True),
                                     inc=(j == 1))
                            Bsp[s_].w = (c.esem["pe"], c.cnt["pe"], "pe")
                            last = (n2 + 1 == nbc - 1)
                            if not last:
                                c.op("act", lambda e: e.activation(out=pt[p_][:], in_=sp[s_][:], func=AF.Exp), reads=[Bsp[s_]], writes=[Bpt[p_]])
                            else:
                                c.op("act", lambda e: e.activation(out=pt[p_][:, 0, :], in_=sp[s_][:, 0, :], func=AF.Exp), reads=[Bsp[s_]], writes=[Bpt[p_]])
                                c.op("act", lambda e: e.activation(out=pt[p_][:, 1, :128], in_=sp[s_][:, 1, :128], func=AF.Exp), reads=[Bsp[s_]], writes=[])
                                Bpt[p_].w = (c.esem["act"], c.cnt["act"], "act")
                            for j in range(2):
                                n = n2 + j; gb = cl * nbc + n
                                oi = gb % 3
                                ot = ops[oi][:65, 0, :]
                                c.op("pe", lambda e: e.matmul(ot, lhsT=vtok[pi][:, gb, h, :], rhs=pt[p_][:, j, 0:128], start=(n == 0), stop=True),
                                     reads=[Bpt[p_], Bvt[pi][gb // 4]], writes=[Bo[oi]] if n == 0 else [], inc=True)
                                Bo[oi].w = (c.esem["pe"], c.cnt["pe"], "pe")
                                av = accv[:, cl, n * 128:(n + 1) * 128]
                                if pi == 0:
                                    c.op("dve", lambda e: e.tensor_copy(out=av, in_=ot), reads=[Bo[oi]], writes=[Bacc[h]])
                                else:
                                    c.op("dve", lambda e: e.tensor_tensor(out=av, in0=av, in1=ot, op=ALU.add), reads=[Bo[oi], Bacc[h]], writes=[Bacc[h]])
                                if n < nbc - 1:
                                    oi2 = (gb + 1) % 3
                                    ot2 = ops[oi2][:65, 0, :]
                                    c.op("pe", lambda e: e.matmul(ot2, lhsT=vtok[pi][:, gb, h, :], rhs=pt[p_][:, j, 128:256], start=True, stop=False),
                                         reads=[Bpt[p_], Bvt[pi][gb // 4]], writes=[Bo[oi2]], inc=False)
            for h in range(2):
                hd = hp * 2 + h
                for tt in range(NTT):
                    sl = slice(tt * TT, (tt + 1) * TT)
                    r_ = tt % 2
                    c.op("pe", lambda e: e.matmul(dps[0][:64, :, :], lhsT=sel[:65, :], rhs=acc[h][:65, sl], start=True, stop=True),
                         reads=[Bacc[h]], writes=[Bdps[0]])
                    c.op("dve", lambda e: e.reciprocal(out=rec[r_][:], in_=dps[0][:64, :, :].rearrange("p a b -> p (a b)")), reads=[Bdps[0]], writes=[Brec[r_]])
                    c.op("pool", lambda e: e.tensor_tensor(out=ao[h][:, sl], in0=acc[h][:64, sl], in1=rec[r_][:], op=ALU.mult),
                         reads=[Bacc[h], Brec[r_]], writes=[Bao[h][tt]])
                c.dma("sp", out=mixedT[hd * 64:(hd + 1) * 64, :], in_=ao[h][:], reads=Bao[h], writes=[Bmixed[hd]])
    c.barrier()


def t5_bucket(dist):
    dist = np.asarray(dist)
    max_exact = 16
    d_f = np.maximum(dist, 1).astype(np.float32)
    large = max_exact + (np.log(d_f / max_exact) / np.log(2048 / max_exact) * (32 - max_exact)).astype(np.int32)
    large = np.minimum(large, 31)
    return np.where(dist < max_exact, dist, large)


def make_tables(rel_bias):
    s = np.arange(128)[:, None]
    t = np.arange(128)[None, :]
    tabs = np.full((128, 24, 256), -30000.0, np.float32)
    for pi, dil in enumerate(DILS):
        bsub = rel_bias[t5_bucket(np.arange(129) * dil)]
        d0 = t - s
        d1 = t + 128 - s
        for h in range(8):
            tabs[:, h * 3 + pi, 0:128] = np.where(d0 >= 0, bsub[np.clip(d0, 0, 128), h], -30000.0)
            tabs[:, h * 3 + pi, 128:256] = np.where(d1 <= 128, bsub[np.clip(d1, 0, 128), h], -30000.0)
    return tabs


def mlstm(c, projT, gatesT, mixedT, Bproj, Bmixed, mcp, gbias, cst, hook=None):
    ident = cst["ident"]; identf = cst["identf"]; cm = cst["cmask"]
    with ExitStack() as st:
        et = c.sb(st, (128, 32, 8), F32, "et"); Bet = Buf()
        decb = c.sb(st, (128, 128), F32, "decb"); Bdecb = Buf()
        with ExitStack() as s0:
            gi = c.sb(s0, (4, S), F32, "gi"); gf = c.sb(s0, (4, S), F32, "gf"); nb = c.sb(s0, (4, S), F32, "nb")
            dmb = c.sb(s0, (4, S), F32, "dmb"); t1 = c.sb(s0, (4, S), F32, "t1"); t2 = c.sb(s0, (4, S), F32, "t2")
            sm = c.sb(s0, (4, 8, 32), F32, "sm")
            decbd = c.sb(s0, (4, 4, 32), F32, "decbd"); nbf = c.sb(s0, (4, 2), F32, "nbf")
            pg = c.ps(s0, (128, 32, 8), F32, "pg"); pd = c.ps(s0, (128, 128), F32, "pd")
            G = Buf()
            c.dma("sp", out=gi[:], in_=gatesT[0:4, :], reads=[Bproj[28]], writes=[G])
            c.dma("sp", out=gf[:], in_=gatesT[4:8, :], reads=[Bproj[28]], writes=[G])
            def g(en, fn):
                c.op(en, fn, reads=[G], writes=[G])
            g("dve", lambda e: e.tensor_scalar(out=nbf[:, 0:1], in0=gbias[:, 1:2], scalar1=-1.0, scalar2=None, op0=ALU.mult))
            g("dve", lambda e: e.memset(nbf[:, 1:2], -0.5 * math.log(128.0)))
            g("act", lambda e: e.activation(out=t1[:], in_=gf[:], func=AF.Exp, scale=-1.0, bias=nbf[:, 0:1]))
            g("act", lambda e: e.activation(out=t1[:], in_=t1[:], func=AF.Ln, bias=cst["one1"][:4, 0:1]))
            g("dve", lambda e: e.tensor_tensor_scan(out=nb[:], data0=cst["rmask"][:4, :], data1=t1[:], initial=0.0, op0=ALU.mult, op1=ALU.add))
            g("dve", lambda e: e.scalar_tensor_tensor(out=dmb[:], in0=gi[:], scalar=gbias[:, 0:1], in1=nb[:], op0=ALU.add, op1=ALU.add))
            g("dve", lambda e: e.tensor_reduce(out=sm[:, 0, :], in_=dmb[:].rearrange("p (c s) -> p c s", s=128), axis=AX.X, op=ALU.max))
            g("dve", lambda e: e.tensor_scalar(out=sm[:, 1, :], in0=nb[:].rearrange("p (c s) -> p c s", s=128)[:, :, 127], scalar1=-1.0, scalar2=None, op0=ALU.mult))
            g("dve", lambda e: e.tensor_tensor_scan(out=sm[:, 2, :], data0=sm[:, 0, :], data1=sm[:, 1, :], initial=0.0, op0=ALU.max, op1=ALU.add))
            g("dve", lambda e: e.memset(sm[:, 3, 0:1], 0.0))
            g("dve", lambda e: e.tensor_copy(out=sm[:, 3, 1:32], in_=sm[:, 2, 0:31]))
            g("dve", lambda e: e.tensor_tensor(out=sm[:, 4, :], in0=sm[:, 3, :], in1=sm[:, 0, :], op=ALU.max))
            g("dve", lambda e: e.tensor_tensor(out=sm[:, 5, :], in0=sm[:, 3, :], in1=sm[:, 4, :], op=ALU.subtract))
            g("act", lambda e: e.activation(out=sm[:, 5, :], in_=sm[:, 5, :], func=AF.Exp))
            Mb = sm[:, 4, :].unsqueeze(2).to_broadcast([4, 32, 128])
            g("dve", lambda e: e.tensor_tensor(out=t1[:].rearrange("p (c s) -> p c s", s=128), in0=dmb[:].rearrange("p (c s) -> p c s", s=128), in1=Mb, op=ALU.subtract))
            g("act", lambda e: e.activation(out=t1[:], in_=t1[:], func=AF.Exp, bias=nbf[:, 1:2]))
            g("dve", lambda e: e.tensor_tensor(out=t2[:].rearrange("p (c s) -> p c s", s=128), in0=nb[:].rearrange("p (c s) -> p c s", s=128), in1=Mb, op=ALU.subtract))
            g("act", lambda e: e.activation(out=t2[:], in_=t2[:], func=AF.Exp))
            for cc in range(32):
                cs = slice(cc * 128, (cc + 1) * 128)
                c.op("pe", lambda e: e.transpose(out=pg[:, cc, 0:4], in_=t1[:, cs], identity=identf[:4, :4]), reads=[G], writes=[G] if cc == 0 else [], inc=False)
                c.op("pe", lambda e: e.transpose(out=pg[:, cc, 4:8], in_=t2[:, cs], identity=identf[:4, :4]), reads=[G], inc=(cc == 31))
            G.w = (c.esem["pe"], c.cnt["pe"], "pe")
            c.op("dve", lambda e: e.tensor_copy(out=et[:], in_=pg[:]), reads=[G], writes=[Bet])
            g("dve", lambda e: e.tensor_tensor(out=decbd[:], in0=sm[:, 5, :].unsqueeze(1).to_broadcast([4, 4, 32]), in1=cst["bdmask"][:4, :].rearrange("p (a b) -> p a b", a=4), op=ALU.mult))
            c.op("pe", lambda e: e.matmul(pd[:], lhsT=cst["ones4"][:4, :], rhs=decbd[:].rearrange("p a b -> p (a b)"), start=True, stop=True), reads=[G], writes=[G])
            c.op("dve", lambda e: e.tensor_copy(out=decb[:], in_=pd[:]), reads=[G], writes=[Bdecb])
            c.barrier()
        raw = [c.sb(st, (128, 3 + S), BF16, "raw") for _ in range(2)]; Braw = bufs(2)
        cv = [c.sb(st, (128, S), F32, "cv") for _ in range(2)]; Bcv = bufs(2)
        qT = c.sb(st, (128, S), BF16, "qT"); kT = c.sb(st, (128, S), BF16, "kT"); BqT = Buf(); BkT = Buf()
        vT = c.sb(st, (128, S), BF16, "vT"); oT = c.sb(st, (128, S), BF16, "oT"); BvT = Buf(); BoT = Buf()
        hmT = c.sb(st, (128, S), BF16, "hmT"); BhmT = bufs(32)
        ktk = [c.sb(st, (128, 128), BF16, "ktk") for _ in range(2)]; Bktk = bufs(2)
        vaug = [c.sb(st, (128, 129), BF16, "vaug") for _ in range(2)]; Bvaug = bufs(2)
        og = [c.sb(st, (128, 128), F32, "og") for _ in range(2)]; Bog = bufs(2)
        pT = [c.sb(st, (128, 128), BF16, "pT") for _ in range(2)]; BpT = bufs(2)
        hmt = [c.sb(st, (128, 128), BF16, "hmt") for _ in range(2)]; Bhmt = bufs(2)
        C = c.sb(st, (128, 129), F32, "C"); BC = Buf()
        Cbf = [c.sb(st, (128, 129), BF16, "Cbf") for _ in range(2)]; BCbf = bufs(2)
        dd = [c.sb(st, (128, 2), F32, "dd") for _ in range(2)]; Bdd = bufs(2)
        tpk = [c.ps(st, (128, 8, 128), BF16, "tpk") for _ in range(2)]; Btpk = bufs(2)
        sps = c.ps(st, (128, 512), F32, "sps"); Bsps = Buf()
        nps = [c.ps(st, (128, 512), F32, "nps") for _ in range(2)]; Bnps = bufs(2)
        dps = c.ps(st, (128, 512), F32, "dps"); Bdps = Buf()
        tph = c.ps(st, (128, 8, 128), BF16, "tph"); Btph = Buf()
        for r in range(2):
            c.op("pool", lambda e: e.memset(raw[r][:, 0:3], 0.0), writes=[Braw[r]])
            c.op("pool", lambda e: e.memset(vaug[r][:, 128:129], 1.0), writes=[Bvaug[r]])
        for hd in range(4):
            c.dma("sp", out=raw[0][:, 3:], in_=projT[1536 + hd * 128:1536 + (hd + 1) * 128, :], reads=[Bproj[12 + hd]], writes=[Braw[0]])
            c.dma("sp", out=raw[1][:, 3:], in_=projT[2048 + hd * 128:2048 + (hd + 1) * 128, :], reads=[Bproj[16 + hd]], writes=[Braw[1]])
            c.dma("sp", out=vT[:], in_=projT[2560 + hd * 128:2560 + (hd + 1) * 128, :], reads=[Bproj[20 + hd]], writes=[BvT])
            c.dma("sp", out=oT[:], in_=projT[3072 + hd * 128:3072 + (hd + 1) * 128, :], reads=[Bproj[24 + hd]], writes=[BoT])
            if hd == 0 and hook is not None:
                hook()
            for i, en, dst, Bdst in ((0, "dve", qT, BqT), (1, "dve", kT, BkT)):
                ci = i * 4 + hd
                c.op(en, lambda e: e.tensor_scalar(out=cv[i][:], in0=raw[i][:, 3:3 + S], scalar1=mcp[:, ci, 3:4], scalar2=mcp[:, ci, 4:5], op0=ALU.mult, op1=ALU.add),
                     reads=[Braw[i]], writes=[Bcv[i]])
                for j in range(3):
                    c.op(en, lambda e: e.scalar_tensor_tensor(out=cv[i][:], in0=raw[i][:, j:j + S], scalar=mcp[:, ci, j:j + 1], in1=cv[i][:], op0=ALU.mult, op1=ALU.add),
                         reads=[Braw[i], Bcv[i]], writes=[Bcv[i]])
                c.op("act", lambda e: e.activation(out=dst[:], in_=cv[i][:], func=AF.Silu), reads=[Bcv[i]], writes=[Bdst])
            for cc in range(32):
                r = cc % 2
                cs = slice(cc * 128, (cc + 1) * 128)
                ecol = et[:, cc, hd:hd + 1]; fcol = et[:, cc, 4 + hd:5 + hd]
                for j, (src, Bsrc) in enumerate(((kT, BkT), (vT, BvT), (oT, BoT))):
                    c.op("pe", lambda e: e.transpose(out=tpk[r][:, j, :], in_=src[:, cs], identity=ident[:]), reads=[Bsrc], writes=[Btpk[r]] if j == 0 else [], inc=(j == 2))
                Btpk[r].w = (c.esem["pe"], c.cnt["pe"], "pe")
                c.op("dve", lambda e: e.tensor_scalar(out=ktk[r][:], in0=tpk[r][:, 0, :], scalar1=ecol, scalar2=None, op0=ALU.mult), reads=[Btpk[r], Bet], writes=[Bktk[r]])
                c.op("act", lambda e: e.activation(out=vaug[r][:, 0:128], in_=tpk[r][:, 1, :], func=AF.Copy), reads=[Btpk[r]], writes=[Bvaug[r]])
                c.op("act", lambda e: e.activation(out=og[r][:], in_=tpk[r][:, 2, :], func=AF.Sigmoid), reads=[Btpk[r]], writes=[Bog[r]])
                c.op("pe", lambda e: e.matmul(sps[:, 0:128], lhsT=kT[:, cs], rhs=qT[:, cs], start=True, stop=True), reads=[BkT, BqT], writes=[Bsps])
                c.op("dve", lambda e: e.scalar_tensor_tensor(out=pT[r][:], in0=sps[:, 0:128], scalar=ecol, in1=cm[:], op0=ALU.mult, op1=ALU.mult), reads=[Bsps, Bet], writes=[BpT[r]])
                if cc > 0:
                    dcol = decb[:, hd * 32 + cc: hd * 32 + cc + 1]
                    c.op("dve", lambda e: e.tensor_scalar(out=C[:], in0=C[:], scalar1=dcol, scalar2=None, op0=ALU.mult), reads=[BC, Bdecb], writes=[BC])
                    c.op("act", lambda e: e.activation(out=Cbf[r][:], in_=C[:], func=AF.Copy), reads=[BC], writes=[BCbf[r]])
                c.op("pe", lambda e: e.matmul(nps[r][:, 0:129], lhsT=pT[r][:], rhs=vaug[r][:], start=True, stop=(cc == 0)), reads=[BpT[r], Bvaug[r]], writes=[Bnps[r]], inc=(cc == 0))
                if cc > 0:
                    c.op("pe", lambda e: e.matmul(nps[r][:, 0:129], lhsT=qT[:, cs], rhs=Cbf[r][:], start=False, stop=True), reads=[BqT, BCbf[r]], inc=True)
                Bnps[r].w = (c.esem["pe"], c.cnt["pe"], "pe")
                c.op("pe", lambda e: e.matmul(dps[:, 0:129], lhsT=ktk[r][:], rhs=vaug[r][:], start=True, stop=True), reads=[Bktk[r], Bvaug[r]], writes=[Bdps])
                if cc == 0:
                    c.op("dve", lambda e: e.tensor_copy(out=C[:], in_=dps[:, 0:129]), reads=[Bdps], writes=[BC])
                else:
                    c.op("dve", lambda e: e.tensor_tensor(out=C[:], in0=C[:], in1=dps[:, 0:129], op=ALU.add), reads=[Bdps, BC], writes=[BC])
                c.op("dve", lambda e: e.tensor_scalar(out=dd[r][:, 1:2], in0=nps[r][:, 128:129], scalar1=-1.0, scalar2=None, op0=ALU.mult), reads=[Bnps[r]], writes=[Bdd[r]])
                c.op("dve", lambda e: e.scalar_tensor_tensor(out=dd[r][:, 0:1], in0=nps[r][:, 128:129], scalar=fcol, in1=dd[r][:, 1:2], op0=ALU.max, op1=ALU.max), reads=[Bnps[r], Bet, Bdd[r]], writes=[Bdd[r]])
                c.op("dve", lambda e: e.reciprocal(out=dd[r][:, 1:2], in_=dd[r][:, 0:1]), reads=[Bdd[r]], writes=[Bdd[r]])
                c.op("dve", lambda e: e.scalar_tensor_tensor(out=hmt[r][:], in0=nps[r][:, 0:128], scalar=dd[r][:, 1:2], in1=og[r][:], op0=ALU.mult, op1=ALU.mult),
                     reads=[Bnps[r], Bdd[r], Bog[r]], writes=[Bhmt[r]])
                c.op("pe", lambda e: e.transpose(out=tph[:, 0, :], in_=hmt[r][:], identity=ident[:]), reads=[Bhmt[r]], writes=[Btph])
                c.op("act", lambda e: e.activation(out=hmT[:, cs], in_=tph[:, 0, :], func=AF.Copy), reads=[Btph], writes=[BhmT[cc]])
            c.dma("sp", out=mixedT[512 + hd * 128: 512 + (hd + 1) * 128, :], in_=hmT[:], reads=BhmT, writes=[Bmixed[8 + hd]])
    c.barrier()


def mlstm_consts(c, st, cd):
    cst = {}; B = Buf()
    cst["identf"] = c.sb(st, (128, 128), F32, "identf"); c.dma("sp", out=cst["identf"][:], in_=cd["identf"], writes=[B])
    cst["ident"] = c.sb(st, (128, 128), BF16, "ident"); c.dma("pool", out=cst["ident"][:], in_=cd["identf"], writes=[B])
    cst["cmask"] = c.sb(st, (128, 128), F32, "cmask"); c.dma("sp", out=cst["cmask"][:], in_=cd["cmask"], writes=[B])
    cst["rmask"] = c.sb(st, (4, S), F32, "rmask"); c.dma("sp", out=cst["rmask"][:], in_=cd["rmask"], writes=[B])
    cst["bdmask"] = c.sb(st, (4, 128), F32, "bdmask"); c.dma("sp", out=cst["bdmask"][:], in_=cd["bdmask"], writes=[B])
    cst["ones4"] = c.sb(st, (4, 128), F32, "ones4"); c.dma("sp", out=cst["ones4"][:], in_=cd["ones4"], writes=[B])
    cst["one1"] = c.sb(st, (128, 1), F32, "one1"); c.op("dve", lambda e: e.memset(cst["one1"][:], 1.0), writes=[B])
    return cst


def mlstm_const_arrays():
    t = np.arange(128)
    cmask = (t[None, :] >= t[:, None]).astype(np.float32)
    rmask = np.ones((4, S), np.float32); rmask[:, ::128] = 0.0
    bd = np.zeros((4, 4, 32), np.float32)
    for h in range(4):
        bd[h, h] = 1.0
    return {"identf": np.eye(128, dtype=np.float32), "cmask": cmask, "rmask": rmask, "bdmask": bd.reshape(4, 128), "ones4": np.ones((4, 128), np.float32)}


def ffn_up(c, w_up, gcol, fcp, hT, BhT, hffT, Bhff):
    wv = w_up.rearrange("(kc p) n -> p kc n", p=128)
    with ExitStack() as st:
        ws = [c.sb(st, (128, 8, 128), F32, "ws") for _ in range(2)]
        wb = [c.sb(st, (128, 8, 128), BF16, "wb") for _ in range(2)]
        u = [[c.sb(st, (128, 2 + S), BF16, "u") for _ in range(2)] for _ in range(2)]
        cv = [c.sb(st, (128, S), F32, "cv") for _ in range(2)]
        gg = c.sb(st, (128, S), BF16, "gg")
        ho = [c.sb(st, (128, S), BF16, "ho") for _ in range(2)]
        pss = [c.ps(st) for _ in range(4)]
        Bws = bufs(2); Bwb = [bufs(8) for _ in range(2)]; Bu = [[bufs(NTT) for _ in range(2)] for _ in range(2)]
        Bcv = bufs(2); Bgg = Buf(); Bho = bufs(2); Bps = bufs(4)
        for par in range(2):
            for half in range(2):
                c.op("pool", lambda e: e.memset(u[par][half][:, 0:2], 0.0), writes=Bu[par][half])
        k = 0

        def ld_w(jj, half):
            ci_ = jj + NFF * half
            c.dma("sp", out=ws[half][:], in_=wv[:, :, ci_ * 128:(ci_ + 1) * 128], writes=[Bws[half]])

        ld_w(0, 0); ld_w(0, 1)
        for j in range(NFF):
            par = j % 2
            for half in range(2):
                ci = j + NFF * half
                b = half
                c.op("pool", lambda e: e.tensor_tensor(out=wb[b][:], in0=ws[b][:], in1=gcol.unsqueeze(2).to_broadcast([128, 8, 128]), op=ALU.mult),
                     reads=[Bws[b]], writes=Bwb[b])
                for tt in range(NTT):
                    sl = slice(tt * TT, (tt + 1) * TT)
                    p = k % 4
                    for kc in range(8):
                        c.op("pe", lambda e: e.matmul(pss[p][:], lhsT=wb[b][:, kc, :], rhs=hT[:, kc, sl], start=(kc == 0), stop=(kc == 7)),
                             reads=[Bwb[b][kc], BhT[tt][kc]], writes=[Bps[p]] if kc == 0 else [], inc=(kc == 7))
                    Bps[p].w = (c.esem["pe"], c.cnt["pe"], "pe")
                    o_ap = u[par][half][:, 2 + tt * TT: 2 + (tt + 1) * TT]
                    c.op("act", lambda e: e.activation(out=o_ap, in_=pss[p][:], func=AF.Copy), reads=[Bps[p]], writes=[Bu[par][half][tt]])
                    k += 1
                if j + 1 < NFF:
                    ld_w(j + 1, half)
            for half, en in ((0, "dve"), (1, "dve")):
                ci = j + NFF * half
                uu = u[par][half]
                c.op(en, lambda e: e.tensor_scalar(out=cv[half][:], in0=uu[:, 2:2 + S], scalar1=fcp[:, ci, 2:3], scalar2=fcp[:, ci, 3:4], op0=ALU.mult, op1=ALU.add),
                     reads=Bu[par][half], writes=[Bcv[half]])
                c.op(en, lambda e: e.scalar_tensor_tensor(out=cv[half][:], in0=uu[:, 1:1 + S], scalar=fcp[:, ci, 1:2], in1=cv[half][:], op0=ALU.mult, op1=ALU.add),
                     reads=Bu[par][half] + [Bcv[half]], writes=[Bcv[half]])
                c.op(en, lambda e: e.scalar_tensor_tensor(out=cv[half][:], in0=uu[:, 0:S], scalar=fcp[:, ci, 0:1], in1=cv[half][:], op0=ALU.mult, op1=ALU.add),
                     reads=Bu[par][half] + [Bcv[half]], writes=[Bcv[half]])
            c.op("act", lambda e: e.activation(out=gg[:], in_=cv[1][:], func=AF.Gelu_apprx_tanh), reads=[Bcv[1]], writes=[Bgg])
            c.op("dve", lambda e: e.tensor_tensor(out=ho[par][:], in0=gg[:], in1=cv[0][:], op=ALU.mult), reads=[Bgg, Bcv[0]], writes=[Bho[par]])
            c.dma("sp", out=hffT[j * 128:(j + 1) * 128, :], in_=ho[par][:], reads=[Bho[par]], writes=[Bhff[j]])
    c.barrier()


def postnorm_residual(c, st_bufs, hsrc, Bh, xt, Bx, gcol, cst, tag):
    sq, Bsq, ps, Bps, rs, Brs, tmp, Btmp = st_bufs
    for oc in range(8):
        c.op("act", lambda e: e.activation(out=sq[:, oc, :], in_=hsrc[:, oc, :], func=AF.Square), reads=[Bh[oc]], writes=[Bsq[oc]])
    for oc in range(8):
        c.op("pe", lambda e: e.matmul(ps[:], lhsT=cst["avgD"][:], rhs=sq[:, oc, :], start=(oc == 0), stop=(oc == 7)),
             reads=[Bsq[oc]], writes=[Bps] if oc == 0 else [], inc=(oc == 7))
    Bps.w = (c.esem["pe"], c.cnt["pe"], "pe")
    c.op("act", lambda e: e.activation(out=rs[:], in_=ps[:], func=AF.Sqrt, bias=cst["epsc"][:, 0:1]), reads=[Bps], writes=[Brs])
    c.op("dve", lambda e: e.reciprocal(out=rs[:], in_=rs[:]), reads=[Brs], writes=[Brs])
    for oc in range(8):
        en = "dve" if oc % 2 == 0 else "pool"
        c.op(en, lambda e: e.tensor_tensor(out=tmp[:, oc, :], in0=hsrc[:, oc, :], in1=rs[:], op=ALU.mult), reads=[Bh[oc], Brs], writes=[Btmp[oc]])
        c.op("dve", lambda e: e.scalar_tensor_tensor(out=xt[:, oc, :], in0=tmp[:, oc, :], scalar=gcol[:, oc:oc + 1], in1=xt[:, oc, :], op0=ALU.mult, op1=ALU.add),
             reads=[Btmp[oc], Bx[oc]], writes=[Bx[oc]])


def ffn_down(c, w_down, gpost, hffT, Bhff, xT, cst):
    wv = w_down.rearrange("(c p) n -> p c n", p=128)
    hv = hffT.rearrange("(c p) t -> p c t", p=128)
    xv = xT.rearrange("(kc p) t -> p kc t", p=128)
    with ExitStack() as st:
        wd = c.sb(st, (128, NFF, D), BF16, "wd")
        wst = [c.sb(st, (128, NFF, 128), F32, "wst") for _ in range(1)]
        hf = [c.sb(st, (128, NFF, TT), BF16, "hf") for _ in range(2)]
        xt = [c.sb(st, (128, 8, TT), F32, "xt") for _ in range(2)]
        hd = c.sb(st, (128, 8, TT), F32, "hd")
        sq = c.sb(st, (128, 8, TT), BF16, "sq"); rs = c.sb(st, (128, TT), F32, "rs")
        pss = [c.ps(st) for _ in range(3)]
        psn = c.ps(st)
        Bwd = bufs(8); Bwst = bufs(1); Bhf = bufs(2); Bxt = [bufs(8) for _ in range(2)]; Bhd = bufs(8)
        Bps = bufs(3)
        sb_ = (sq, bufs(8), psn, Buf(), rs, Buf(), hd, Bhd)
        for oc in range(8):
            b = 0
            c.dma("sp", out=wst[b][:], in_=wv[:, :, oc * 128:(oc + 1) * 128], writes=[Bwst[b]])
            c.op("pool", lambda e: e.tensor_copy(out=wd[:, :, oc * 128:(oc + 1) * 128], in_=wst[b][:]), reads=[Bwst[b]], writes=[Bwd[oc]])
        k = 0

        def ld_t(t_):
            b_ = t_ % 2
            sl_ = slice(t_ * TT, (t_ + 1) * TT)
            c.dma("sp", out=hf[b_][:], in_=hv[:, :, sl_], reads=Bhff, writes=[Bhf[b_]])
            c.dma("sp", out=xt[b_][:], in_=xv[:, :, sl_], writes=Bxt[b_])

        ld_t(0)
        for tt in range(NTT):
            b = tt % 2
            sl = slice(tt * TT, (tt + 1) * TT)
            for oc in range(8):
                p = k % 3; k += 1
                for fc in range(NFF):
                    c.op("pe", lambda e: e.matmul(pss[p][:], lhsT=wd[:, fc, oc * 128:(oc + 1) * 128], rhs=hf[b][:, fc, :], start=(fc == 0), stop=(fc == NFF - 1)),
                         reads=[Bwd[oc], Bhf[b]], writes=[Bps[p]] if fc == 0 else [], inc=(fc == NFF - 1))
                Bps[p].w = (c.esem["pe"], c.cnt["pe"], "pe")
                c.op("dve", lambda e: e.tensor_copy(out=hd[:, oc, :], in_=pss[p][:]), reads=[Bps[p]], writes=[Bhd[oc]])
            postnorm_residual(c, sb_, hd, Bhd, xt[b], Bxt[b], gpost, cst, "ffn")
            if tt + 1 < NTT:
                ld_t(tt + 1)
            c.dma("sp", out=xv[:, :, sl], in_=xt[b][:], reads=Bxt[b])
    c.barrier()


def token_weights_alloc(c, st):
    tw = {}
    tw["wres"] = {n: c.sb(st, (128, 8, D), BF16, n) for n in ("w_out", "wq", "wo")}
    tw["Bwres"] = {n: bufs(8) for n in tw["wres"]}
    tw["ws"] = [c.sb(st, (128, 8, 128), F32, "ws") for _ in range(2)]
    tw["Bws"] = bufs(2)
    tw["kw"] = 0
    return tw


def token_weights_issue(c, tw, W, prm):
    wres, Bwres, ws, Bws = tw["wres"], tw["Bwres"], tw["ws"], tw["Bws"]
    kw = tw["kw"]
    for name, g in (("w_out", prm["gmix"]), ("wq", prm["pre_mem"]), ("wo", None)):
        wv_ = W[name].rearrange("(kc p) n -> p kc n", p=128)
        for oc in range(8):
            b = kw % 2; kw += 1
            c.dma("sp", out=ws[b][:], in_=wv_[:, :, oc * 128:(oc + 1) * 128], writes=[Bws[b]])
            if g is None:
                c.op("pool", lambda e: e.tensor_copy(out=wres[name][:, :, oc * 128:(oc + 1) * 128], in_=ws[b][:]), reads=[Bws[b]], writes=[Bwres[name][oc]])
            else:
                c.op("pool", lambda e: e.tensor_tensor(out=wres[name][:, :, oc * 128:(oc + 1) * 128], in0=ws[b][:], in1=g.unsqueeze(2).to_broadcast([128, 8, 128]), op=ALU.mult),
                     reads=[Bws[b]], writes=[Bwres[name][oc]])
    tw["kw"] = kw


def token_phase(c, l, W, prm, mixedT, Bmixed, xT, memT, cst, tw=None):
    mv = mixedT.rearrange("(kc p) t -> p kc t", p=128)
    xv = xT.rearrange("(kc p) t -> p kc t", p=128)
    with ExitStack() as st:
        preloaded = tw is not None
        if tw is None:
            tw = token_weights_alloc(c, st)
        wres, Bwres, ws, Bws = tw["wres"], tw["Bwres"], tw["ws"], tw["Bws"]
        KT = c.sb(st, (128, 8, NMEM), BF16, "KT"); V = c.sb(st, (128, 2, D), BF16, "V"); mem = c.sb(st, (128, 8, NMEM), BF16, "mem")
        BKT = bufs(8); BV = bufs(8); Bmem = Buf()
        wtmp = [c.sb(st, (128, 8, 128), BF16, "wtmp") for _ in range(2)]; Bwtmp = bufs(2)
        mx = c.sb(st, (128, 8, TT), BF16, "mx"); Bmx = bufs(8)
        sq = c.sb(st, (128, 8, TT), BF16, "sq"); Bsq = bufs(8)
        hd = c.sb(st, (128, 8, TT), F32, "hd"); Bhd = bufs(8)
        xt = [c.sb(st, (128, 8, TT), F32, "xt") for _ in range(2)]; Bxt = [bufs(8) for _ in range(2)]
        qT = c.sb(st, (128, 8, TT), BF16, "qT"); BqT = bufs(8)
        on = c.sb(st, (128, 8, TT), BF16, "on"); Bon = bufs(8)
        P = [c.sb(st, (128, TT), BF16, "P") for _ in range(2)]; BP = bufs(2)
        rs = c.sb(st, (128, TT), F32, "rs"); Brs = Buf()
        rA = c.sb(st, (128, TT), F32, "rA"); BrA = Buf()
        rM = c.sb(st, (128, TT), F32, "rM"); BrM = Buf()
        rden = c.sb(st, (128, TT), F32, "rden"); Brden = Buf()
        NPS = 7
        pss = [c.ps(st) for _ in range(NPS)]; Bps = bufs(NPS)
        psn = c.ps(st); Bpsn = Buf()
        pk = [0]

        def nextps():
            i = pk[0] % NPS; pk[0] += 1
            return pss[i], Bps[i]

        def mm_group(ps, Bp, parts, reads_list):
            n = len(parts)
            for i, (lh, rh) in enumerate(parts):
                c.op("pe", lambda e: e.matmul(ps, lhsT=lh, rhs=rh, start=(i == 0), stop=(i == n - 1)),
                     reads=reads_list[i], writes=[Bp] if i == 0 else [], inc=(i == n - 1))
            Bp.w = (c.esem["pe"], c.cnt["pe"], "pe")

        with ExitStack() as s0:
            mf = c.sb(s0, (128, 8, NMEM), F32, "mf"); Bmf = Buf()
            c.dma("sp", out=mf[:], in_=memT.rearrange("(kc p) m -> p kc m", p=128), writes=[Bmf])
            c.op("pool", lambda e: e.tensor_copy(out=mem[:], in_=mf[:]), reads=[Bmf], writes=[Bmem])
            c.barrier()
        if not preloaded:
            token_weights_issue(c, tw, W, prm)
        kw = tw["kw"]
        for name in ("wk", "wv"):
            wv_ = W[name].rearrange("(kc p) n -> p kc n", p=128)
            for oc in range(8):
                b = kw % 2; kw += 1
                c.dma("sp", out=ws[b][:], in_=wv_[:, :, oc * 128:(oc + 1) * 128], writes=[Bws[b]])
                c.op("pool", lambda e: e.tensor_copy(out=wtmp[b][:], in_=ws[b][:]), reads=[Bws[b]], writes=[Bwtmp[b]])
                if name == "wk":
                    ps, Bp = nextps()
                    mm_group(ps[:, :NMEM], Bp, [(wtmp[b][:, kc, :], mem[:, kc, :]) for kc in range(8)], [[Bwtmp[b], Bmem]] * 8)
                    c.op("dve", lambda e: e.tensor_copy(out=KT[:, oc, :], in_=ps[:, :NMEM]), reads=[Bp], writes=[BKT[oc]])
                else:
                    for mb in range(2):
                        ps, Bp = nextps()
                        mm_group(ps[:, :128], Bp, [(mem[:, kc, mb * 128:(mb + 1) * 128], wtmp[b][:, kc, :]) for kc in range(8)], [[Bwtmp[b], Bmem]] * 8)
                        c.op("dve", lambda e: e.tensor_copy(out=V[:, mb, oc * 128:(oc + 1) * 128], in_=ps[:, :128]), reads=[Bp], writes=[BV[oc]])
        sb_ = (sq, Bsq, psn, Bpsn, rs, Brs, hd, Bhd)
        ek = 0
        for tt in range(NTT):
            b = tt % 2
            sl = slice(tt * TT, (tt + 1) * TT)
            c.dma("sp", out=mx[:], in_=mv[:, :, sl], reads=Bmixed, writes=Bmx)
            c.dma("sp", out=xt[b][:], in_=xv[:, :, sl], writes=Bxt[b])
            for kc in range(8):
                c.op("act", lambda e: e.activation(out=sq[:, kc, :], in_=mx[:, kc, :], func=AF.Square), reads=[Bmx[kc]], writes=[Bsq[kc]])
            for grp, (rr, Brr) in enumerate(((rA, BrA), (rM, BrM))):
                ps, Bp = nextps()
                mm_group(ps[:], Bp, [(cst["avgH"][:], sq[:, grp * 4 + i, :]) for i in range(4)], [[Bsq[grp * 4 + i]] for i in range(4)])
                c.op("act", lambda e: e.activation(out=rr[:], in_=ps[:], func=AF.Sqrt, bias=cst["epsc"][:, 0:1]), reads=[Bp], writes=[Brr])
                c.op("dve", lambda e: e.reciprocal(out=rr[:], in_=rr[:]), reads=[Brr], writes=[Brr])
            for kc in range(8):
                rr, Brr = (rA, BrA) if kc < 4 else (rM, BrM)
                en = "dve" if kc % 2 == 0 else "pool"
                c.op(en, lambda e: e.tensor_tensor(out=mx[:, kc, :], in0=mx[:, kc, :], in1=rr[:], op=ALU.mult), reads=[Bmx[kc], Brr], writes=[Bmx[kc]])

            def proj(wname, src, Bsrc, evac):
                nonlocal ek
                for oc in range(8):
                    ps, Bp = nextps()
                    mm_group(ps[:], Bp, [(wres[wname][:, kc, oc * 128:(oc + 1) * 128], src[:, kc, :]) for kc in range(8)],
                             [[Bwres[wname][oc], Bsrc[kc]] for kc in range(8)])
                    evac(oc, ps, Bp)

            def evac_hd(oc, ps, Bp):
                nonlocal ek
                ek += 1
                if ek % 2 == 0:
                    c.op("dve", lambda e: e.tensor_copy(out=hd[:, oc, :], in_=ps[:]), reads=[Bp], writes=[Bhd[oc]])
                else:
                    c.op("act", lambda e: e.activation(out=hd[:, oc, :], in_=ps[:], func=AF.Copy), reads=[Bp], writes=[Bhd[oc]])

            proj("w_out", mx, Bmx, evac_hd)
            postnorm_residual(c, sb_, hd, Bhd, xt[b], Bxt[b], prm["post_mix"], cst, "mix")
            for kc in range(8):
                c.op("act", lambda e: e.activation(out=sq[:, kc, :], in_=xt[b][:, kc, :], func=AF.Square), reads=[Bxt[b][kc]], writes=[Bsq[kc]])
            mm_group(psn[:], Bpsn, [(cst["avgD"][:], sq[:, kc, :]) for kc in range(8)], [[Bsq[kc]] for kc in range(8)])
            c.op("act", lambda e: e.activation(out=rs[:], in_=psn[:], func=AF.Sqrt, bias=cst["epsc"][:, 0:1]), reads=[Bpsn], writes=[Brs])
            c.op("dve", lambda e: e.reciprocal(out=rs[:], in_=rs[:]), reads=[Brs], writes=[Brs])
            for kc in range(8):
                en = "dve" if kc % 2 == 0 else "pool"
                c.op(en, lambda e: e.tensor_tensor(out=mx[:, kc, :], in0=xt[b][:, kc, :], in1=rs[:], op=ALU.mult), reads=[Bxt[b][kc], Brs], writes=[Bmx[kc]])

            def evac_q(oc, ps, Bp):
                nonlocal ek
                ek += 1
                if ek % 2 == 0:
                    c.op("dve", lambda e: e.tensor_scalar(out=qT[:, oc, :], in0=ps[:], scalar1=1.0 / 16, scalar2=None, op0=ALU.mult), reads=[Bp], writes=[BqT[oc]])
                else:
                    c.op("act", lambda e: e.activation(out=qT[:, oc, :], in_=ps[:], func=AF.Copy, scale=1.0 / 16), reads=[Bp], writes=[BqT[oc]])

            proj("wq", mx, Bmx, evac_q)
            for xh in range(4):
                for mb in range(2):
                    ps, Bp = nextps()
                    mm_group(ps[:], Bp, [(KT[:, 2 * xh + dc, mb * 128:(mb + 1) * 128], qT[:, 2 * xh + dc, :]) for dc in range(2)],
                             [[BKT[2 * xh + dc], BqT[2 * xh + dc]] for dc in range(2)])
                    c.op("act", lambda e: e.activation(out=P[mb][:], in_=ps[:], func=AF.Exp), reads=[Bp], writes=[BP[mb]])
                ps, Bp = nextps()
                mm_group(ps[:], Bp, [(cst["ones"][:], P[mb][:]) for mb in range(2)], [[BP[mb]] for mb in range(2)])
                c.op("dve", lambda e: e.reciprocal(out=rden[:], in_=ps[:]), reads=[Bp], writes=[Brden])
                for dc in range(2):
                    oc = 2 * xh + dc
                    ps, Bp = nextps()
                    mm_group(ps[:], Bp, [(V[:, mb, oc * 128:(oc + 1) * 128], P[mb][:]) for mb in range(2)], [[BV[oc], BP[mb]] for mb in range(2)])
                    c.op("dve", lambda e: e.tensor_tensor(out=on[:, oc, :], in0=ps[:], in1=rden[:], op=ALU.mult), reads=[Bp, Brden], writes=[Bon[oc]])
            proj("wo", on, Bon, evac_hd)
            postnorm_residual(c, sb_, hd, Bhd, xt[b], Bxt[b], prm["post_mem"], cst, "mem")
            c.dma("sp", out=xv[:, :, sl], in_=xt[b][:], reads=Bxt[b])
    c.barrier()


NPAR = 272


def build_program(nl=NL, debug=False):
    nc = bass.Bass("TRN2", target_bir_lowering=False)
    di = lambda n, shp: nc.dram_tensor(n, list(shp), F32, kind="ExternalInput").ap()
    xin = di("xin", (D, S)); memT = di("memT", (D, NMEM))
    w_in = di("w_in", (NL, D, IN_COLS)); w_out = di("w_out", (NL, D, D))
    wq = di("wq_mem", (NL, D, D)); wk = di("wk_mem", (NL, D, D)); wv = di("wv_mem", (NL, D, D)); wo = di("wo_mem", (NL, D, D))
    w_up = di("w_up", (NL, D, 2 * DFF)); w_down = di("w_down", (NL, DFF, D))
    par = di("par", (128, NL, NPAR)); gb = di("gb", (4, NL, 2))
    cin = di("cin", (128, 3, 128)); tabd = di("tab", (128, 24, 256)); seld = di("sel65", (65, 64))
    ca = mlstm_const_arrays()
    cd = {k: di(k, v.shape) for k, v in ca.items()}
    yT = nc.dram_tensor("yT", [D, S], F32, kind="ExternalOutput").ap()
    sk = "ExternalOutput" if debug else "Internal"
    projT = nc.dram_tensor("projT", [3584, S], BF16, kind=sk).ap()
    gatesT = nc.dram_tensor("gatesT", [8, S], F32, kind=sk).ap()
    mixedT = nc.dram_tensor("mixedT", [D, S], BF16, kind=sk).ap()
    hffT = nc.dram_tensor("hffT", [DFF, S], BF16, kind=sk).ap()
    with ExitStack() as es:
        c = Ctx(nc, es)
        cst = mlstm_consts(c, es, cd)
        B = Buf()
        cc = c.sb(es, (128, 3, 128), BF16, "cc")
        for i in range(3):
            c.dma("pool", out=cc[:, i, :], in_=cin[:, i, :], writes=[B])
        cst["avgD"] = cc[:, 0, :]; cst["avgH"] = cc[:, 1, :]; cst["ones"] = cc[:, 2, :]
        cst["epsc"] = c.sb(es, (128, 1), F32, "epsc"); c.op("dve", lambda e: e.memset(cst["epsc"][:], EPS), writes=[B])
        cst["sel65"] = c.sb(es, (65, 64), F32, "sel65"); c.dma("sp", out=cst["sel65"][:], in_=seld, writes=[B])
        cst["tab"] = c.sb(es, (128, 24, 256), BF16, "tab")
        with ExitStack() as s0:
            tf = c.sb(s0, (128, 24, 256), F32, "tabf"); Bt = Buf()
            c.dma("sp", out=tf[:], in_=tabd, writes=[Bt])
            c.op("pool", lambda e: e.tensor_copy(out=cst["tab"][:], in_=tf[:]), reads=[Bt], writes=[B])
            c.barrier()
        pt = c.sb(es, (128, NL, NPAR), F32, "par"); c.dma("sp", out=pt[:], in_=par, writes=[B])
        gbt = c.sb(es, (4, NL, 2), F32, "gb"); c.dma("sp", out=gbt[:], in_=gb, writes=[B])
        c.dma("sp", out=yT, in_=xin, writes=[B])
        c.barrier()
        for l in range(nl):
            P = lambda a, b_: pt[:, l, a:b_]
            Bproj = bufs(29); Bmixed = bufs(12); Bhff = bufs(NFF)
            with ExitStack() as s1:
                hT = c.sb(s1, (128, 8, S), BF16, "hT")
                BhT = [bufs(8) for _ in range(NTT)]
                norm_pass(c, yT, hT, BhT, cst)
                inproj(c, w_in[l], P(0, 8), hT, BhT, projT, gatesT, Bproj)
            attention(c, projT, mixedT, cst, Bproj, Bmixed)
            W = {"w_out": w_out[l], "wq": wq[l], "wk": wk[l], "wv": wv[l], "wo": wo[l]}
            prm = {"gmix": P(8, 16), "post_mix": P(16, 24), "pre_mem": P(24, 32), "post_mem": P(32, 40)}
            with ExitStack() as s2:
                tw = token_weights_alloc(c, s2)
                mlstm(c, projT, gatesT, mixedT, Bproj, Bmixed, P(56, 96).rearrange("p (c f) -> p c f", f=5), gbt[:, l, :], cst,
                      hook=lambda: token_weights_issue(c, tw, W, prm))
                token_phase(c, l, W, prm, mixedT, Bmixed, yT, memT, cst, tw=tw)
            with ExitStack() as s1:
                hT = c.sb(s1, (128, 8, S), BF16, "hT")
                BhT = [bufs(8) for _ in range(NTT)]
                norm_pass(c, yT, hT, BhT, cst)
                ffn_up(c, w_up[l], P(40, 48), P(96, 272).rearrange("p (c f) -> p c f", f=4), hT, BhT, hffT, Bhff)
            ffn_down(c, w_down[l], P(48, 56), hffT, Bhff, yT, cst)
        c.barrier()
    return nc


def host_inputs(inp):
    col = lambda v: np.asarray(v, np.float32).reshape(-1, 128).T
    par = np.zeros((128, NL, NPAR), np.float32)
    gb = np.zeros((4, NL, 2), np.float32)
    for l in range(NL):
        par[:, l, 0:8] = col(inp["pre_mix_g"][l])
        par[:, l, 8:16] = col(np.concatenate([inp["attn_out_g"][l], inp["mlstm_out_g"][l]]))
        par[:, l, 16:24] = col(inp["post_mix_g"][l]); par[:, l, 24:32] = col(inp["pre_mem_g"][l]); par[:, l, 32:40] = col(inp["post_mem_g"][l])
        par[:, l, 40:48] = col(inp["pre_ffn_g"][l]); par[:, l, 48:56] = col(inp["post_ffn_g"][l])
        mc = np.concatenate([inp["mconv_w"][l], inp["mconv_b"][l][None]], 0)
        par[:, l, 56:96] = mc.reshape(5, 8, 128).transpose(2, 1, 0).reshape(128, 40)
        fc = np.concatenate([inp["fconv_w"][l], inp["fconv_b"][l][None]], 0)
        par[:, l, 96:272] = fc.reshape(4, 44, 128).transpose(2, 1, 0).reshape(128, 176)
        gb[:, l, 0] = inp["b_igate"][l]; gb[:, l, 1] = inp["b_fgate"][l]
    sel = np.zeros((65, 64), np.float32); sel[64] = 1.0
    cin = np.stack([np.full((128, 128), 1 / 1024), np.full((128, 128), 1 / 512), np.ones((128, 128))], 1).astype(np.float32)
    shared = {"par": par, "gb": gb, "cin": cin, "tab": make_tables(np.asarray(inp["rel_bias"], np.float32)), "sel65": sel}
    shared.update(mlstm_const_arrays())
    for k in ("w_in", "w_out", "wq_mem", "wk_mem", "wv_mem", "wo_mem", "w_up", "w_down"):
        shared[k] = np.ascontiguousarray(inp[k], dtype=np.float32)
    maps = []
    for b in range(4):
        m = dict(shared)
        m["xin"] = np.ascontiguousarray(np.asarray(inp["x"][b], np.float32).T)
        m["memT"] = np.ascontiguousarray(np.asarray(inp["mem"][b], np.float32).T)
        maps.append(m)
    return maps


def kernel(**inputs):
    nc = build_program(NL)
    maps = host_inputs(inputs)
    res = run_bass_kernel_spmd(nc, maps, core_ids=[0, 1, 2, 3])
    out = np.stack([np.ascontiguousarray(np.asarray(r["yT"]).T) for r in res.results], 0)
    return out.astype(np.float32)
```

```python
import numpy as np
from contextlib import ExitStack
import concourse.bass as bass
import concourse.mybir as mybir
from concourse.bass_utils import run_bass_kernel_spmd
from concourse.alu_op_type import AluOpType as ALU

AF = mybir.ActivationFunctionType
AX = mybir.AxisListType
F32 = mybir.dt.float32
BF16 = mybir.dt.bfloat16

S = 4096
D = 1024
NL = 4
TT = 512
NTT = S // TT
EPS = 1e-6
IN_COLS = 3592
DFF = 2816
NFF = DFF // 128
NMEM = 256


class Buf:
    __slots__ = ("w", "r")

    def __init__(self):
        self.w = None
        self.r = {}


def bufs(n):
    return [Buf() for _ in range(n)]


class Ctx:
    def __init__(self, nc, es, n_dma_sems=40):
        self.nc = nc
        self.es = es
        self.eng = dict(pe=nc.tensor, dve=nc.vector, act=nc.scalar, pool=nc.gpsimd, sp=nc.sync)
        self.esem = {}
        self.cnt = {}
        self.nsem = 0
        for e in self.eng:
            self._new_esem(e)
        self.waited = {}
        self.dsems = [es.enter_context(nc.semaphore(f"dq{i}")) for i in range(n_dma_sems)]
        self.dval = [0] * n_dma_sems
        self.dnext = 0
        self.n_hw = n_dma_sems - 6
        self.dnext_sw = 0
        self.recent_dma = {}
        self.uid = 0
        self.ninstr = 0

    def _new_esem(self, e):
        self.nsem += 1
        self.esem[e] = self.es.enter_context(self.nc.semaphore(f"e{e}{self.nsem}"))
        self.cnt[e] = 0

    def name(self, p):
        self.uid += 1
        return f"{p}_{self.uid}"

    def sb(self, st, shape, dtype, name="t"):
        return st.enter_context(self.nc.sbuf_tensor(self.name(name), list(shape), dtype))

    def ps(self, st, shape=(128, 512), dtype=F32, name="ps"):
        return st.enter_context(self.nc.psum_tensor(self.name(name), list(shape), dtype))

    def _wait(self, e, tok, raw=False):
        sem, val, owner = tok
        if owner == e and (e == "pe" or e == "sp"):
            return
        key = (e, id(sem))
        if self.waited.get(key, 0) >= val:
            return
        self.waited[key] = val
        self.eng[e].wait_ge(sem, val)
        self.ninstr += 1

    def _deps(self, e, reads, writes):
        for b in reads:
            if b.w is not None:
                self._wait(e, b.w, raw=True)
        for b in writes:
            if b.w is not None:
                self._wait(e, b.w)
            for t in b.r.values():
                self._wait(e, t)

    def _mark(self, tok, reads, writes):
        for b in reads:
            k = id(tok[0])
            o = b.r.get(k)
            if o is None or o[1] < tok[1]:
                b.r[k] = tok
        for b in writes:
            b.w = tok
            b.r = {}

    def op(self, e, fn, reads=(), writes=(), inc=True):
        self._deps(e, reads, writes)
        ins = fn(self.eng[e])
        self.ninstr += 1
        if inc:
            if self.cnt[e] >= 30000:
                self._new_esem(e)
            self.cnt[e] += 1
            ins.then_inc(self.esem[e], 1)
            tok = (self.esem[e], self.cnt[e], e)
        else:
            if self.cnt[e] >= 30000:
                self._new_esem(e)
            tok = (self.esem[e], self.cnt[e] + 1, e)
        self._mark(tok, reads, writes)
        return tok

    def dma(self, q, out, in_, reads=(), writes=()):
        if q == "pool":
            i = self.n_hw + self.dnext_sw
            self.dnext_sw = (self.dnext_sw + 1) % (len(self.dsems) - self.n_hw)
        else:
            i = self.dnext
            self.dnext = (i + 1) % self.n_hw
        sem = self.dsems[i]
        old = self.dval[i]
        self._deps(q, reads, writes)
        if old > 0:
            self._wait(q, (sem, old, None))
        ins = self.eng[q].dma_start(out=out, in_=in_)
        self.ninstr += 1
        self.dval[i] = old + 16
        ins.then_inc(sem, 16)
        tok = (sem, old + 16, "dma")
        self.recent_dma[id(sem)] = tok
        self._mark(tok, reads, writes)
        return tok

    def barrier(self, engines=("pe", "dve", "act", "pool", "sp")):
        toks = [(self.esem[e], self.cnt[e], e) for e in self.eng if self.cnt[e] > 0]
        toks += list(self.recent_dma.values())
        for e in engines:
            for t in toks:
                self._wait(e, t, raw=True)
        self.recent_dma = {}


import math


def norm_pass(c, xT, hT, BhT, cst):
    nc = c.nc
    xv = xT.rearrange("(kc p) t -> p kc t", p=128)
    with ExitStack() as st:
        xt = [c.sb(st, (128, 8, TT), F32, "xt") for _ in range(2)]
        sq = [c.sb(st, (128, 8, TT), BF16, "sq") for _ in range(2)]
        rs = [c.sb(st, (128, TT), F32, "rs") for _ in range(2)]
        pss = [c.ps(st) for _ in range(2)]
        Bxt = bufs(2); Bsq = [bufs(8) for _ in range(2)]; Brs = bufs(2); Bps = bufs(2)
        for tt in range(NTT):
            b = tt % 2
            sl = slice(tt * TT, (tt + 1) * TT)
            c.dma("sp", out=xt[b][:], in_=xv[:, :, sl], writes=[Bxt[b]])
            for kc in range(8):
                c.op("act", lambda e: e.activation(out=sq[b][:, kc, :], in_=xt[b][:, kc, :], func=AF.Square),
                     reads=[Bxt[b]], writes=[Bsq[b][kc]])
            for kc in range(8):
                c.op("pe", lambda e: e.matmul(pss[b][:], lhsT=cst["avgD"][:], rhs=sq[b][:, kc, :], start=(kc == 0), stop=(kc == 7)),
                     reads=[Bsq[b][kc]], writes=[Bps[b]] if kc == 0 else [], inc=(kc == 7))
            Bps[b].w = (c.esem["pe"], c.cnt["pe"], "pe")
            c.op("act", lambda e: e.activation(out=rs[b][:], in_=pss[b][:], func=AF.Sqrt, bias=cst["epsc"][:, 0:1]),
                 reads=[Bps[b]], writes=[Brs[b]])
            c.op("dve", lambda e: e.reciprocal(out=rs[b][:], in_=rs[b][:]),
                 reads=[Brs[b]], writes=[Brs[b]])
            for kc in range(8):
                en = "dve" if kc % 2 == 0 else "pool"
                c.op(en, lambda e: e.tensor_tensor(out=hT[:, kc, sl], in0=xt[b][:, kc, :], in1=rs[b][:], op=ALU.mult),
                     reads=[Bxt[b], Brs[b]], writes=[BhT[tt][kc]])
    c.barrier()


def inproj(c, w_in, gcol, hT, BhT, projT, gatesT, Bproj):
    wv = w_in.rearrange("(kc p) n -> p kc n", p=128)
    with ExitStack() as st:
        ws = [c.sb(st, (128, 8, 128), F32, "ws") for _ in range(2)]
        wb = [c.sb(st, (128, 8, 128), BF16, "wb") for _ in range(2)]
        ob = [c.sb(st, (128, S), BF16, "ob") for _ in range(2)]
        og = c.sb(st, (8, S), F32, "og")
        pss = [c.ps(st) for _ in range(4)]
        Bws = bufs(2); Bwb = [bufs(8) for _ in range(2)]; Bob = [bufs(NTT) for _ in range(2)]; Bps = bufs(4)
        Bog = bufs(NTT)
        k = 0
        for fc in range(29):
            ncol = 128 if fc < 28 else 8
            b = fc % 2
            c.dma("sp", out=ws[b][:, :, :ncol], in_=wv[:, :, fc * 128: fc * 128 + ncol], writes=[Bws[b]])
            c.op("pool", lambda e: e.tensor_tensor(out=wb[b][:, :, :ncol], in0=ws[b][:, :, :ncol], in1=gcol.unsqueeze(2).to_broadcast([128, 8, ncol]), op=ALU.mult),
                 reads=[Bws[b]], writes=Bwb[b])
            for tt in range(NTT):
                sl = slice(tt * TT, (tt + 1) * TT)
                p = k % 4
                for kc in range(8):
                    c.op("pe", lambda e: e.matmul(pss[p][:ncol, :], lhsT=wb[b][:, kc, :ncol], rhs=hT[:, kc, sl], start=(kc == 0), stop=(kc == 7)),
                         reads=[Bwb[b][kc], BhT[tt][kc]], writes=[Bps[p]] if kc == 0 else [], inc=(kc == 7))
                Bps[p].w = (c.esem["pe"], c.cnt["pe"], "pe")
                if fc == 28:
                    c.op("dve", lambda e: e.tensor_copy(out=og[:, sl], in_=pss[p][:8, :]), reads=[Bps[p]], writes=[Bog[tt]])
                elif fc < 4:
                    if k % 2 == 0:
                        c.op("act", lambda e: e.activation(out=ob[b][:, sl], in_=pss[p][:], func=AF.Copy, scale=0.125), reads=[Bps[p]], writes=[Bob[b][tt]])
                    else:
                        c.op("dve", lambda e: e.tensor_scalar(out=ob[b][:, sl], in0=pss[p][:], scalar1=0.125, scalar2=None, op0=ALU.mult), reads=[Bps[p]], writes=[Bob[b][tt]])
                else:
                    if k % 2 == 0:
                        c.op("act", lambda e: e.activation(out=ob[b][:, sl], in_=pss[p][:], func=AF.Copy), reads=[Bps[p]], writes=[Bob[b][tt]])
                    else:
                        c.op("dve", lambda e: e.tensor_copy(out=ob[b][:, sl], in_=pss[p][:]), reads=[Bps[p]], writes=[Bob[b][tt]])
                k += 1
            if fc == 28:
                c.dma("sp", out=gatesT[:, :], in_=og[:], reads=Bog, writes=[Bproj[28]])
            else:
                c.dma("sp", out=projT[fc * 128:(fc + 1) * 128, :], in_=ob[b][:], reads=Bob[b], writes=[Bproj[fc]])
    c.barrier()


def load_consts(c, st, cdram):
    cst = {}
    B = Buf()
    def ld(name, shape, dtype):
        t = c.sb(st, shape, dtype, name)
        c.dma("pool" if dtype == BF16 else "sp", out=t[:], in_=cdram[name], writes=[B])
        cst[name] = t
    ld("avgD", (128, 128), BF16)
    t = c.sb(st, (128, 1), F32, "epsc")
    c.op("dve", lambda e: e.memset(t[:], EPS), writes=[B])
    cst["epsc"] = t
    return cst, B


DILS = (1, 4, 16)

def attention(c, projT, mixedT, cst, Bproj, Bmixed):
    with ExitStack() as st:
        qkv = [c.sb(st, (128, S), BF16, "qkv") for _ in range(3)]
        perm = [[c.sb(st, (128, S), BF16, "perm") for _ in range(3)] for _ in range(2)]
        vtok = [c.sb(st, (128, 32, 2, 65), BF16, "vtok") for _ in range(3)]
        acc = [c.sb(st, (65, S), F32, "acc") for _ in range(2)]
        pt = [c.sb(st, (128, 2, 256), BF16, "pt") for _ in range(3)]
        rec = [c.sb(st, (64, TT), F32, "rec") for _ in range(2)]
        ao = [c.sb(st, (64, S), BF16, "ao") for _ in range(2)]
        sp = [c.ps(st, (128, 2, 256), F32, "sp") for _ in range(2)]
        tps = [c.ps(st, (128, 8, 128), BF16, "tp") for _ in range(2)]
        ops = [c.ps(st, (128, 4, 128), F32, "ops") for _ in range(3)]
        dps = ops[2:3]
        Bqkv = bufs(3); Bperm = [bufs(3) for _ in range(2)]; Bvt = [bufs(8) for _ in range(3)]
        Bacc = bufs(2); Bpt = bufs(3); Brec = bufs(2); Bao = [bufs(NTT) for _ in range(2)]
        Bsp = bufs(2); Btp = bufs(2); Bo = bufs(3); Bdps = Bo[2:3]
        ident = cst["ident"]; tab = cst["tab"]; sel = cst["sel65"]
        for pi in range(3):
            c.op("pool", lambda e: e.memset(vtok[pi][:, :, :, 64:65], 1.0), writes=Bvt[pi])
        kq = 0
        for hp in range(4):
            for i in range(3):
                c.dma("sp", out=qkv[i][:], in_=projT[i * 512 + hp * 128: i * 512 + hp * 128 + 128, :], reads=[Bproj[i * 4 + hp]], writes=[Bqkv[i]])
            for pi, dil in enumerate(DILS):
                nbc = 32 // dil
                if dil == 1:
                    src = qkv; Bsrc = Bqkv
                else:
                    src = perm[pi - 1]; Bsrc = Bperm[pi - 1]
                    for i in range(3):
                        c.op("pool", lambda e: e.tensor_copy(out=src[i][:].rearrange("p (c i) -> p c i", c=dil),
                                                             in_=qkv[i][:].rearrange("p (i c) -> p c i", c=dil)),
                             reads=[Bqkv[i]], writes=[Bsrc[i]])
                qP, kP, vP = src
                for g in range(8):
                    tb = g % 2
                    for j in range(4):
                        blk = g * 4 + j
                        c.op("pe", lambda e: e.transpose(out=tps[tb][:, j, :], in_=vP[:, blk * 128:(blk + 1) * 128], identity=ident[:]),
                             reads=[Bsrc[2]], writes=[Btp[tb]] if j == 0 else [], inc=(j == 3))
                    Btp[tb].w = (c.esem["pe"], c.cnt["pe"], "pe")
                    en = "dve" if g % 2 == 0 else "act"
                    o_ap = vtok[pi][:, g * 4:(g + 1) * 4, :, 0:64]
                    i_ap = tps[tb][:, 0:4, :].rearrange("p j (h d) -> p j h d", h=2)
                    if en == "dve":
                        c.op("dve", lambda e: e.tensor_copy(out=o_ap, in_=i_ap), reads=[Btp[tb]], writes=[Bvt[pi][g]])
                    else:
                        c.op("act", lambda e: e.activation(out=o_ap, in_=i_ap, func=AF.Copy), reads=[Btp[tb]], writes=[Bvt[pi][g]])
                for h in range(2):
                    hd = hp * 2 + h
                    rows = slice(h * 64, h * 64 + 64)
                    accv = acc[h][:, :].rearrange("p (i c) -> p c i", c=dil)
                    for cl in range(dil):
                        for n2 in range(0, nbc, 2):
                            s_ = kq % 2; p_ = kq % 3; kq += 1
                            for j in range(2):
                                n = n2 + j; gb = cl * nbc + n
                                nq = 256 if n < nbc - 1 else 128
                                c.op("pe", lambda e: e.matmul(sp[s_][:, j, :nq], lhsT=kP[rows, gb * 128:(gb + 1) * 128], rhs=qP[rows, gb * 128: gb * 128 + nq],
                                                              start=True, stop=False),
                                     reads=[Bsrc[0], Bsrc[1]], writes=[Bsp[s_]] if j == 0 else [], inc=False)
                                c.op("pe", lambda e: e.matmul(sp[s_][:, j, :nq], lhsT=ident[:], rhs=tab[:, hd * 3 + pi, :nq], start=False, stop=True),
                                     inc=(j == 1))
                            Bsp[s_].w = (c.esem["pe"], c.cnt["pe"], "pe")
                            last = (n2 + 1 == nbc - 1)
                            if not last:
                                c.op("act", lambda e: e.activation(out=pt[p_][:], in_=sp[s_][:], func=AF.Exp), reads=[Bsp[s_]], writes=[Bpt[p_]])
                            else:
                                c.op("act", lambda e: e.activation(out=pt[p_][:, 0, :], in_=sp[s_][:, 0, :], func=AF.Exp), reads=[Bsp[s_]], writes=[Bpt[p_]])
                                c.op("act", lambda e: e.activation(out=pt[p_][:, 1, :128], in_=sp[s_][:, 1, :128], func=AF.Exp), reads=[Bsp[s_]], writes=[])
                                Bpt[p_].w = (c.esem["act"], c.cnt["act"], "act")
                            for j in range(2):
                                n = n2 + j; gb = cl * nbc + n
                                oi = gb % 3
                                ot = ops[oi][:65, 0, :]
                                c.op("pe", lambda e: e.matmul(ot, lhsT=vtok[pi][:, gb, h, :], rhs=pt[p_][:, j, 0:128], start=(n == 0), stop=True),
                                     reads=[Bpt[p_], Bvt[pi][gb // 4]], writes=[Bo[oi]] if n == 0 else [], inc=True)
                                Bo[oi].w = (c.esem["pe"], c.cnt["pe"], "pe")
                                av = accv[:, cl, n * 128:(n + 1) * 128]
                                if pi == 0:
                                    c.op("dve", lambda e: e.tensor_copy(out=av, in_=ot), reads=[Bo[oi]], writes=[Bacc[h]])
                                else:
                                    c.op("dve", lambda e: e.tensor_tensor(out=av, in0=av, in1=ot, op=ALU.add), reads=[Bo[oi], Bacc[h]], writes=[Bacc[h]])
                                if n < nbc - 1:
                                    oi2 = (gb + 1) % 3
                                    ot2 = ops[oi2][:65, 0, :]
                                    c.op("pe", lambda e: e.matmul(ot2, lhsT=vtok[pi][:, gb, h, :], rhs=pt[p_][:, j, 128:256], start=True, stop=False),
                                         reads=[Bpt[p_], Bvt[pi][gb // 4]], writes=[Bo[oi2]], inc=False)
            for h in range(2):
                hd = hp * 2 + h
                for tt in range(NTT):
                    sl = slice(tt * TT, (tt + 1) * TT)
                    r_ = tt % 2
                    c.op("pe", lambda e: e.matmul(dps[0][:64, :, :], lhsT=sel[:65, :], rhs=acc[h][:65, sl], start=True, stop=True),
                         reads=[Bacc[h]], writes=[Bdps[0]])
                    c.op("dve", lambda e: e.reciprocal(out=rec[r_][:], in_=dps[0][:64, :, :].rearrange("p a b -> p (a b)")), reads=[Bdps[0]], writes=[Brec[r_]])
                    c.op("pool", lambda e: e.tensor_tensor(out=ao[h][:, sl], in0=acc[h][:64, sl], in1=rec[r_][:], op=ALU.mult),
                         reads=[Bacc[h], Brec[r_]], writes=[Bao[h][tt]])
                c.dma("sp", out=mixedT[hd * 64:(hd + 1) * 64, :], in_=ao[h][:], reads=Bao[h], writes=[Bmixed[hd]])
    c.barrier()


def t5_bucket(dist):
    dist = np.asarray(dist)
    max_exact = 16
    d_f = np.maximum(dist, 1).astype(np.float32)
    large = max_exact + (np.log(d_f / max_exact) / np.log(2048 / max_exact) * (32 - max_exact)).astype(np.int32)
    large = np.minimum(large, 31)
    return np.where(dist < max_exact, dist, large)


def make_tables(rel_bias):
    s = np.arange(128)[:, None]
    t = np.arange(128)[None, :]
    tabs = np.full((128, 24, 256), -30000.0, np.float32)
    for pi, dil in enumerate(DILS):
        bsub = rel_bias[t5_bucket(np.arange(129) * dil)]
        d0 = t - s
        d1 = t + 128 - s
        for h in range(8):
            tabs[:, h * 3 + pi, 0:128] = np.where(d0 >= 0, bsub[np.clip(d0, 0, 128), h], -30000.0)
            tabs[:, h * 3 + pi, 128:256] = np.where(d1 <= 128, bsub[np.clip(d1, 0, 128), h], -30000.0)
    return tabs


def mlstm(c, projT, gatesT, mixedT, Bproj, Bmixed, mcp, gbias, cst, hook=None):
    ident = cst["ident"]; identf = cst["identf"]; cm = cst["cmask"]
    with ExitStack() as st:
        et = c.sb(st, (128, 32, 8), F32, "et"); Bet = Buf()
        decb = c.sb(st, (128, 128), F32, "decb"); Bdecb = Buf()
        with ExitStack() as s0:
            gi = c.sb(s0, (4, S), F32, "gi"); gf = c.sb(s0, (4, S), F32, "gf"); nb = c.sb(s0, (4, S), F32, "nb")
            dmb = c.sb(s0, (4, S), F32, "dmb"); t1 = c.sb(s0, (4, S), F32, "t1"); t2 = c.sb(s0, (4, S), F32, "t2")
            sm = c.sb(s0, (4, 8, 32), F32, "sm")
            decbd = c.sb(s0, (4, 4, 32), F32, "decbd"); nbf = c.sb(s0, (4, 2), F32, "nbf")
            pg = c.ps(s0, (128, 32, 8), F32, "pg"); pd = c.ps(s0, (128, 128), F32, "pd")
            G = Buf()
            c.dma("sp", out=gi[:], in_=gatesT[0:4, :], reads=[Bproj[28]], writes=[G])
            c.dma("sp", out=gf[:], in_=gatesT[4:8, :], reads=[Bproj[28]], writes=[G])
            def g(en, fn):
                c.op(en, fn, reads=[G], writes=[G])
            g("dve", lambda e: e.tensor_scalar(out=nbf[:, 0:1], in0=gbias[:, 1:2], scalar1=-1.0, scalar2=None, op0=ALU.mult))
            g("dve", lambda e: e.memset(nbf[:, 1:2], -0.5 * math.log(128.0)))
            g("act", lambda e: e.activation(out=t1[:], in_=gf[:], func=AF.Exp, scale=-1.0, bias=nbf[:, 0:1]))
            g("act", lambda e: e.activation(out=t1[:], in_=t1[:], func=AF.Ln, bias=cst["one1"][:4, 0:1]))
            g("dve", lambda e: e.tensor_tensor_scan(out=nb[:], data0=cst["rmask"][:4, :], data1=t1[:], initial=0.0, op0=ALU.mult, op1=ALU.add))
            g("dve", lambda e: e.scalar_tensor_tensor(out=dmb[:], in0=gi[:], scalar=gbias[:, 0:1], in1=nb[:], op0=ALU.add, op1=ALU.add))
            g("dve", lambda e: e.tensor_reduce(out=sm[:, 0, :], in_=dmb[:].rearrange("p (c s) -> p c s", s=128), axis=AX.X, op=ALU.max))
            g("dve", lambda e: e.tensor_scalar(out=sm[:, 1, :], in0=nb[:].rearrange("p (c s) -> p c s", s=128)[:, :, 127], scalar1=-1.0, scalar2=None, op0=ALU.mult))
            g("dve", lambda e: e.tensor_tensor_scan(out=sm[:, 2, :], data0=sm[:, 0, :], data1=sm[:, 1, :], initial=0.0, op0=ALU.max, op1=ALU.add))
            g("dve", lambda e: e.memset(sm[:, 3, 0:1], 0.0))
            g("dve", lambda e: e.tensor_copy(out=sm[:, 3, 1:32], in_=sm[:, 2, 0:31]))
            g("dve", lambda e: e.tensor_tensor(out=sm[:, 4, :], in0=sm[:, 3, :], in1=sm[:, 0, :], op=ALU.max))
            g("dve", lambda e: e.tensor_tensor(out=sm[:, 5, :], in0=sm[:, 3, :], in1=sm[:, 4, :], op=ALU.subtract))
            g("act", lambda e: e.activation(out=sm[:, 5, :], in_=sm[:, 5, :], func=AF.Exp))
            Mb = sm[:, 4, :].unsqueeze(2).to_broadcast([4, 32, 128])
            g("dve", lambda e: e.tensor_tensor(out=t1[:].rearrange("p (c s) -> p c s", s=128), in0=dmb[:].rearrange("p (c s) -> p c s", s=128), in1=Mb, op=ALU.subtract))
            g("act", lambda e: e.activation(out=t1[:], in_=t1[:], func=AF.Exp, bias=nbf[:, 1:2]))
            g("dve", lambda e: e.tensor_tensor(out=t2[:].rearrange("p (c s) -> p c s", s=128), in0=nb[:].rearrange("p (c s) -> p c s", s=128), in1=Mb, op=ALU.subtract))
            g("act", lambda e: e.activation(out=t2[:], in_=t2[:], func=AF.Exp))
            for cc in range(32):
                cs = slice(cc * 128, (cc + 1) * 128)
                c.op("pe", lambda e: e.transpose(out=pg[:, cc, 0:4], in_=t1[:, cs], identity=identf[:4, :4]), reads=[G], writes=[G] if cc == 0 else [], inc=False)
                c.op("pe", lambda e: e.transpose(out=pg[:, cc, 4:8], in_=t2[:, cs], identity=identf[:4, :4]), reads=[G], inc=(cc == 31))
            G.w = (c.esem["pe"], c.cnt["pe"], "pe")
            c.op("dve", lambda e: e.tensor_copy(out=et[:], in_=pg[:]), reads=[G], writes=[Bet])
            g("dve", lambda e: e.tensor_tensor(out=decbd[:], in0=sm[:, 5, :].unsqueeze(1).to_broadcast([4, 4, 32]), in1=cst["bdmask"][:4, :].rearrange("p (a b) -> p a b", a=4), op=ALU.mult))
            c.op("pe", lambda e: e.matmul(pd[:], lhsT=cst["ones4"][:4, :], rhs=decbd[:].rearrange("p a b -> p (a b)"), start=True, stop=True), reads=[G], writes=[G])
            c.op("dve", lambda e: e.tensor_copy(out=decb[:], in_=pd[:]), reads=[G], writes=[Bdecb])
            c.barrier()
        raw = [c.sb(st, (128, 3 + S), BF16, "raw") for _ in range(2)]; Braw = bufs(2)
        cv = [c.sb(st, (128, S), F32, "cv") for _ in range(2)]; Bcv = bufs(2)
        qT = c.sb(st, (128, S), BF16, "qT"); kT = c.sb(st, (128, S), BF16, "kT"); BqT = Buf(); BkT = Buf()
        vT = c.sb(st, (128, S), BF16, "vT"); oT = c.sb(st, (128, S), BF16, "oT"); BvT = Buf(); BoT = Buf()
        hmT = c.sb(st, (128, S), BF16, "hmT"); BhmT = bufs(32)
        ktk = [c.sb(st, (128, 128), BF16, "ktk") for _ in range(2)]; Bktk = bufs(2)
        vaug = [c.sb(st, (128, 129), BF16, "vaug") for _ in range(2)]; Bvaug = bufs(2)
        og = [c.sb(st, (128, 128), F32, "og") for _ in range(2)]; Bog = bufs(2)
        pT = [c.sb(st, (128, 128), BF16, "pT") for _ in range(2)]; BpT = bufs(2)
        hmt = [c.sb(st, (128, 128), BF16, "hmt") for _ in range(2)]; Bhmt = bufs(2)
        C = c.sb(st, (128, 129), F32, "C"); BC = Buf()
        Cbf = [c.sb(st, (128, 129), BF16, "Cbf") for _ in range(2)]; BCbf = bufs(2)
        dd = [c.sb(st, (128, 2), F32, "dd") for _ in range(2)]; Bdd = bufs(2)
        tpk = [c.ps(st, (128, 8, 128), BF16, "tpk") for _ in range(2)]; Btpk = bufs(2)
        sps = c.ps(st, (128, 512), F32, "sps"); Bsps = Buf()
        nps = [c.ps(st, (128, 512), F32, "nps") for _ in range(2)]; Bnps = bufs(2)
        dps = c.ps(st, (128, 512), F32, "dps"); Bdps = Buf()
        tph = c.ps(st, (128, 8, 128), BF16, "tph"); Btph = Buf()
        for r in range(2):
            c.op("pool", lambda e: e.memset(raw[r][:, 0:3], 0.0), writes=[Braw[r]])
            c.op("pool", lambda e: e.memset(vaug[r][:, 128:129], 1.0), writes=[Bvaug[r]])
        for hd in range(4):
            c.dma("sp", out=raw[0][:, 3:], in_=projT[1536 + hd * 128:1536 + (hd + 1) * 128, :], reads=[Bproj[12 + hd]], writes=[Braw[0]])
            c.dma("sp", out=raw[1][:, 3:], in_=projT[2048 + hd * 128:2048 + (hd + 1) * 128, :], reads=[Bproj[16 + hd]], writes=[Braw[1]])
            c.dma("sp", out=vT[:], in_=projT[2560 + hd * 128:2560 + (hd + 1) * 128, :], reads=[Bproj[20 + hd]], writes=[BvT])
            c.dma("sp", out=oT[:], in_=projT[3072 + hd * 128:3072 + (hd + 1) * 128, :], reads=[Bproj[24 + hd]], writes=[BoT])
            if hd == 0 and hook is not None:
                hook()
            for i, en, dst, Bdst in ((0, "dve", qT, BqT), (1, "dve", kT, BkT)):
                ci = i * 4 + hd
                c.op(en, lambda e: e.tensor_scalar(out=cv[i][:], in0=raw[i][:, 3:3 + S], scalar1=mcp[:, ci, 3:4], scalar2=mcp[:, ci, 4:5], op0=ALU.mult, op1=ALU.add),
                     reads=[Braw[i]], writes=[Bcv[i]])
                for j in range(3):
                    c.op(en, lambda e: e.scalar_tensor_tensor(out=cv[i][:], in0=raw[i][:, j:j + S], scalar=mcp[:, ci, j:j + 1], in1=cv[i][:], op0=ALU.mult, op1=ALU.add),
                         reads=[Braw[i], Bcv[i]], writes=[Bcv[i]])
                c.op("act", lambda e: e.activation(out=dst[:], in_=cv[i][:], func=AF.Silu), reads=[Bcv[i]], writes=[Bdst])
            for cc in range(32):
                r = cc % 2
                cs = slice(cc * 128, (cc + 1) * 128)
                ecol = et[:, cc, hd:hd + 1]; fcol = et[:, cc, 4 + hd:5 + hd]
                for j, (src, Bsrc) in enumerate(((kT, BkT), (vT, BvT), (oT, BoT))):
                    c.op("pe", lambda e: e.transpose(out=tpk[r][:, j, :], in_=src[:, cs], identity=ident[:]), reads=[Bsrc], writes=[Btpk[r]] if j == 0 else [], inc=(j == 2))
                Btpk[r].w = (c.esem["pe"], c.cnt["pe"], "pe")
                c.op("dve", lambda e: e.tensor_scalar(out=ktk[r][:], in0=tpk[r][:, 0, :], scalar1=ecol, scalar2=None, op0=ALU.mult), reads=[Btpk[r], Bet], writes=[Bktk[r]])
                c.op("act", lambda e: e.activation(out=vaug[r][:, 0:128], in_=tpk[r][:, 1, :], func=AF.Copy), reads=[Btpk[r]], writes=[Bvaug[r]])
                c.op("act", lambda e: e.activation(out=og[r][:], in_=tpk[r][:, 2, :], func=AF.Sigmoid), reads=[Btpk[r]], writes=[Bog[r]])
                c.op("pe", lambda e: e.matmul(sps[:, 0:128], lhsT=kT[:, cs], rhs=qT[:, cs], start=True, stop=True), reads=[BkT, BqT], writes=[Bsps])
                c.op("dve", lambda e: e.scalar_tensor_tensor(out=pT[r][:], in0=sps[:, 0:128], scalar=ecol, in1=cm[:], op0=ALU.mult, op1=ALU.mult), reads=[Bsps, Bet], writes=[BpT[r]])
                if cc > 0:
                    dcol = decb[:, hd * 32 + cc: hd * 32 + cc + 1]
                    c.op("dve", lambda e: e.tensor_scalar(out=C[:], in0=C[:], scalar1=dcol, scalar2=None, op0=ALU.mult), reads=[BC, Bdecb], writes=[BC])
                    c.op("act", lambda e: e.activation(out=Cbf[r][:], in_=C[:], func=AF.Copy), reads=[BC], writes=[BCbf[r]])
                c.op("pe", lambda e: e.matmul(nps[r][:, 0:129], lhsT=pT[r][:], rhs=vaug[r][:], start=True, stop=(cc == 0)), reads=[BpT[r], Bvaug[r]], writes=[Bnps[r]], inc=(cc == 0))
                if cc > 0:
                    c.op("pe", lambda e: e.matmul(nps[r][:, 0:129], lhsT=qT[:, cs], rhs=Cbf[r][:], start=False, stop=True), reads=[BqT, BCbf[r]], inc=True)
                Bnps[r].w = (c.esem["pe"], c.cnt["pe"], "pe")
                c.op("pe", lambda e: e.matmul(dps[:, 0:129], lhsT=ktk[r][:], rhs=vaug[r][:], start=True, stop=True), reads=[Bktk[r], Bvaug[r]], writes=[Bdps])
                if cc == 0:
                    c.op("dve", lambda e: e.tensor_copy(out=C[:], in_=dps[:, 0:129]), reads=[Bdps], writes=[BC])
                else:
                    c.op("dve", lambda e: e.tensor_tensor(out=C[:], in0=C[:], in1=dps[:, 0:129], op=ALU.add), reads=[Bdps, BC], writes=[BC])
                c.op("dve", lambda e: e.tensor_scalar(out=dd[r][:, 1:2], in0=nps[r][:, 128:129], scalar1=-1.0, scalar2=None, op0=ALU.mult), reads=[Bnps[r]], writes=[Bdd[r]])
                c.op("dve", lambda e: e.scalar_tensor_tensor(out=dd[r][:, 0:1], in0=nps[r][:, 128:129], scalar=fcol, in1=dd[r][:, 1:2], op0=ALU.max, op1=ALU.max), reads=[Bnps[r], Bet, Bdd[r]], writes=[Bdd[r]])
                c.op("dve", lambda e: e.reciprocal(out=dd[r][:, 1:2], in_=dd[r][:, 0:1]), reads=[Bdd[r]], writes=[Bdd[r]])
                c.op("dve", lambda e: e.scalar_tensor_tensor(out=hmt[r][:], in0=nps[r][:, 0:128], scalar=dd[r][:, 1:2], in1=og[r][:], op0=ALU.mult, op1=ALU.mult),
                     reads=[Bnps[r], Bdd[r], Bog[r]], writes=[Bhmt[r]])
                c.op("pe", lambda e: e.transpose(out=tph[:, 0, :], in_=hmt[r][:], identity=ident[:]), reads=[Bhmt[r]], writes=[Btph])
                c.op("act", lambda e: e.activation(out=hmT[:, cs], in_=tph[:, 0, :], func=AF.Copy), reads=[Btph], writes=[BhmT[cc]])
            c.dma("sp", out=mixedT[512 + hd * 128: 512 + (hd + 1) * 128, :], in_=hmT[:], reads=BhmT, writes=[Bmixed[8 + hd]])
    c.barrier()


def mlstm_consts(c, st, cd):
    cst = {}; B = Buf()
    cst["identf"] = c.sb(st, (128, 128), F32, "identf"); c.dma("sp", out=cst["identf"][:], in_=cd["identf"], writes=[B])
    cst["ident"] = c.sb(st, (128, 128), BF16, "ident"); c.dma("pool", out=cst["ident"][:], in_=cd["identf"], writes=[B])
    cst["cmask"] = c.sb(st, (128, 128), F32, "cmask"); c.dma("sp", out=cst["cmask"][:], in_=cd["cmask"], writes=[B])
    cst["rmask"] = c.sb(st, (4, S), F32, "rmask"); c.dma("sp", out=cst["rmask"][:], in_=cd["rmask"], writes=[B])
    cst["bdmask"] = c.sb(st, (4, 128), F32, "bdmask"); c.dma("sp", out=cst["bdmask"][:], in_=cd["bdmask"], writes=[B])
    cst["ones4"] = c.sb(st, (4, 128), F32, "ones4"); c.dma("sp", out=cst["ones4"][:], in_=cd["ones4"], writes=[B])
    cst["one1"] = c.sb(st, (128, 1), F32, "one1"); c.op("dve", lambda e: e.memset(cst["one1"][:], 1.0), writes=[B])
    return cst


def mlstm_const_arrays():
    t = np.arange(128)
    cmask = (t[None, :] >= t[:, None]).astype(np.float32)
    rmask = np.ones((4, S), np.float32); rmask[:, ::128] = 0.0
    bd = np.zeros((4, 4, 32), np.float32)
    for h in range(4):
        bd[h, h] = 1.0
    return {"identf": np.eye(128, dtype=np.float32), "cmask": cmask, "rmask": rmask, "bdmask": bd.reshape(4, 128), "ones4": np.ones((4, 128), np.float32)}


def ffn_up(c, w_up, gcol, fcp, hT, BhT, hffT, Bhff):
    wv = w_up.rearrange("(kc p) n -> p kc n", p=128)
    with ExitStack() as st:
        ws = [c.sb(st, (128, 8, 128), F32, "ws") for _ in range(2)]
        wb = [c.sb(st, (128, 8, 128), BF16, "wb") for _ in range(2)]
        u = [[c.sb(st, (128, 2 + S), BF16, "u") for _ in range(2)] for _ in range(2)]
        cv = [c.sb(st, (128, S), F32, "cv") for _ in range(2)]
        gg = c.sb(st, (128, S), BF16, "gg")
        ho = [c.sb(st, (128, S), BF16, "ho") for _ in range(2)]
        pss = [c.ps(st) for _ in range(4)]
        Bws = bufs(2); Bwb = [bufs(8) for _ in range(2)]; Bu = [[bufs(NTT) for _ in range(2)] for _ in range(2)]
        Bcv = bufs(2); Bgg = Buf(); Bho = bufs(2); Bps = bufs(4)
        for par in range(2):
            for half in range(2):
                c.op("pool", lambda e: e.memset(u[par][half][:, 0:2], 0.0), writes=Bu[par][half])
        k = 0

        def ld_w(jj, half):
            ci_ = jj + NFF * half
            c.dma("sp", out=ws[half][:], in_=wv[:, :, ci_ * 128:(ci_ + 1) * 128], writes=[Bws[half]])

        ld_w(0, 0); ld_w(0, 1)
        for j in range(NFF):
            par = j % 2
            for half in range(2):
                ci = j + NFF * half
                b = half
                c.op("pool", lambda e: e.tensor_tensor(out=wb[b][:], in0=ws[b][:], in1=gcol.unsqueeze(2).to_broadcast([128, 8, 128]), op=ALU.mult),
                     reads=[Bws[b]], writes=Bwb[b])
                for tt in range(NTT):
                    sl = slice(tt * TT, (tt + 1) * TT)
                    p = k % 4
                    for kc in range(8):
                        c.op("pe", lambda e: e.matmul(pss[p][:], lhsT=wb[b][:, kc, :], rhs=hT[:, kc, sl], start=(kc == 0), stop=(kc == 7)),
                             reads=[Bwb[b][kc], BhT[tt][kc]], writes=[Bps[p]] if kc == 0 else [], inc=(kc == 7))
                    Bps[p].w = (c.esem["pe"], c.cnt["pe"], "pe")
                    o_ap = u[par][half][:, 2 + tt * TT: 2 + (tt + 1) * TT]
                    c.op("act", lambda e: e.activation(out=o_ap, in_=pss[p][:], func=AF.Copy), reads=[Bps[p]], writes=[Bu[par][half][tt]])
                    k += 1
                if j + 1 < NFF:
                    ld_w(j + 1, half)
            for half, en in ((0, "dve"), (1, "dve")):
                ci = j + NFF * half
                uu = u[par][half]
                c.op(en, lambda e: e.tensor_scalar(out=cv[half][:], in0=uu[:, 2:2 + S], scalar1=fcp[:, ci, 2:3], scalar2=fcp[:, ci, 3:4], op0=ALU.mult, op1=ALU.add),
                     reads=Bu[par][half], writes=[Bcv[half]])
                c.op(en, lambda e: e.scalar_tensor_tensor(out=cv[half][:], in0=uu[:, 1:1 + S], scalar=fcp[:, ci, 1:2], in1=cv[half][:], op0=ALU.mult, op1=ALU.add),
                     reads=Bu[par][half] + [Bcv[half]], writes=[Bcv[half]])
                c.op(en, lambda e: e.scalar_tensor_tensor(out=cv[half][:], in0=uu[:, 0:S], scalar=fcp[:, ci, 0:1], in1=cv[half][:], op0=ALU.mult, op1=ALU.add),
                     reads=Bu[par][half] + [Bcv[half]], writes=[Bcv[half]])
            c.op("act", lambda e: e.activation(out=gg[:], in_=cv[1][:], func=AF.Gelu_apprx_tanh), reads=[Bcv[1]], writes=[Bgg])
            c.op("dve", lambda e: e.tensor_tensor(out=ho[par][:], in0=gg[:], in1=cv[0][:], op=ALU.mult), reads=[Bgg, Bcv[0]], writes=[Bho[par]])
            c.dma("sp", out=hffT[j * 128:(j + 1) * 128, :], in_=ho[par][:], reads=[Bho[par]], writes=[Bhff[j]])
    c.barrier()


def postnorm_residual(c, st_bufs, hsrc, Bh, xt, Bx, gcol, cst, tag):
    sq, Bsq, ps, Bps, rs, Brs, tmp, Btmp = st_bufs
    for oc in range(8):
        c.op("act", lambda e: e.activation(out=sq[:, oc, :], in_=hsrc[:, oc, :], func=AF.Square), reads=[Bh[oc]], writes=[Bsq[oc]])
    for oc in range(8):
        c.op("pe", lambda e: e.matmul(ps[:], lhsT=cst["avgD"][:], rhs=sq[:, oc, :], start=(oc == 0), stop=(oc == 7)),
             reads=[Bsq[oc]], writes=[Bps] if oc == 0 else [], inc=(oc == 7))
    Bps.w = (c.esem["pe"], c.cnt["pe"], "pe")
    c.op("act", lambda e: e.activation(out=rs[:], in_=ps[:], func=AF.Sqrt, bias=cst["epsc"][:, 0:1]), reads=[Bps], writes=[Brs])
    c.op("dve", lambda e: e.reciprocal(out=rs[:], in_=rs[:]), reads=[Brs], writes=[Brs])
    for oc in range(8):
        en = "dve" if oc % 2 == 0 else "pool"
        c.op(en, lambda e: e.tensor_tensor(out=tmp[:, oc, :], in0=hsrc[:, oc, :], in1=rs[:], op=ALU.mult), reads=[Bh[oc], Brs], writes=[Btmp[oc]])
        c.op("dve", lambda e: e.scalar_tensor_tensor(out=xt[:, oc, :], in0=tmp[:, oc, :], scalar=gcol[:, oc:oc + 1], in1=xt[:, oc, :], op0=ALU.mult, op1=ALU.add),
             reads=[Btmp[oc], Bx[oc]], writes=[Bx[oc]])


def ffn_down(c, w_down, gpost, hffT, Bhff, xT, cst):
    wv = w_down.rearrange("(c p) n -> p c n", p=128)
    hv = hffT.rearrange("(c p) t -> p c t", p=128)
    xv = xT.rearrange("(kc p) t -> p kc t", p=128)
    with ExitStack() as st:
        wd = c.sb(st, (128, NFF, D), BF16, "wd")
        wst = [c.sb(st, (128, NFF, 128), F32, "wst") for _ in range(1)]
        hf = [c.sb(st, (128, NFF, TT), BF16, "hf") for _ in range(2)]
        xt = [c.sb(st, (128, 8, TT), F32, "xt") for _ in range(2)]
        hd = c.sb(st, (128, 8, TT), F32, "hd")
        sq = c.sb(st, (128, 8, TT), BF16, "sq"); rs = c.sb(st, (128, TT), F32, "rs")
        pss = [c.ps(st) for _ in range(3)]
        psn = c.ps(st)
        Bwd = bufs(8); Bwst = bufs(1); Bhf = bufs(2); Bxt = [bufs(8) for _ in range(2)]; Bhd = bufs(8)
        Bps = bufs(3)
        sb_ = (sq, bufs(8), psn, Buf(), rs, Buf(), hd, Bhd)
        for oc in range(8):
            b = 0
            c.dma("sp", out=wst[b][:], in_=wv[:, :, oc * 128:(oc + 1) * 128], writes=[Bwst[b]])
            c.op("pool", lambda e: e.tensor_copy(out=wd[:, :, oc * 128:(oc + 1) * 128], in_=wst[b][:]), reads=[Bwst[b]], writes=[Bwd[oc]])
        k = 0

        def ld_t(t_):
            b_ = t_ % 2
            sl_ = slice(t_ * TT, (t_ + 1) * TT)
            c.dma("sp", out=hf[b_][:], in_=hv[:, :, sl_], reads=Bhff, writes=[Bhf[b_]])
            c.dma("sp", out=xt[b_][:], in_=xv[:, :, sl_], writes=Bxt[b_])

        ld_t(0)
        for tt in range(NTT):
            b = tt % 2
            sl = slice(tt * TT, (tt + 1) * TT)
            for oc in range(8):
                p = k % 3; k += 1
                for fc in range(NFF):
                    c.op("pe", lambda e: e.matmul(pss[p][:], lhsT=wd[:, fc, oc * 128:(oc + 1) * 128], rhs=hf[b][:, fc, :], start=(fc == 0), stop=(fc == NFF - 1)),
                         reads=[Bwd[oc], Bhf[b]], writes=[Bps[p]] if fc == 0 else [], inc=(fc == NFF - 1))
                Bps[p].w = (c.esem["pe"], c.cnt["pe"], "pe")
                c.op("dve", lambda e: e.tensor_copy(out=hd[:, oc, :], in_=pss[p][:]), reads=[Bps[p]], writes=[Bhd[oc]])
            postnorm_residual(c, sb_, hd, Bhd, xt[b], Bxt[b], gpost, cst, "ffn")
            if tt + 1 < NTT:
                ld_t(tt + 1)
            c.dma("sp", out=xv[:, :, sl], in_=xt[b][:], reads=Bxt[b])
    c.barrier()


def token_weights_alloc(c, st):
    tw = {}
    tw["wres"] = {n: c.sb(st, (128, 8, D), BF16, n) for n in ("w_out", "wq", "wo")}
    tw["Bwres"] = {n: bufs(8) for n in tw["wres"]}
    tw["ws"] = [c.sb(st, (128, 8, 128), F32, "ws") for _ in range(2)]
    tw["Bws"] = bufs(2)
    tw["kw"] = 0
    return tw


def token_weights_issue(c, tw, W, prm):
    wres, Bwres, ws, Bws = tw["wres"], tw["Bwres"], tw["ws"], tw["Bws"]
    kw = tw["kw"]
    for name, g in (("w_out", prm["gmix"]), ("wq", prm["pre_mem"]), ("wo", None)):
        wv_ = W[name].rearrange("(kc p) n -> p kc n", p=128)
        for oc in range(8):
            b = kw % 2; kw += 1
            c.dma("sp", out=ws[b][:], in_=wv_[:, :, oc * 128:(oc + 1) * 128], writes=[Bws[b]])
            if g is None:
                c.op("pool", lambda e: e.tensor_copy(out=wres[name][:, :, oc * 128:(oc + 1) * 128], in_=ws[b][:]), reads=[Bws[b]], writes=[Bwres[name][oc]])
            else:
                c.op("pool", lambda e: e.tensor_tensor(out=wres[name][:, :, oc * 128:(oc + 1) * 128], in0=ws[b][:], in1=g.unsqueeze(2).to_broadcast([128, 8, 128]), op=ALU.mult),
                     reads=[Bws[b]], writes=[Bwres[name][oc]])
    tw["kw"] = kw


def token_phase(c, l, W, prm, mixedT, Bmixed, xT, memT, cst, tw=None):
    mv = mixedT.rearrange("(kc p) t -> p kc t", p=128)
    xv = xT.rearrange("(kc p) t -> p kc t", p=128)
    with ExitStack() as st:
        preloaded = tw is not None
        if tw is None:
            tw = token_weights_alloc(c, st)
        wres, Bwres, ws, Bws = tw["wres"], tw["Bwres"], tw["ws"], tw["Bws"]
        KT = c.sb(st, (128, 8, NMEM), BF16, "KT"); V = c.sb(st, (128, 2, D), BF16, "V"); mem = c.sb(st, (128, 8, NMEM), BF16, "mem")
        BKT = bufs(8); BV = bufs(8); Bmem = Buf()
        wtmp = [c.sb(st, (128, 8, 128), BF16, "wtmp") for _ in range(2)]; Bwtmp = bufs(2)
        mx = c.sb(st, (128, 8, TT), BF16, "mx"); Bmx = bufs(8)
        sq = c.sb(st, (128, 8, TT), BF16, "sq"); Bsq = bufs(8)
        hd = c.sb(st, (128, 8, TT), F32, "hd"); Bhd = bufs(8)
        xt = [c.sb(st, (128, 8, TT), F32, "xt") for _ in range(2)]; Bxt = [bufs(8) for _ in range(2)]
        qT = c.sb(st, (128, 8, TT), BF16, "qT"); BqT = bufs(8)
        on = c.sb(st, (128, 8, TT), BF16, "on"); Bon = bufs(8)
        P = [c.sb(st, (128, TT), BF16, "P") for _ in range(2)]; BP = bufs(2)
        rs = c.sb(st, (128, TT), F32, "rs"); Brs = Buf()
        rA = c.sb(st, (128, TT), F32, "rA"); BrA = Buf()
        rM = c.sb(st, (128, TT), F32, "rM"); BrM = Buf()
        rden = c.sb(st, (128, TT), F32, "rden"); Brden = Buf()
        NPS = 7
        pss = [c.ps(st) for _ in range(NPS)]; Bps = bufs(NPS)
        psn = c.ps(st); Bpsn = Buf()
        pk = [0]

        def nextps():
            i = pk[0] % NPS; pk[0] += 1
            return pss[i], Bps[i]

        def mm_group(ps, Bp, parts, reads_list):
            n = len(parts)
            for i, (lh, rh) in enumerate(parts):
                c.op("pe", lambda e: e.matmul(ps, lhsT=lh, rhs=rh, start=(i == 0), stop=(i == n - 1)),
                     reads=reads_list[i], writes=[Bp] if i == 0 else [], inc=(i == n - 1))
            Bp.w = (c.esem["pe"], c.cnt["pe"], "pe")

        with ExitStack() as s0:
            mf = c.sb(s0, (128, 8, NMEM), F32, "mf"); Bmf = Buf()
            c.dma("sp", out=mf[:], in_=memT.rearrange("(kc p) m -> p kc m", p=128), writes=[Bmf])
            c.op("pool", lambda e: e.tensor_copy(out=mem[:], in_=mf[:]), reads=[Bmf], writes=[Bmem])
            c.barrier()
        if not preloaded:
            token_weights_issue(c, tw, W, prm)
        kw = tw["kw"]
        for name in ("wk", "wv"):
            wv_ = W[name].rearrange("(kc p) n -> p kc n", p=128)
            for oc in range(8):
                b = kw % 2; kw += 1
                c.dma("sp", out=ws[b][:], in_=wv_[:, :, oc * 128:(oc + 1) * 128], writes=[Bws[b]])
                c.op("pool", lambda e: e.tensor_copy(out=wtmp[b][:], in_=ws[b][:]), reads=[Bws[b]], writes=[Bwtmp[b]])
                if name == "wk":
                    ps, Bp = nextps()
                    mm_group(ps[:, :NMEM], Bp, [(wtmp[b][:, kc, :], mem[:, kc, :]) for kc in range(8)], [[Bwtmp[b], Bmem]] * 8)
                    c.op("dve", lambda e: e.tensor_copy(out=KT[:, oc, :], in_=ps[:, :NMEM]), reads=[Bp], writes=[BKT[oc]])
                else:
                    for mb in range(2):
                        ps, Bp = nextps()
                        mm_group(ps[:, :128], Bp, [(mem[:, kc, mb * 128:(mb + 1) * 128], wtmp[b][:, kc, :]) for kc in range(8)], [[Bwtmp[b], Bmem]] * 8)
                        c.op("dve", lambda e: e.tensor_copy(out=V[:, mb, oc * 128:(oc + 1) * 128], in_=ps[:, :128]), reads=[Bp], writes=[BV[oc]])
        sb_ = (sq, Bsq, psn, Bpsn, rs, Brs, hd, Bhd)
        ek = 0
        def ld_t(t_):
            sl_ = slice(t_ * TT, (t_ + 1) * TT)
            c.dma("sp", out=mx[:], in_=mv[:, :, sl_], reads=Bmixed, writes=Bmx)
            c.dma("sp", out=xt[t_ % 2][:], in_=xv[:, :, sl_], writes=Bxt[t_ % 2])

        ld_t(0)
        for tt in range(NTT):
            b = tt % 2
            sl = slice(tt * TT, (tt + 1) * TT)
            for kc in range(8):
                c.op("act", lambda e: e.activation(out=sq[:, kc, :], in_=mx[:, kc, :], func=AF.Square), reads=[Bmx[kc]], writes=[Bsq[kc]])
            for grp, (rr, Brr) in enumerate(((rA, BrA), (rM, BrM))):
                ps, Bp = nextps()
                mm_group(ps[:], Bp, [(cst["avgH"][:], sq[:, grp * 4 + i, :]) for i in range(4)], [[Bsq[grp * 4 + i]] for i in range(4)])
                c.op("act", lambda e: e.activation(out=rr[:], in_=ps[:], func=AF.Sqrt, bias=cst["epsc"][:, 0:1]), reads=[Bp], writes=[Brr])
                c.op("dve", lambda e: e.reciprocal(out=rr[:], in_=rr[:]), reads=[Brr], writes=[Brr])
            for kc in range(8):
                rr, Brr = (rA, BrA) if kc < 4 else (rM, BrM)
                en = "dve" if kc % 2 == 0 else "pool"
                c.op(en, lambda e: e.tensor_tensor(out=mx[:, kc, :], in0=mx[:, kc, :], in1=rr[:], op=ALU.mult), reads=[Bmx[kc], Brr], writes=[Bmx[kc]])

            def proj(wname, src, Bsrc, evac):
                nonlocal ek
                for oc in range(8):
                    ps, Bp = nextps()
                    mm_group(ps[:], Bp, [(wres[wname][:, kc, oc * 128:(oc + 1) * 128], src[:, kc, :]) for kc in range(8)],
                             [[Bwres[wname][oc], Bsrc[kc]] for kc in range(8)])
                    evac(oc, ps, Bp)

            def evac_hd(oc, ps, Bp):
                nonlocal ek
                ek += 1
                if ek % 2 == 0:
                    c.op("dve", lambda e: e.tensor_copy(out=hd[:, oc, :], in_=ps[:]), reads=[Bp], writes=[Bhd[oc]])
                else:
                    c.op("act", lambda e: e.activation(out=hd[:, oc, :], in_=ps[:], func=AF.Copy), reads=[Bp], writes=[Bhd[oc]])

            proj("w_out", mx, Bmx, evac_hd)
            postnorm_residual(c, sb_, hd, Bhd, xt[b], Bxt[b], prm["post_mix"], cst, "mix")
            for kc in range(8):
                c.op("act", lambda e: e.activation(out=sq[:, kc, :], in_=xt[b][:, kc, :], func=AF.Square), reads=[Bxt[b][kc]], writes=[Bsq[kc]])
            mm_group(psn[:], Bpsn, [(cst["avgD"][:], sq[:, kc, :]) for kc in range(8)], [[Bsq[kc]] for kc in range(8)])
            c.op("act", lambda e: e.activation(out=rs[:], in_=psn[:], func=AF.Sqrt, bias=cst["epsc"][:, 0:1]), reads=[Bpsn], writes=[Brs])
            c.op("dve", lambda e: e.reciprocal(out=rs[:], in_=rs[:]), reads=[Brs], writes=[Brs])
            for kc in range(8):
                en = "dve" if kc % 2 == 0 else "pool"
                c.op(en, lambda e: e.tensor_tensor(out=mx[:, kc, :], in0=xt[b][:, kc, :], in1=rs[:], op=ALU.mult), reads=[Bxt[b][kc], Brs], writes=[Bmx[kc]])

            def evac_q(oc, ps, Bp):
                nonlocal ek
                ek += 1
                if ek % 2 == 0:
                    c.op("dve", lambda e: e.tensor_scalar(out=qT[:, oc, :], in0=ps[:], scalar1=1.0 / 16, scalar2=None, op0=ALU.mult), reads=[Bp], writes=[BqT[oc]])
                else:
                    c.op("act", lambda e: e.activation(out=qT[:, oc, :], in_=ps[:], func=AF.Copy, scale=1.0 / 16), reads=[Bp], writes=[BqT[oc]])

            proj("wq", mx, Bmx, evac_q)
            for xh in range(4):
                for mb in range(2):
                    ps, Bp = nextps()
                    mm_group(ps[:], Bp, [(KT[:, 2 * xh + dc, mb * 128:(mb + 1) * 128], qT[:, 2 * xh + dc, :]) for dc in range(2)],
                             [[BKT[2 * xh + dc], BqT[2 * xh + dc]] for dc in range(2)])
                    c.op("act", lambda e: e.activation(out=P[mb][:], in_=ps[:], func=AF.Exp), reads=[Bp], writes=[BP[mb]])
                ps, Bp = nextps()
                mm_group(ps[:], Bp, [(cst["ones"][:], P[mb][:]) for mb in range(2)], [[BP[mb]] for mb in range(2)])
                c.op("dve", lambda e: e.reciprocal(out=rden[:], in_=ps[:]), reads=[Bp], writes=[Brden])
                for dc in range(2):
                    oc = 2 * xh + dc
                    ps, Bp = nextps()
                    mm_group(ps[:], Bp, [(V[:, mb, oc * 128:(oc + 1) * 128], P[mb][:]) for mb in range(2)], [[BV[oc], BP[mb]] for mb in range(2)])
                    c.op("dve", lambda e: e.tensor_tensor(out=on[:, oc, :], in0=ps[:], in1=rden[:], op=ALU.mult), reads=[Bp, Brden], writes=[Bon[oc]])
            proj("wo", on, Bon, evac_hd)
            postnorm_residual(c, sb_, hd, Bhd, xt[b], Bxt[b], prm["post_mem"], cst, "mem")
            if tt + 1 < NTT:
                ld_t(tt + 1)
            c.dma("sp", out=xv[:, :, sl], in_=xt[b][:], reads=Bxt[b])
    c.barrier()


NPAR = 272


def build_program(nl=NL, debug=False):
    nc = bass.Bass("TRN2", target_bir_lowering=False)
    di = lambda n, shp: nc.dram_tensor(n, list(shp), F32, kind="ExternalInput").ap()
    xin = di("xin", (D, S)); memT = di("memT", (D, NMEM))
    w_in = di("w_in", (NL, D, IN_COLS)); w_out = di("w_out", (NL, D, D))
    wq = di("wq_mem", (NL, D, D)); wk = di("wk_mem", (NL, D, D)); wv = di("wv_mem", (NL, D, D)); wo = di("wo_mem", (NL, D, D))
    w_up = di("w_up", (NL, D, 2 * DFF)); w_down = di("w_down", (NL, DFF, D))
    par = di("par", (128, NL, NPAR)); gb = di("gb", (4, NL, 2))
    cin = di("cin", (128, 3, 128)); tabd = di("tab", (128, 24, 256)); seld = di("sel65", (65, 64))
    ca = mlstm_const_arrays()
    cd = {k: di(k, v.shape) for k, v in ca.items()}
    yT = nc.dram_tensor("yT", [D, S], F32, kind="ExternalOutput").ap()
    sk = "ExternalOutput" if debug else "Internal"
    projT = nc.dram_tensor("projT", [3584, S], BF16, kind=sk).ap()
    gatesT = nc.dram_tensor("gatesT", [8, S], F32, kind=sk).ap()
    mixedT = nc.dram_tensor("mixedT", [D, S], BF16, kind=sk).ap()
    hffT = nc.dram_tensor("hffT", [DFF, S], BF16, kind=sk).ap()
    with ExitStack() as es:
        c = Ctx(nc, es)
        cst = mlstm_consts(c, es, cd)
        B = Buf()
        cc = c.sb(es, (128, 3, 128), BF16, "cc")
        for i in range(3):
            c.dma("pool", out=cc[:, i, :], in_=cin[:, i, :], writes=[B])
        cst["avgD"] = cc[:, 0, :]; cst["avgH"] = cc[:, 1, :]; cst["ones"] = cc[:, 2, :]
        cst["epsc"] = c.sb(es, (128, 1), F32, "epsc"); c.op("dve", lambda e: e.memset(cst["epsc"][:], EPS), writes=[B])
        cst["sel65"] = c.sb(es, (65, 64), F32, "sel65"); c.dma("sp", out=cst["sel65"][:], in_=seld, writes=[B])
        cst["tab"] = c.sb(es, (128, 24, 256), BF16, "tab")
        with ExitStack() as s0:
            tf = c.sb(s0, (128, 24, 256), F32, "tabf"); Bt = Buf()
            c.dma("sp", out=tf[:], in_=tabd, writes=[Bt])
            c.op("pool", lambda e: e.tensor_copy(out=cst["tab"][:], in_=tf[:]), reads=[Bt], writes=[B])
            c.barrier()
        pt = c.sb(es, (128, NL, NPAR), F32, "par"); c.dma("sp", out=pt[:], in_=par, writes=[B])
        gbt = c.sb(es, (4, NL, 2), F32, "gb"); c.dma("sp", out=gbt[:], in_=gb, writes=[B])
        c.dma("sp", out=yT, in_=xin, writes=[B])
        c.barrier()
        for l in range(nl):
            P = lambda a, b_: pt[:, l, a:b_]
            Bproj = bufs(29); Bmixed = bufs(12); Bhff = bufs(NFF)
            with ExitStack() as s1:
                hT = c.sb(s1, (128, 8, S), BF16, "hT")
                BhT = [bufs(8) for _ in range(NTT)]
                norm_pass(c, yT, hT, BhT, cst)
                inproj(c, w_in[l], P(0, 8), hT, BhT, projT, gatesT, Bproj)
            attention(c, projT, mixedT, cst, Bproj, Bmixed)
            W = {"w_out": w_out[l], "wq": wq[l], "wk": wk[l], "wv": wv[l], "wo": wo[l]}
            prm = {"gmix": P(8, 16), "post_mix": P(16, 24), "pre_mem": P(24, 32), "post_mem": P(32, 40)}
            with ExitStack() as s2:
                tw = token_weights_alloc(c, s2)
                mlstm(c, projT, gatesT, mixedT, Bproj, Bmixed, P(56, 96).rearrange("p (c f) -> p c f", f=5), gbt[:, l, :], cst,
                      hook=lambda: token_weights_issue(c, tw, W, prm))
                token_phase(c, l, W, prm, mixedT, Bmixed, yT, memT, cst, tw=tw)
            with ExitStack() as s1:
                hT = c.sb(s1, (128, 8, S), BF16, "hT")
                BhT = [bufs(8) for _ in range(NTT)]
                norm_pass(c, yT, hT, BhT, cst)
                ffn_up(c, w_up[l], P(40, 48), P(96, 272).rearrange("p (c f) -> p c f", f=4), hT, BhT, hffT, Bhff)
            ffn_down(c, w_down[l], P(48, 56), hffT, Bhff, yT, cst)
        c.barrier()
    return nc


def host_inputs(inp):
    col = lambda v: np.asarray(v, np.float32).reshape(-1, 128).T
    par = np.zeros((128, NL, NPAR), np.float32)
    gb = np.zeros((4, NL, 2), np.float32)
    for l in range(NL):
        par[:, l, 0:8] = col(inp["pre_mix_g"][l])
        par[:, l, 8:16] = col(np.concatenate([inp["attn_out_g"][l], inp["mlstm_out_g"][l]]))
        par[:, l, 16:24] = col(inp["post_mix_g"][l]); par[:, l, 24:32] = col(inp["pre_mem_g"][l]); par[:, l, 32:40] = col(inp["post_mem_g"][l])
        par[:, l, 40:48] = col(inp["pre_ffn_g"][l]); par[:, l, 48:56] = col(inp["post_ffn_g"][l])
        mc = np.concatenate([inp["mconv_w"][l], inp["mconv_b"][l][None]], 0)
        par[:, l, 56:96] = mc.reshape(5, 8, 128).transpose(2, 1, 0).reshape(128, 40)
        fc = np.concatenate([inp["fconv_w"][l], inp["fconv_b"][l][None]], 0)
        par[:, l, 96:272] = fc.reshape(4, 44, 128).transpose(2, 1, 0).reshape(128, 176)
        gb[:, l, 0] = inp["b_igate"][l]; gb[:, l, 1] = inp["b_fgate"][l]
    sel = np.zeros((65, 64), np.float32); sel[64] = 1.0
    cin = np.stack([np.full((128, 128), 1 / 1024), np.full((128, 128), 1 / 512), np.ones((128, 128))], 1).astype(np.float32)
    shared = {"par": par, "gb": gb, "cin": cin, "tab": make_tables(np.asarray(inp["rel_bias"], np.float32)), "sel65": sel}
    shared.update(mlstm_const_arrays())
    for k in ("w_in", "w_out", "wq_mem", "wk_mem", "wv_mem", "wo_mem", "w_up", "w_down"):
        shared[k] = np.ascontiguousarray(inp[k], dtype=np.float32)
    maps = []
    for b in range(4):
        m = dict(shared)
        m["xin"] = np.ascontiguousarray(np.asarray(inp["x"][b], np.float32).T)
        m["memT"] = np.ascontiguousarray(np.asarray(inp["mem"][b], np.float32).T)
        maps.append(m)
    return maps


def kernel(**inputs):
    nc = build_program(NL)
    maps = host_inputs(inputs)
    res = run_bass_kernel_spmd(nc, maps, core_ids=[0, 1, 2, 3])
    out = np.stack([np.ascontiguousarray(np.asarray(r["yT"]).T) for r in res.results], 0)
    return out.astype(np.float32)
```

```python
import numpy as np
from contextlib import ExitStack
import concourse.bass as bass
import concourse.mybir as mybir
from concourse.bass_utils import run_bass_kernel_spmd
from concourse.alu_op_type import AluOpType as ALU

AF = mybir.ActivationFunctionType
AX = mybir.AxisListType
F32 = mybir.dt.float32
BF16 = mybir.dt.bfloat16

S = 4096
D = 1024
NL = 4
TT = 512
NTT = S // TT
EPS = 1e-6
IN_COLS = 3592
DFF = 2816
NFF = DFF // 128
NMEM = 256


class Buf:
    __slots__ = ("w", "r")

    def __init__(self):
        self.w = None
        self.r = {}


def bufs(n):
    return [Buf() for _ in range(n)]


class Ctx:
    def __init__(self, nc, es, n_dma_sems=40):
        self.nc = nc
        self.es = es
        self.eng = dict(pe=nc.tensor, dve=nc.vector, act=nc.scalar, pool=nc.gpsimd, sp=nc.sync)
        self.esem = {}
        self.cnt = {}
        self.nsem = 0
        for e in self.eng:
            self._new_esem(e)
        self.waited = {}
        self.dsems = [es.enter_context(nc.semaphore(f"dq{i}")) for i in range(n_dma_sems)]
        self.dval = [0] * n_dma_sems
        self.dnext = 0
        self.n_hw = n_dma_sems - 6
        self.dnext_sw = 0
        self.recent_dma = {}
        self.uid = 0
        self.ninstr = 0

    def _new_esem(self, e):
        self.nsem += 1
        self.esem[e] = self.es.enter_context(self.nc.semaphore(f"e{e}{self.nsem}"))
        self.cnt[e] = 0

    def name(self, p):
        self.uid += 1
        return f"{p}_{self.uid}"

    def sb(self, st, shape, dtype, name="t"):
        return st.enter_context(self.nc.sbuf_tensor(self.name(name), list(shape), dtype))

    def ps(self, st, shape=(128, 512), dtype=F32, name="ps"):
        return st.enter_context(self.nc.psum_tensor(self.name(name), list(shape), dtype))

    def _wait(self, e, tok, raw=False):
        sem, val, owner = tok
        if owner == e and (e == "pe" or e == "sp"):
            return
        key = (e, id(sem))
        if self.waited.get(key, 0) >= val:
            return
        self.waited[key] = val
        self.eng[e].wait_ge(sem, val)
        self.ninstr += 1

    def _deps(self, e, reads, writes):
        for b in reads:
            if b.w is not None:
                self._wait(e, b.w, raw=True)
        for b in writes:
            if b.w is not None:
                self._wait(e, b.w)
            for t in b.r.values():
                self._wait(e, t)

    def _mark(self, tok, reads, writes):
        for b in reads:
            k = id(tok[0])
            o = b.r.get(k)
            if o is None or o[1] < tok[1]:
                b.r[k] = tok
        for b in writes:
            b.w = tok
            b.r = {}

    def op(self, e, fn, reads=(), writes=(), inc=True):
        self._deps(e, reads, writes)
        ins = fn(self.eng[e])
        self.ninstr += 1
        if inc:
            if self.cnt[e] >= 30000:
                self._new_esem(e)
            self.cnt[e] += 1
            ins.then_inc(self.esem[e], 1)
            tok = (self.esem[e], self.cnt[e], e)
        else:
            if self.cnt[e] >= 30000:
                self._new_esem(e)
            tok = (self.esem[e], self.cnt[e] + 1, e)
        self._mark(tok, reads, writes)
        return tok

    def dma(self, q, out, in_, reads=(), writes=()):
        if q == "pool":
            i = self.n_hw + self.dnext_sw
            self.dnext_sw = (self.dnext_sw + 1) % (len(self.dsems) - self.n_hw)
        else:
            i = self.dnext
            self.dnext = (i + 1) % self.n_hw
        sem = self.dsems[i]
        old = self.dval[i]
        self._deps(q, reads, writes)
        if old > 0:
            self._wait(q, (sem, old, None))
        ins = self.eng[q].dma_start(out=out, in_=in_)
        self.ninstr += 1
        self.dval[i] = old + 16
        ins.then_inc(sem, 16)
        tok = (sem, old + 16, "dma")
        self.recent_dma[id(sem)] = tok
        self._mark(tok, reads, writes)
        return tok

    def barrier(self, engines=("pe", "dve", "act", "pool", "sp")):
        toks = [(self.esem[e], self.cnt[e], e) for e in self.eng if self.cnt[e] > 0]
        toks += list(self.recent_dma.values())
        for e in engines:
            for t in toks:
                self._wait(e, t, raw=True)
        self.recent_dma = {}


import math


def norm_pass(c, xT, hT, BhT, cst):
    nc = c.nc
    xv = xT.rearrange("(kc p) t -> p kc t", p=128)
    with ExitStack() as st:
        xt = [c.sb(st, (128, 8, TT), F32, "xt") for _ in range(2)]
        sq = [c.sb(st, (128, 8, TT), BF16, "sq") for _ in range(2)]
        rs = [c.sb(st, (128, TT), F32, "rs") for _ in range(2)]
        pss = [c.ps(st) for _ in range(2)]
        Bxt = bufs(2); Bsq = [bufs(8) for _ in range(2)]; Brs = bufs(2); Bps = bufs(2)
        for tt in range(NTT):
            b = tt % 2
            sl = slice(tt * TT, (tt + 1) * TT)
            c.dma("sp", out=xt[b][:], in_=xv[:, :, sl], writes=[Bxt[b]])
            for kc in range(8):
                c.op("act", lambda e: e.activation(out=sq[b][:, kc, :], in_=xt[b][:, kc, :], func=AF.Square),
                     reads=[Bxt[b]], writes=[Bsq[b][kc]])
            for kc in range(8):
                c.op("pe", lambda e: e.matmul(pss[b][:], lhsT=cst["avgD"][:], rhs=sq[b][:, kc, :], start=(kc == 0), stop=(kc == 7)),
                     reads=[Bsq[b][kc]], writes=[Bps[b]] if kc == 0 else [], inc=(kc == 7))
            Bps[b].w = (c.esem["pe"], c.cnt["pe"], "pe")
            c.op("act", lambda e: e.activation(out=rs[b][:], in_=pss[b][:], func=AF.Sqrt, bias=cst["epsc"][:, 0:1]),
                 reads=[Bps[b]], writes=[Brs[b]])
            c.op("dve", lambda e: e.reciprocal(out=rs[b][:], in_=rs[b][:]),
                 reads=[Brs[b]], writes=[Brs[b]])
            for kc in range(8):
                en = "dve" if kc % 2 == 0 else "pool"
                c.op(en, lambda e: e.tensor_tensor(out=hT[:, kc, sl], in0=xt[b][:, kc, :], in1=rs[b][:], op=ALU.mult),
                     reads=[Bxt[b], Brs[b]], writes=[BhT[tt][kc]])
    c.barrier()


def inproj(c, w_in, gcol, hT, BhT, projT, gatesT, Bproj):
    wv = w_in.rearrange("(kc p) n -> p kc n", p=128)
    with ExitStack() as st:
        ws = [c.sb(st, (128, 8, 128), F32, "ws") for _ in range(2)]
        wb = [c.sb(st, (128, 8, 128), BF16, "wb") for _ in range(2)]
        ob = [c.sb(st, (128, S), BF16, "ob") for _ in range(2)]
        og = c.sb(st, (8, S), F32, "og")
        pss = [c.ps(st) for _ in range(4)]
        Bws = bufs(2); Bwb = [bufs(8) for _ in range(2)]; Bob = [bufs(NTT) for _ in range(2)]; Bps = bufs(4)
        Bog = bufs(NTT)
        k = 0
        for fc in range(29):
            ncol = 128 if fc < 28 else 8
            b = fc % 2
            c.dma("sp", out=ws[b][:, :, :ncol], in_=wv[:, :, fc * 128: fc * 128 + ncol], writes=[Bws[b]])
            c.op("pool", lambda e: e.tensor_tensor(out=wb[b][:, :, :ncol], in0=ws[b][:, :, :ncol], in1=gcol.unsqueeze(2).to_broadcast([128, 8, ncol]), op=ALU.mult),
                 reads=[Bws[b]], writes=Bwb[b])
            for tt in range(NTT):
                sl = slice(tt * TT, (tt + 1) * TT)
                p = k % 4
                for kc in range(8):
                    c.op("pe", lambda e: e.matmul(pss[p][:ncol, :], lhsT=wb[b][:, kc, :ncol], rhs=hT[:, kc, sl], start=(kc == 0), stop=(kc == 7)),
                         reads=[Bwb[b][kc], BhT[tt][kc]], writes=[Bps[p]] if kc == 0 else [], inc=(kc == 7))
                Bps[p].w = (c.esem["pe"], c.cnt["pe"], "pe")
                if fc == 28:
                    c.op("dve", lambda e: e.tensor_copy(out=og[:, sl], in_=pss[p][:8, :]), reads=[Bps[p]], writes=[Bog[tt]])
                elif fc < 4:
                    if k % 2 == 0:
                        c.op("act", lambda e: e.activation(out=ob[b][:, sl], in_=pss[p][:], func=AF.Copy, scale=0.125), reads=[Bps[p]], writes=[Bob[b][tt]])
                    else:
                        c.op("dve", lambda e: e.tensor_scalar(out=ob[b][:, sl], in0=pss[p][:], scalar1=0.125, scalar2=None, op0=ALU.mult), reads=[Bps[p]], writes=[Bob[b][tt]])
                else:
                    if k % 2 == 0:
                        c.op("act", lambda e: e.activation(out=ob[b][:, sl], in_=pss[p][:], func=AF.Copy), reads=[Bps[p]], writes=[Bob[b][tt]])
                    else:
                        c.op("dve", lambda e: e.tensor_copy(out=ob[b][:, sl], in_=pss[p][:]), reads=[Bps[p]], writes=[Bob[b][tt]])
                k += 1
            if fc == 28:
                c.dma("sp", out=gatesT[:, :], in_=og[:], reads=Bog, writes=[Bproj[28]])
            else:
                c.dma("sp", out=projT[fc * 128:(fc + 1) * 128, :], in_=ob[b][:], reads=Bob[b], writes=[Bproj[fc]])
    c.barrier()


def load_consts(c, st, cdram):
    cst = {}
    B = Buf()
    def ld(name, shape, dtype):
        t = c.sb(st, shape, dtype, name)
        c.dma("pool" if dtype == BF16 else "sp", out=t[:], in_=cdram[name], writes=[B])
        cst[name] = t
    ld("avgD", (128, 128), BF16)
    t = c.sb(st, (128, 1), F32, "epsc")
    c.op("dve", lambda e: e.memset(t[:], EPS), writes=[B])
    cst["epsc"] = t
    return cst, B


DILS = (1, 4, 16)

def attention(c, projT, mixedT, cst, Bproj, Bmixed):
    with ExitStack() as st:
        qkv = [c.sb(st, (128, S), BF16, "qkv") for _ in range(3)]
        perm = [[c.sb(st, (128, S), BF16, "perm") for _ in range(3)] for _ in range(2)]
        vtok = [c.sb(st, (128, 32, 2, 65), BF16, "vtok") for _ in range(3)]
        acc = [c.sb(st, (65, S), F32, "acc") for _ in range(2)]
        pt = [c.sb(st, (128, 2, 256), BF16, "pt") for _ in range(3)]
        rec = [c.sb(st, (64, TT), F32, "rec") for _ in range(2)]
        ao = [c.sb(st, (64, S), BF16, "ao") for _ in range(2)]
        sp = [c.ps(st, (128, 2, 256), F32, "sp") for _ in range(2)]
        tps = [c.ps(st, (128, 8, 128), BF16, "tp") for _ in range(2)]
        ops = [c.ps(st, (128, 4, 128), F32, "ops") for _ in range(3)]
        dps = ops[2:3]
        Bqkv = bufs(3); Bperm = [bufs(3) for _ in range(2)]; Bvt = [bufs(8) for _ in range(3)]
        Bacc = bufs(2); Bpt = bufs(3); Brec = bufs(2); Bao = [bufs(NTT) for _ in range(2)]
        Bsp = bufs(2); Btp = bufs(2); Bo = bufs(3); Bdps = Bo[2:3]
        ident = cst["ident"]; tab = cst["tab"]; sel = cst["sel65"]
        for pi in range(3):
            c.op("pool", lambda e: e.memset(vtok[pi][:, :, :, 64:65], 1.0), writes=Bvt[pi])
        kq = 0
        def ld_qkv(hp_):
            for i_ in range(3):
                c.dma("sp", out=qkv[i_][:], in_=projT[i_ * 512 + hp_ * 128: i_ * 512 + hp_ * 128 + 128, :], reads=[Bproj[i_ * 4 + hp_]], writes=[Bqkv[i_]])

        ld_qkv(0)
        for hp in range(4):
            for pi, dil in enumerate(DILS):
                nbc = 32 // dil
                if dil == 1:
                    src = qkv; Bsrc = Bqkv
                else:
                    src = perm[pi - 1]; Bsrc = Bperm[pi - 1]
                    for i in range(3):
                        c.op("pool", lambda e: e.tensor_copy(out=src[i][:].rearrange("p (c i) -> p c i", c=dil),
                                                             in_=qkv[i][:].rearrange("p (i c) -> p c i", c=dil)),
                             reads=[Bqkv[i]], writes=[Bsrc[i]])
                qP, kP, vP = src
                for g in range(8):
                    tb = g % 2
                    for j in range(4):
                        blk = g * 4 + j
                        c.op("pe", lambda e: e.transpose(out=tps[tb][:, j, :], in_=vP[:, blk * 128:(blk + 1) * 128], identity=ident[:]),
                             reads=[Bsrc[2]], writes=[Btp[tb]] if j == 0 else [], inc=(j == 3))
                    Btp[tb].w = (c.esem["pe"], c.cnt["pe"], "pe")
                    en = "dve" if g % 2 == 0 else "act"
                    o_ap = vtok[pi][:, g * 4:(g + 1) * 4, :, 0:64]
                    i_ap = tps[tb][:, 0:4, :].rearrange("p j (h d) -> p j h d", h=2)
                    if en == "dve":
                        c.op("dve", lambda e: e.tensor_copy(out=o_ap, in_=i_ap), reads=[Btp[tb]], writes=[Bvt[pi][g]])
                    else:
                        c.op("act", lambda e: e.activation(out=o_ap, in_=i_ap, func=AF.Copy), reads=[Btp[tb]], writes=[Bvt[pi][g]])
                for h in range(2):
                    hd = hp * 2 + h
                    rows = slice(h * 64, h * 64 + 64)
                    accv = acc[h][:, :].rearrange("p (i c) -> p c i", c=dil)
                    for cl in range(dil):
                        for n2 in range(0, nbc, 2):
                            s_ = kq % 2; p_ = kq % 3; kq += 1
                            for j in range(2):
                                n = n2 + j; gb = cl * nbc + n
                                nq = 256 if n < nbc - 1 else 128
                                c.op("pe", lambda e: e.matmul(sp[s_][:, j, :nq], lhsT=kP[rows, gb * 128:(gb + 1) * 128], rhs=qP[rows, gb * 128: gb * 128 + nq],
                                                              start=True, stop=False),
                                     reads=[Bsrc[0], Bsrc[1]], writes=[Bsp[s_]] if j == 0 else [], inc=False)
                                c.op("pe", lambda e: e.matmul(sp[s_][:, j, :nq], lhsT=ident[:], rhs=tab[:, hd * 3 + pi, :nq], start=False, stop=True),
                                     inc=(j == 1))
                            Bsp[s_].w = (c.esem["pe"], c.cnt["pe"], "pe")
                            last = (n2 + 1 == nbc - 1)
                            if not last:
                                c.op("act", lambda e: e.activation(out=pt[p_][:], in_=sp[s_][:], func=AF.Exp), reads=[Bsp[s_]], writes=[Bpt[p_]])
                            else:
                                c.op("act", lambda e: e.activation(out=pt[p_][:, 0, :], in_=sp[s_][:, 0, :], func=AF.Exp), reads=[Bsp[s_]], writes=[Bpt[p_]])
                                c.op("act", lambda e: e.activation(out=pt[p_][:, 1, :128], in_=sp[s_][:, 1, :128], func=AF.Exp), reads=[Bsp[s_]], writes=[])
                                Bpt[p_].w = (c.esem["act"], c.cnt["act"], "act")
                            for j in range(2):
                                n = n2 + j; gb = cl * nbc + n
                                oi = gb % 3
                                ot = ops[oi][:65, 0, :]
                                c.op("pe", lambda e: e.matmul(ot, lhsT=vtok[pi][:, gb, h, :], rhs=pt[p_][:, j, 0:128], start=(n == 0), stop=True),
                                     reads=[Bpt[p_], Bvt[pi][gb // 4]], writes=[Bo[oi]] if n == 0 else [], inc=True)
                                Bo[oi].w = (c.esem["pe"], c.cnt["pe"], "pe")
                                av = accv[:, cl, n * 128:(n + 1) * 128]
                                if pi == 0:
                                    c.op("dve", lambda e: e.tensor_copy(out=av, in_=ot), reads=[Bo[oi]], writes=[Bacc[h]])
                                else:
                                    c.op("dve", lambda e: e.tensor_tensor(out=av, in0=av, in1=ot, op=ALU.add), reads=[Bo[oi], Bacc[h]], writes=[Bacc[h]])
                                if n < nbc - 1:
                                    oi2 = (gb + 1) % 3
                                    ot2 = ops[oi2][:65, 0, :]
                                    c.op("pe", lambda e: e.matmul(ot2, lhsT=vtok[pi][:, gb, h, :], rhs=pt[p_][:, j, 128:256], start=True, stop=False),
                                         reads=[Bpt[p_], Bvt[pi][gb // 4]], writes=[Bo[oi2]], inc=False)
            if hp + 1 < 4:
                ld_qkv(hp + 1)
            for h in range(2):
                hd = hp * 2 + h
                for tt in range(NTT):
                    sl = slice(tt * TT, (tt + 1) * TT)
                    r_ = tt % 2
                    c.op("pe", lambda e: e.matmul(dps[0][:64, :, :], lhsT=sel[:65, :], rhs=acc[h][:65, sl], start=True, stop=True),
                         reads=[Bacc[h]], writes=[Bdps[0]])
                    c.op("dve", lambda e: e.reciprocal(out=rec[r_][:], in_=dps[0][:64, :, :].rearrange("p a b -> p (a b)")), reads=[Bdps[0]], writes=[Brec[r_]])
                    c.op("pool", lambda e: e.tensor_tensor(out=ao[h][:, sl], in0=acc[h][:64, sl], in1=rec[r_][:], op=ALU.mult),
                         reads=[Bacc[h], Brec[r_]], writes=[Bao[h][tt]])
                c.dma("sp", out=mixedT[hd * 64:(hd + 1) * 64, :], in_=ao[h][:], reads=Bao[h], writes=[Bmixed[hd]])
    c.barrier()


def t5_bucket(dist):
    dist = np.asarray(dist)
    max_exact = 16
    d_f = np.maximum(dist, 1).astype(np.float32)
    large = max_exact + (np.log(d_f / max_exact) / np.log(2048 / max_exact) * (32 - max_exact)).astype(np.int32)
    large = np.minimum(large, 31)
    return np.where(dist < max_exact, dist, large)


def make_tables(rel_bias):
    s = np.arange(128)[:, None]
    t = np.arange(128)[None, :]
    tabs = np.full((128, 24, 256), -30000.0, np.float32)
    for pi, dil in enumerate(DILS):
        bsub = rel_bias[t5_bucket(np.arange(129) * dil)]
        d0 = t - s
        d1 = t + 128 - s
        for h in range(8):
            tabs[:, h * 3 + pi, 0:128] = np.where(d0 >= 0, bsub[np.clip(d0, 0, 128), h], -30000.0)
            tabs[:, h * 3 + pi, 128:256] = np.where(d1 <= 128, bsub[np.clip(d1, 0, 128), h], -30000.0)
    return tabs


def mlstm(c, projT, gatesT, mixedT, Bproj, Bmixed, mcp, gbias, cst, hook=None):
    ident = cst["ident"]; identf = cst["identf"]; cm = cst["cmask"]
    with ExitStack() as st:
        et = c.sb(st, (128, 32, 8), F32, "et"); Bet = Buf()
        decb = c.sb(st, (128, 128), F32, "decb"); Bdecb = Buf()
        with ExitStack() as s0:
            gi = c.sb(s0, (4, S), F32, "gi"); gf = c.sb(s0, (4, S), F32, "gf"); nb = c.sb(s0, (4, S), F32, "nb")
            dmb = c.sb(s0, (4, S), F32, "dmb"); t1 = c.sb(s0, (4, S), F32, "t1"); t2 = c.sb(s0, (4, S), F32, "t2")
            sm = c.sb(s0, (4, 8, 32), F32, "sm")
            decbd = c.sb(s0, (4, 4, 32), F32, "decbd"); nbf = c.sb(s0, (4, 2), F32, "nbf")
            pg = c.ps(s0, (128, 32, 8), F32, "pg"); pd = c.ps(s0, (128, 128), F32, "pd")
            G = Buf()
            c.dma("sp", out=gi[:], in_=gatesT[0:4, :], reads=[Bproj[28]], writes=[G])
            c.dma("sp", out=gf[:], in_=gatesT[4:8, :], reads=[Bproj[28]], writes=[G])
            def g(en, fn):
                c.op(en, fn, reads=[G], writes=[G])
            g("dve", lambda e: e.tensor_scalar(out=nbf[:, 0:1], in0=gbias[:, 1:2], scalar1=-1.0, scalar2=None, op0=ALU.mult))
            g("dve", lambda e: e.memset(nbf[:, 1:2], -0.5 * math.log(128.0)))
            g("act", lambda e: e.activation(out=t1[:], in_=gf[:], func=AF.Exp, scale=-1.0, bias=nbf[:, 0:1]))
            g("act", lambda e: e.activation(out=t1[:], in_=t1[:], func=AF.Ln, bias=cst["one1"][:4, 0:1]))
            g("dve", lambda e: e.tensor_tensor_scan(out=nb[:], data0=cst["rmask"][:4, :], data1=t1[:], initial=0.0, op0=ALU.mult, op1=ALU.add))
            g("dve", lambda e: e.scalar_tensor_tensor(out=dmb[:], in0=gi[:], scalar=gbias[:, 0:1], in1=nb[:], op0=ALU.add, op1=ALU.add))
            g("dve", lambda e: e.tensor_reduce(out=sm[:, 0, :], in_=dmb[:].rearrange("p (c s) -> p c s", s=128), axis=AX.X, op=ALU.max))
            g("dve", lambda e: e.tensor_scalar(out=sm[:, 1, :], in0=nb[:].rearrange("p (c s) -> p c s", s=128)[:, :, 127], scalar1=-1.0, scalar2=None, op0=ALU.mult))
            g("dve", lambda e: e.tensor_tensor_scan(out=sm[:, 2, :], data0=sm[:, 0, :], data1=sm[:, 1, :], initial=0.0, op0=ALU.max, op1=ALU.add))
            g("dve", lambda e: e.memset(sm[:, 3, 0:1], 0.0))
            g("dve", lambda e: e.tensor_copy(out=sm[:, 3, 1:32], in_=sm[:, 2, 0:31]))
            g("dve", lambda e: e.tensor_tensor(out=sm[:, 4, :], in0=sm[:, 3, :], in1=sm[:, 0, :], op=ALU.max))
            g("dve", lambda e: e.tensor_tensor(out=sm[:, 5, :], in0=sm[:, 3, :], in1=sm[:, 4, :], op=ALU.subtract))
            g("act", lambda e: e.activation(out=sm[:, 5, :], in_=sm[:, 5, :], func=AF.Exp))
            Mb = sm[:, 4, :].unsqueeze(2).to_broadcast([4, 32, 128])
            g("dve", lambda e: e.tensor_tensor(out=t1[:].rearrange("p (c s) -> p c s", s=128), in0=dmb[:].rearrange("p (c s) -> p c s", s=128), in1=Mb, op=ALU.subtract))
            g("act", lambda e: e.activation(out=t1[:], in_=t1[:], func=AF.Exp, bias=nbf[:, 1:2]))
            g("dve", lambda e: e.tensor_tensor(out=t2[:].rearrange("p (c s) -> p c s", s=128), in0=nb[:].rearrange("p (c s) -> p c s", s=128), in1=Mb, op=ALU.subtract))
            g("act", lambda e: e.activation(out=t2[:], in_=t2[:], func=AF.Exp))
            for cc in range(32):
                cs = slice(cc * 128, (cc + 1) * 128)
                c.op("pe", lambda e: e.transpose(out=pg[:, cc, 0:4], in_=t1[:, cs], identity=identf[:4, :4]), reads=[G], writes=[G] if cc == 0 else [], inc=False)
                c.op("pe", lambda e: e.transpose(out=pg[:, cc, 4:8], in_=t2[:, cs], identity=identf[:4, :4]), reads=[G], inc=(cc == 31))
            G.w = (c.esem["pe"], c.cnt["pe"], "pe")
            c.op("dve", lambda e: e.tensor_copy(out=et[:], in_=pg[:]), reads=[G], writes=[Bet])
            g("dve", lambda e: e.tensor_tensor(out=decbd[:], in0=sm[:, 5, :].unsqueeze(1).to_broadcast([4, 4, 32]), in1=cst["bdmask"][:4, :].rearrange("p (a b) -> p a b", a=4), op=ALU.mult))
            c.op("pe", lambda e: e.matmul(pd[:], lhsT=cst["ones4"][:4, :], rhs=decbd[:].rearrange("p a b -> p (a b)"), start=True, stop=True), reads=[G], writes=[G])
            c.op("dve", lambda e: e.tensor_copy(out=decb[:], in_=pd[:]), reads=[G], writes=[Bdecb])
            c.barrier()
        raw = [c.sb(st, (128, 3 + S), BF16, "raw") for _ in range(2)]; Braw = bufs(2)
        cv = [c.sb(st, (128, S), F32, "cv") for _ in range(2)]; Bcv = bufs(2)
        qT = c.sb(st, (128, S), BF16, "qT"); kT = c.sb(st, (128, S), BF16, "kT"); BqT = Buf(); BkT = Buf()
        vT = c.sb(st, (128, S), BF16, "vT"); oT = c.sb(st, (128, S), BF16, "oT"); BvT = Buf(); BoT = Buf()
        hmT = c.sb(st, (128, S), BF16, "hmT"); BhmT = bufs(32)
        ktk = [c.sb(st, (128, 128), BF16, "ktk") for _ in range(2)]; Bktk = bufs(2)
        vaug = [c.sb(st, (128, 129), BF16, "vaug") for _ in range(2)]; Bvaug = bufs(2)
        og = [c.sb(st, (128, 128), F32, "og") for _ in range(2)]; Bog = bufs(2)
        pT = [c.sb(st, (128, 128), BF16, "pT") for _ in range(2)]; BpT = bufs(2)
        hmt = [c.sb(st, (128, 128), BF16, "hmt") for _ in range(2)]; Bhmt = bufs(2)
        C = c.sb(st, (128, 129), F32, "C"); BC = Buf()
        Cbf = [c.sb(st, (128, 129), BF16, "Cbf") for _ in range(2)]; BCbf = bufs(2)
        dd = [c.sb(st, (128, 2), F32, "dd") for _ in range(2)]; Bdd = bufs(2)
        tpk = [c.ps(st, (128, 8, 128), BF16, "tpk") for _ in range(2)]; Btpk = bufs(2)
        sps = c.ps(st, (128, 512), F32, "sps"); Bsps = Buf()
        nps = [c.ps(st, (128, 512), F32, "nps") for _ in range(2)]; Bnps = bufs(2)
        dps = c.ps(st, (128, 512), F32, "dps"); Bdps = Buf()
        tph = c.ps(st, (128, 8, 128), BF16, "tph"); Btph = Buf()
        for r in range(2):
            c.op("pool", lambda e: e.memset(raw[r][:, 0:3], 0.0), writes=[Braw[r]])
            c.op("pool", lambda e: e.memset(vaug[r][:, 128:129], 1.0), writes=[Bvaug[r]])
        for hd in range(4):
            c.dma("sp", out=raw[0][:, 3:], in_=projT[1536 + hd * 128:1536 + (hd + 1) * 128, :], reads=[Bproj[12 + hd]], writes=[Braw[0]])
            c.dma("sp", out=raw[1][:, 3:], in_=projT[2048 + hd * 128:2048 + (hd + 1) * 128, :], reads=[Bproj[16 + hd]], writes=[Braw[1]])
            c.dma("sp", out=vT[:], in_=projT[2560 + hd * 128:2560 + (hd + 1) * 128, :], reads=[Bproj[20 + hd]], writes=[BvT])
            c.dma("sp", out=oT[:], in_=projT[3072 + hd * 128:3072 + (hd + 1) * 128, :], reads=[Bproj[24 + hd]], writes=[BoT])
            if hd == 0 and hook is not None:
                hook()
            for i, en, dst, Bdst in ((0, "dve", qT, BqT), (1, "dve", kT, BkT)):
                ci = i * 4 + hd
                c.op(en, lambda e: e.tensor_scalar(out=cv[i][:], in0=raw[i][:, 3:3 + S], scalar1=mcp[:, ci, 3:4], scalar2=mcp[:, ci, 4:5], op0=ALU.mult, op1=ALU.add),
                     reads=[Braw[i]], writes=[Bcv[i]])
                for j in range(3):
                    c.op(en, lambda e: e.scalar_tensor_tensor(out=cv[i][:], in0=raw[i][:, j:j + S], scalar=mcp[:, ci, j:j + 1], in1=cv[i][:], op0=ALU.mult, op1=ALU.add),
                         reads=[Braw[i], Bcv[i]], writes=[Bcv[i]])
                c.op("act", lambda e: e.activation(out=dst[:], in_=cv[i][:], func=AF.Silu), reads=[Bcv[i]], writes=[Bdst])
            for cc in range(32):
                r = cc % 2
                cs = slice(cc * 128, (cc + 1) * 128)
                ecol = et[:, cc, hd:hd + 1]; fcol = et[:, cc, 4 + hd:5 + hd]
                for j, (src, Bsrc) in enumerate(((kT, BkT), (vT, BvT), (oT, BoT))):
                    c.op("pe", lambda e: e.transpose(out=tpk[r][:, j, :], in_=src[:, cs], identity=ident[:]), reads=[Bsrc], writes=[Btpk[r]] if j == 0 else [], inc=(j == 2))
                Btpk[r].w = (c.esem["pe"], c.cnt["pe"], "pe")
                c.op("dve", lambda e: e.tensor_scalar(out=ktk[r][:], in0=tpk[r][:, 0, :], scalar1=ecol, scalar2=None, op0=ALU.mult), reads=[Btpk[r], Bet], writes=[Bktk[r]])
                c.op("act", lambda e: e.activation(out=vaug[r][:, 0:128], in_=tpk[r][:, 1, :], func=AF.Copy), reads=[Btpk[r]], writes=[Bvaug[r]])
                c.op("act", lambda e: e.activation(out=og[r][:], in_=tpk[r][:, 2, :], func=AF.Sigmoid), reads=[Btpk[r]], writes=[Bog[r]])
                c.op("pe", lambda e: e.matmul(sps[:, 0:128], lhsT=kT[:, cs], rhs=qT[:, cs], start=True, stop=True), reads=[BkT, BqT], writes=[Bsps])
                c.op("dve", lambda e: e.scalar_tensor_tensor(out=pT[r][:], in0=sps[:, 0:128], scalar=ecol, in1=cm[:], op0=ALU.mult, op1=ALU.mult), reads=[Bsps, Bet], writes=[BpT[r]])
                if cc > 0:
                    dcol = decb[:, hd * 32 + cc: hd * 32 + cc + 1]
                    c.op("dve", lambda e: e.tensor_scalar(out=C[:], in0=C[:], scalar1=dcol, scalar2=None, op0=ALU.mult), reads=[BC, Bdecb], writes=[BC])
                    c.op("act", lambda e: e.activation(out=Cbf[r][:], in_=C[:], func=AF.Copy), reads=[BC], writes=[BCbf[r]])
                c.op("pe", lambda e: e.matmul(nps[r][:, 0:129], lhsT=pT[r][:], rhs=vaug[r][:], start=True, stop=(cc == 0)), reads=[BpT[r], Bvaug[r]], writes=[Bnps[r]], inc=(cc == 0))
                if cc > 0:
                    c.op("pe", lambda e: e.matmul(nps[r][:, 0:129], lhsT=qT[:, cs], rhs=Cbf[r][:], start=False, stop=True), reads=[BqT, BCbf[r]], inc=True)
                Bnps[r].w = (c.esem["pe"], c.cnt["pe"], "pe")
                c.op("pe", lambda e: e.matmul(dps[:, 0:129], lhsT=ktk[r][:], rhs=vaug[r][:], start=True, stop=True), reads=[Bktk[r], Bvaug[r]], writes=[Bdps])
                if cc == 0:
                    c.op("dve", lambda e: e.tensor_copy(out=C[:], in_=dps[:, 0:129]), reads=[Bdps], writes=[BC])
                else:
                    c.op("dve", lambda e: e.tensor_tensor(out=C[:], in0=C[:], in1=dps[:, 0:129], op=ALU.add), reads=[Bdps, BC], writes=[BC])
                c.op("dve", lambda e: e.tensor_scalar(out=dd[r][:, 1:2], in0=nps[r][:, 128:129], scalar1=-1.0, scalar2=None, op0=ALU.mult), reads=[Bnps[r]], writes=[Bdd[r]])
                c.op("dve", lambda e: e.scalar_tensor_tensor(out=dd[r][:, 0:1], in0=nps[r][:, 128:129], scalar=fcol, in1=dd[r][:, 1:2], op0=ALU.max, op1=ALU.max), reads=[Bnps[r], Bet, Bdd[r]], writes=[Bdd[r]])
                c.op("dve", lambda e: e.reciprocal(out=dd[r][:, 1:2], in_=dd[r][:, 0:1]), reads=[Bdd[r]], writes=[Bdd[r]])
                c.op("dve", lambda e: e.scalar_tensor_tensor(out=hmt[r][:], in0=nps[r][:, 0:128], scalar=dd[r][:, 1:2], in1=og[r][:], op0=ALU.mult, op1=ALU.mult),
                     reads=[Bnps[r], Bdd[r], Bog[r]], writes=[Bhmt[r]])
                c.op("pe", lambda e: e.transpose(out=tph[:, 0, :], in_=hmt[r][:], identity=ident[:]), reads=[Bhmt[r]], writes=[Btph])
                c.op("act", lambda e: e.activation(out=hmT[:, cs], in_=tph[:, 0, :], func=AF.Copy), reads=[Btph], writes=[BhmT[cc]])
            c.dma("sp", out=mixedT[512 + hd * 128: 512 + (hd + 1) * 128, :], in_=hmT[:], reads=BhmT, writes=[Bmixed[8 + hd]])
    c.barrier()


def mlstm_consts(c, st, cd):
    cst = {}; B = Buf()
    cst["identf"] = c.sb(st, (128, 128), F32, "identf"); c.dma("sp", out=cst["identf"][:], in_=cd["identf"], writes=[B])
    cst["ident"] = c.sb(st, (128, 128), BF16, "ident"); c.dma("pool", out=cst["ident"][:], in_=cd["identf"], writes=[B])
    cst["cmask"] = c.sb(st, (128, 128), F32, "cmask"); c.dma("sp", out=cst["cmask"][:], in_=cd["cmask"], writes=[B])
    cst["rmask"] = c.sb(st, (4, S), F32, "rmask"); c.dma("sp", out=cst["rmask"][:], in_=cd["rmask"], writes=[B])
    cst["bdmask"] = c.sb(st, (4, 128), F32, "bdmask"); c.dma("sp", out=cst["bdmask"][:], in_=cd["bdmask"], writes=[B])
    cst["ones4"] = c.sb(st, (4, 128), F32, "ones4"); c.dma("sp", out=cst["ones4"][:], in_=cd["ones4"], writes=[B])
    cst["one1"] = c.sb(st, (128, 1), F32, "one1"); c.op("dve", lambda e: e.memset(cst["one1"][:], 1.0), writes=[B])
    return cst


def mlstm_const_arrays():
    t = np.arange(128)
    cmask = (t[None, :] >= t[:, None]).astype(np.float32)
    rmask = np.ones((4, S), np.float32); rmask[:, ::128] = 0.0
    bd = np.zeros((4, 4, 32), np.float32)
    for h in range(4):
        bd[h, h] = 1.0
    return {"identf": np.eye(128, dtype=np.float32), "cmask": cmask, "rmask": rmask, "bdmask": bd.reshape(4, 128), "ones4": np.ones((4, 128), np.float32)}


def ffn_up(c, w_up, gcol, fcp, hT, BhT, hffT, Bhff):
    wv = w_up.rearrange("(kc p) n -> p kc n", p=128)
    with ExitStack() as st:
        ws = [c.sb(st, (128, 8, 128), F32, "ws") for _ in range(2)]
        wb = [c.sb(st, (128, 8, 128), BF16, "wb") for _ in range(2)]
        u = [[c.sb(st, (128, 2 + S), BF16, "u") for _ in range(2)] for _ in range(2)]
        cv = [c.sb(st, (128, S), F32, "cv") for _ in range(2)]
        gg = c.sb(st, (128, S), BF16, "gg")
        ho = [c.sb(st, (128, S), BF16, "ho") for _ in range(2)]
        pss = [c.ps(st) for _ in range(4)]
        Bws = bufs(2); Bwb = [bufs(8) for _ in range(2)]; Bu = [[bufs(NTT) for _ in range(2)] for _ in range(2)]
        Bcv = bufs(2); Bgg = Buf(); Bho = bufs(2); Bps = bufs(4)
        for par in range(2):
            for half in range(2):
                c.op("pool", lambda e: e.memset(u[par][half][:, 0:2], 0.0), writes=Bu[par][half])
        k = 0

        def ld_w(jj, half):
            ci_ = jj + NFF * half
            c.dma("sp", out=ws[half][:], in_=wv[:, :, ci_ * 128:(ci_ + 1) * 128], writes=[Bws[half]])

        ld_w(0, 0); ld_w(0, 1)
        for j in range(NFF):
            par = j % 2
            for half in range(2):
                ci = j + NFF * half
                b = half
                c.op("pool", lambda e: e.tensor_tensor(out=wb[b][:], in0=ws[b][:], in1=gcol.unsqueeze(2).to_broadcast([128, 8, 128]), op=ALU.mult),
                     reads=[Bws[b]], writes=Bwb[b])
                for tt in range(NTT):
                    sl = slice(tt * TT, (tt + 1) * TT)
                    p = k % 4
                    for kc in range(8):
                        c.op("pe", lambda e: e.matmul(pss[p][:], lhsT=wb[b][:, kc, :], rhs=hT[:, kc, sl], start=(kc == 0), stop=(kc == 7)),
                             reads=[Bwb[b][kc], BhT[tt][kc]], writes=[Bps[p]] if kc == 0 else [], inc=(kc == 7))
                    Bps[p].w = (c.esem["pe"], c.cnt["pe"], "pe")
                    o_ap = u[par][half][:, 2 + tt * TT: 2 + (tt + 1) * TT]
                    c.op("act", lambda e: e.activation(out=o_ap, in_=pss[p][:], func=AF.Copy), reads=[Bps[p]], writes=[Bu[par][half][tt]])
                    k += 1
                if j + 1 < NFF:
                    ld_w(j + 1, half)
            for half, en in ((0, "dve"), (1, "dve")):
                ci = j + NFF * half
                uu = u[par][half]
                c.op(en, lambda e: e.tensor_scalar(out=cv[half][:], in0=uu[:, 2:2 + S], scalar1=fcp[:, ci, 2:3], scalar2=fcp[:, ci, 3:4], op0=ALU.mult, op1=ALU.add),
                     reads=Bu[par][half], writes=[Bcv[half]])
                c.op(en, lambda e: e.scalar_tensor_tensor(out=cv[half][:], in0=uu[:, 1:1 + S], scalar=fcp[:, ci, 1:2], in1=cv[half][:], op0=ALU.mult, op1=ALU.add),
                     reads=Bu[par][half] + [Bcv[half]], writes=[Bcv[half]])
                c.op(en, lambda e: e.scalar_tensor_tensor(out=cv[half][:], in0=uu[:, 0:S], scalar=fcp[:, ci, 0:1], in1=cv[half][:], op0=ALU.mult, op1=ALU.add),
                     reads=Bu[par][half] + [Bcv[half]], writes=[Bcv[half]])
            c.op("act", lambda e: e.activation(out=gg[:], in_=cv[1][:], func=AF.Gelu_apprx_tanh), reads=[Bcv[1]], writes=[Bgg])
            c.op("dve", lambda e: e.tensor_tensor(out=ho[par][:], in0=gg[:], in1=cv[0][:], op=ALU.mult), reads=[Bgg, Bcv[0]], writes=[Bho[par]])
            c.dma("sp", out=hffT[j * 128:(j + 1) * 128, :], in_=ho[par][:], reads=[Bho[par]], writes=[Bhff[j]])
    c.barrier()


def postnorm_residual(c, st_bufs, hsrc, Bh, xt, Bx, gcol, cst, tag):
    sq, Bsq, ps, Bps, rs, Brs, tmp, Btmp = st_bufs
    for oc in range(8):
        c.op("act", lambda e: e.activation(out=sq[:, oc, :], in_=hsrc[:, oc, :], func=AF.Square), reads=[Bh[oc]], writes=[Bsq[oc]])
    for oc in range(8):
        c.op("pe", lambda e: e.matmul(ps[:], lhsT=cst["avgD"][:], rhs=sq[:, oc, :], start=(oc == 0), stop=(oc == 7)),
             reads=[Bsq[oc]], writes=[Bps] if oc == 0 else [], inc=(oc == 7))
    Bps.w = (c.esem["pe"], c.cnt["pe"], "pe")
    c.op("act", lambda e: e.activation(out=rs[:], in_=ps[:], func=AF.Sqrt, bias=cst["epsc"][:, 0:1]), reads=[Bps], writes=[Brs])
    c.op("dve", lambda e: e.reciprocal(out=rs[:], in_=rs[:]), reads=[Brs], writes=[Brs])
    for oc in range(8):
        en = "dve" if oc % 2 == 0 else "pool"
        c.op(en, lambda e: e.tensor_tensor(out=tmp[:, oc, :], in0=hsrc[:, oc, :], in1=rs[:], op=ALU.mult), reads=[Bh[oc], Brs], writes=[Btmp[oc]])
        c.op("dve", lambda e: e.scalar_tensor_tensor(out=xt[:, oc, :], in0=tmp[:, oc, :], scalar=gcol[:, oc:oc + 1], in1=xt[:, oc, :], op0=ALU.mult, op1=ALU.add),
             reads=[Btmp[oc], Bx[oc]], writes=[Bx[oc]])


def ffn_down(c, w_down, gpost, hffT, Bhff, xT, cst):
    wv = w_down.rearrange("(c p) n -> p c n", p=128)
    hv = hffT.rearrange("(c p) t -> p c t", p=128)
    xv = xT.rearrange("(kc p) t -> p kc t", p=128)
    with ExitStack() as st:
        wd = c.sb(st, (128, NFF, D), BF16, "wd")
        wst = [c.sb(st, (128, NFF, 128), F32, "wst") for _ in range(2)]
        hf = [c.sb(st, (128, NFF, TT), BF16, "hf") for _ in range(2)]
        xt = [c.sb(st, (128, 8, TT), F32, "xt") for _ in range(2)]
        hd = c.sb(st, (128, 8, TT), F32, "hd")
        sq = c.sb(st, (128, 8, TT), BF16, "sq"); rs = c.sb(st, (128, TT), F32, "rs")
        pss = [c.ps(st) for _ in range(3)]
        psn = c.ps(st)
        Bwd = bufs(8); Bwst = bufs(2); Bhf = bufs(2); Bxt = [bufs(8) for _ in range(2)]; Bhd = bufs(8)
        Bps = bufs(3)
        sb_ = (sq, bufs(8), psn, Buf(), rs, Buf(), hd, Bhd)
        for oc in range(8):
            b = oc % 2
            c.dma("sp", out=wst[b][:], in_=wv[:, :, oc * 128:(oc + 1) * 128], writes=[Bwst[b]])
            c.op("pool", lambda e: e.tensor_copy(out=wd[:, :, oc * 128:(oc + 1) * 128], in_=wst[b][:]), reads=[Bwst[b]], writes=[Bwd[oc]])
        k = 0

        def ld_t(t_):
            b_ = t_ % 2
            sl_ = slice(t_ * TT, (t_ + 1) * TT)
            c.dma("sp", out=hf[b_][:], in_=hv[:, :, sl_], reads=Bhff, writes=[Bhf[b_]])
            c.dma("sp", out=xt[b_][:], in_=xv[:, :, sl_], writes=Bxt[b_])

        ld_t(0)
        for tt in range(NTT):
            b = tt % 2
            sl = slice(tt * TT, (tt + 1) * TT)
            for oc in range(8):
                p = k % 3; k += 1
                for fc in range(NFF):
                    c.op("pe", lambda e: e.matmul(pss[p][:], lhsT=wd[:, fc, oc * 128:(oc + 1) * 128], rhs=hf[b][:, fc, :], start=(fc == 0), stop=(fc == NFF - 1)),
                         reads=[Bwd[oc], Bhf[b]], writes=[Bps[p]] if fc == 0 else [], inc=(fc == NFF - 1))
                Bps[p].w = (c.esem["pe"], c.cnt["pe"], "pe")
                c.op("dve", lambda e: e.tensor_copy(out=hd[:, oc, :], in_=pss[p][:]), reads=[Bps[p]], writes=[Bhd[oc]])
            postnorm_residual(c, sb_, hd, Bhd, xt[b], Bxt[b], gpost, cst, "ffn")
            if tt + 1 < NTT:
                ld_t(tt + 1)
            c.dma("sp", out=xv[:, :, sl], in_=xt[b][:], reads=Bxt[b])
    c.barrier()


def token_weights_alloc(c, st):
    tw = {}
    tw["wres"] = {n: c.sb(st, (128, 8, D), BF16, n) for n in ("w_out", "wq", "wo")}
    tw["Bwres"] = {n: bufs(8) for n in tw["wres"]}
    tw["ws"] = [c.sb(st, (128, 8, 128), F32, "ws") for _ in range(2)]
    tw["Bws"] = bufs(2)
    tw["kw"] = 0
    return tw


def token_weights_issue(c, tw, W, prm):
    wres, Bwres, ws, Bws = tw["wres"], tw["Bwres"], tw["ws"], tw["Bws"]
    kw = tw["kw"]
    for name, g in (("w_out", prm["gmix"]), ("wq", prm["pre_mem"]), ("wo", None)):
        wv_ = W[name].rearrange("(kc p) n -> p kc n", p=128)
        for oc in range(8):
            b = kw % 2; kw += 1
            c.dma("sp", out=ws[b][:], in_=wv_[:, :, oc * 128:(oc + 1) * 128], writes=[Bws[b]])
            if g is None:
                c.op("pool", lambda e: e.tensor_copy(out=wres[name][:, :, oc * 128:(oc + 1) * 128], in_=ws[b][:]), reads=[Bws[b]], writes=[Bwres[name][oc]])
            else:
                c.op("pool", lambda e: e.tensor_tensor(out=wres[name][:, :, oc * 128:(oc + 1) * 128], in0=ws[b][:], in1=g.unsqueeze(2).to_broadcast([128, 8, 128]), op=ALU.mult),
                     reads=[Bws[b]], writes=[Bwres[name][oc]])
    tw["kw"] = kw


def token_phase(c, l, W, prm, mixedT, Bmixed, xT, memT, cst, tw=None):
    mv = mixedT.rearrange("(kc p) t -> p kc t", p=128)
    xv = xT.rearrange("(kc p) t -> p kc t", p=128)
    with ExitStack() as st:
        preloaded = tw is not None
        if tw is None:
            tw = token_weights_alloc(c, st)
        wres, Bwres, ws, Bws = tw["wres"], tw["Bwres"], tw["ws"], tw["Bws"]
        KT = c.sb(st, (128, 8, NMEM), BF16, "KT"); V = c.sb(st, (128, 2, D), BF16, "V"); mem = c.sb(st, (128, 8, NMEM), BF16, "mem")
        BKT = bufs(8); BV = bufs(8); Bmem = Buf()
        wtmp = [c.sb(st, (128, 8, 128), BF16, "wtmp") for _ in range(2)]; Bwtmp = bufs(2)
        mx = c.sb(st, (128, 8, TT), BF16, "mx"); Bmx = bufs(8)
        sq = c.sb(st, (128, 8, TT), BF16, "sq"); Bsq = bufs(8)
        hd = c.sb(st, (128, 8, TT), F32, "hd"); Bhd = bufs(8)
        xt = [c.sb(st, (128, 8, TT), F32, "xt") for _ in range(2)]; Bxt = [bufs(8) for _ in range(2)]
        qT = c.sb(st, (128, 8, TT), BF16, "qT"); BqT = bufs(8)
        on = c.sb(st, (128, 8, TT), BF16, "on"); Bon = bufs(8)
        P = [c.sb(st, (128, TT), BF16, "P") for _ in range(2)]; BP = bufs(2)
        rs = c.sb(st, (128, TT), F32, "rs"); Brs = Buf()
        rA = c.sb(st, (128, TT), F32, "rA"); BrA = Buf()
        rM = c.sb(st, (128, TT), F32, "rM"); BrM = Buf()
        rden = c.sb(st, (128, TT), F32, "rden"); Brden = Buf()
        NPS = 7
        pss = [c.ps(st) for _ in range(NPS)]; Bps = bufs(NPS)
        psn = c.ps(st); Bpsn = Buf()
        pk = [0]

        def nextps():
            i = pk[0] % NPS; pk[0] += 1
            return pss[i], Bps[i]

        def mm_group(ps, Bp, parts, reads_list):
            n = len(parts)
            for i, (lh, rh) in enumerate(parts):
                c.op("pe", lambda e: e.matmul(ps, lhsT=lh, rhs=rh, start=(i == 0), stop=(i == n - 1)),
                     reads=reads_list[i], writes=[Bp] if i == 0 else [], inc=(i == n - 1))
            Bp.w = (c.esem["pe"], c.cnt["pe"], "pe")

        with ExitStack() as s0:
            mf = c.sb(s0, (128, 8, NMEM), F32, "mf"); Bmf = Buf()
            c.dma("sp", out=mf[:], in_=memT.rearrange("(kc p) m -> p kc m", p=128), writes=[Bmf])
            c.op("pool", lambda e: e.tensor_copy(out=mem[:], in_=mf[:]), reads=[Bmf], writes=[Bmem])
            c.barrier()
        if not preloaded:
            token_weights_issue(c, tw, W, prm)
        kw = tw["kw"]
        for name in ("wk", "wv"):
            wv_ = W[name].rearrange("(kc p) n -> p kc n", p=128)
            for oc in range(8):
                b = kw % 2; kw += 1
                c.dma("sp", out=ws[b][:], in_=wv_[:, :, oc * 128:(oc + 1) * 128], writes=[Bws[b]])
                c.op("pool", lambda e: e.tensor_copy(out=wtmp[b][:], in_=ws[b][:]), reads=[Bws[b]], writes=[Bwtmp[b]])
                if name == "wk":
                    ps, Bp = nextps()
                    mm_group(ps[:, :NMEM], Bp, [(wtmp[b][:, kc, :], mem[:, kc, :]) for kc in range(8)], [[Bwtmp[b], Bmem]] * 8)
                    c.op("dve", lambda e: e.tensor_copy(out=KT[:, oc, :], in_=ps[:, :NMEM]), reads=[Bp], writes=[BKT[oc]])
                else:
                    for mb in range(2):
                        ps, Bp = nextps()
                        mm_group(ps[:, :128], Bp, [(mem[:, kc, mb * 128:(mb + 1) * 128], wtmp[b][:, kc, :]) for kc in range(8)], [[Bwtmp[b], Bmem]] * 8)
                        c.op("dve", lambda e: e.tensor_copy(out=V[:, mb, oc * 128:(oc + 1) * 128], in_=ps[:, :128]), reads=[Bp], writes=[BV[oc]])
        sb_ = (sq, Bsq, psn, Bpsn, rs, Brs, hd, Bhd)
        ek = 0
        def ld_t(t_):
            sl_ = slice(t_ * TT, (t_ + 1) * TT)
            c.dma("sp", out=mx[:], in_=mv[:, :, sl_], reads=Bmixed, writes=Bmx)
            c.dma("sp", out=xt[t_ % 2][:], in_=xv[:, :, sl_], writes=Bxt[t_ % 2])

        ld_t(0)
        for tt in range(NTT):
            b = tt % 2
            sl = slice(tt * TT, (tt + 1) * TT)
            for kc in range(8):
                c.op("act", lambda e: e.activation(out=sq[:, kc, :], in_=mx[:, kc, :], func=AF.Square), reads=[Bmx[kc]], writes=[Bsq[kc]])
            for grp, (rr, Brr) in enumerate(((rA, BrA), (rM, BrM))):
                ps, Bp = nextps()
                mm_group(ps[:], Bp, [(cst["avgH"][:], sq[:, grp * 4 + i, :]) for i in range(4)], [[Bsq[grp * 4 + i]] for i in range(4)])
                c.op("act", lambda e: e.activation(out=rr[:], in_=ps[:], func=AF.Sqrt, bias=cst["epsc"][:, 0:1]), reads=[Bp], writes=[Brr])
                c.op("dve", lambda e: e.reciprocal(out=rr[:], in_=rr[:]), reads=[Brr], writes=[Brr])
            for kc in range(8):
                rr, Brr = (rA, BrA) if kc < 4 else (rM, BrM)
                en = "dve" if kc % 2 == 0 else "pool"
                c.op(en, lambda e: e.tensor_tensor(out=mx[:, kc, :], in0=mx[:, kc, :], in1=rr[:], op=ALU.mult), reads=[Bmx[kc], Brr], writes=[Bmx[kc]])

            def proj(wname, src, Bsrc, evac):
                nonlocal ek
                for oc in range(8):
                    ps, Bp = nextps()
                    mm_group(ps[:], Bp, [(wres[wname][:, kc, oc * 128:(oc + 1) * 128], src[:, kc, :]) for kc in range(8)],
                             [[Bwres[wname][oc], Bsrc[kc]] for kc in range(8)])
                    evac(oc, ps, Bp)

            def evac_hd(oc, ps, Bp):
                nonlocal ek
                ek += 1
                if ek % 2 == 0:
                    c.op("dve", lambda e: e.tensor_copy(out=hd[:, oc, :], in_=ps[:]), reads=[Bp], writes=[Bhd[oc]])
                else:
                    c.op("act", lambda e: e.activation(out=hd[:, oc, :], in_=ps[:], func=AF.Copy), reads=[Bp], writes=[Bhd[oc]])

            proj("w_out", mx, Bmx, evac_hd)
            postnorm_residual(c, sb_, hd, Bhd, xt[b], Bxt[b], prm["post_mix"], cst, "mix")
            for kc in range(8):
                c.op("act", lambda e: e.activation(out=sq[:, kc, :], in_=xt[b][:, kc, :], func=AF.Square), reads=[Bxt[b][kc]], writes=[Bsq[kc]])
            mm_group(psn[:], Bpsn, [(cst["avgD"][:], sq[:, kc, :]) for kc in range(8)], [[Bsq[kc]] for kc in range(8)])
            c.op("act", lambda e: e.activation(out=rs[:], in_=psn[:], func=AF.Sqrt, bias=cst["epsc"][:, 0:1]), reads=[Bpsn], writes=[Brs])
            c.op("dve", lambda e: e.reciprocal(out=rs[:], in_=rs[:]), reads=[Brs], writes=[Brs])
            for kc in range(8):
                en = "dve" if kc % 2 == 0 else "pool"
                c.op(en, lambda e: e.tensor_tensor(out=mx[:, kc, :], in0=xt[b][:, kc, :], in1=rs[:], op=ALU.mult), reads=[Bxt[b][kc], Brs], writes=[Bmx[kc]])

            def evac_q(oc, ps, Bp):
                nonlocal ek
                ek += 1
                if ek % 2 == 0:
                    c.op("dve", lambda e: e.tensor_scalar(out=qT[:, oc, :], in0=ps[:], scalar1=1.0 / 16, scalar2=None, op0=ALU.mult), reads=[Bp], writes=[BqT[oc]])
                else:
                    c.op("act", lambda e: e.activation(out=qT[:, oc, :], in_=ps[:], func=AF.Copy, scale=1.0 / 16), reads=[Bp], writes=[BqT[oc]])

            proj("wq", mx, Bmx, evac_q)
            for xh in range(4):
                for mb in range(2):
                    ps, Bp = nextps()
                    mm_group(ps[:], Bp, [(KT[:, 2 * xh + dc, mb * 128:(mb + 1) * 128], qT[:, 2 * xh + dc, :]) for dc in range(2)],
                             [[BKT[2 * xh + dc], BqT[2 * xh + dc]] for dc in range(2)])
                    c.op("act", lambda e: e.activation(out=P[mb][:], in_=ps[:], func=AF.Exp), reads=[Bp], writes=[BP[mb]])
                ps, Bp = nextps()
                mm_group(ps[:], Bp, [(cst["ones"][:], P[mb][:]) for mb in range(2)], [[BP[mb]] for mb in range(2)])
                c.op("dve", lambda e: e.reciprocal(out=rden[:], in_=ps[:]), reads=[Bp], writes=[Brden])
                for dc in range(2):
                    oc = 2 * xh + dc
                    ps, Bp = nextps()
                    mm_group(ps[:], Bp, [(V[:, mb, oc * 128:(oc + 1) * 128], P[mb][:]) for mb in range(2)], [[BV[oc], BP[mb]] for mb in range(2)])
                    c.op("dve", lambda e: e.tensor_tensor(out=on[:, oc, :], in0=ps[:], in1=rden[:], op=ALU.mult), reads=[Bp, Brden], writes=[Bon[oc]])
            proj("wo", on, Bon, evac_hd)
            postnorm_residual(c, sb_, hd, Bhd, xt[b], Bxt[b], prm["post_mem"], cst, "mem")
            if tt + 1 < NTT:
                ld_t(tt + 1)
            c.dma("sp", out=xv[:, :, sl], in_=xt[b][:], reads=Bxt[b])
    c.barrier()


NPAR = 272


def build_program(nl=NL, debug=False):
    nc = bass.Bass("TRN2", target_bir_lowering=False)
    di = lambda n, shp: nc.dram_tensor(n, list(shp), F32, kind="ExternalInput").ap()
    xin = di("xin", (D, S)); memT = di("memT", (D, NMEM))
    w_in = di("w_in", (NL, D, IN_COLS)); w_out = di("w_out", (NL, D, D))
    wq = di("wq_mem", (NL, D, D)); wk = di("wk_mem", (NL, D, D)); wv = di("wv_mem", (NL, D, D)); wo = di("wo_mem", (NL, D, D))
    w_up = di("w_up", (NL, D, 2 * DFF)); w_down = di("w_down", (NL, DFF, D))
    par = di("par", (128, NL, NPAR)); gb = di("gb", (4, NL, 2))
    cin = di("cin", (128, 3, 128)); tabd = di("tab", (128, 24, 256)); seld = di("sel65", (65, 64))
    ca = mlstm_const_arrays()
    cd = {k: di(k, v.shape) for k, v in ca.items()}
    yT = nc.dram_tensor("yT", [D, S], F32, kind="ExternalOutput").ap()
    sk = "ExternalOutput" if debug else "Internal"
    projT = nc.dram_tensor("projT", [3584, S], BF16, kind=sk).ap()
    gatesT = nc.dram_tensor("gatesT", [8, S], F32, kind=sk).ap()
    mixedT = nc.dram_tensor("mixedT", [D, S], BF16, kind=sk).ap()
    hffT = nc.dram_tensor("hffT", [DFF, S], BF16, kind=sk).ap()
    with ExitStack() as es:
        c = Ctx(nc, es)
        cst = mlstm_consts(c, es, cd)
        B = Buf()
        cc = c.sb(es, (128, 3, 128), BF16, "cc")
        for i in range(3):
            c.dma("pool", out=cc[:, i, :], in_=cin[:, i, :], writes=[B])
        cst["avgD"] = cc[:, 0, :]; cst["avgH"] = cc[:, 1, :]; cst["ones"] = cc[:, 2, :]
        cst["epsc"] = c.sb(es, (128, 1), F32, "epsc"); c.op("dve", lambda e: e.memset(cst["epsc"][:], EPS), writes=[B])
        cst["sel65"] = c.sb(es, (65, 64), F32, "sel65"); c.dma("sp", out=cst["sel65"][:], in_=seld, writes=[B])
        cst["tab"] = c.sb(es, (128, 24, 256), BF16, "tab")
        with ExitStack() as s0:
            tf = c.sb(s0, (128, 24, 256), F32, "tabf"); Bt = Buf()
            c.dma("sp", out=tf[:], in_=tabd, writes=[Bt])
            c.op("pool", lambda e: e.tensor_copy(out=cst["tab"][:], in_=tf[:]), reads=[Bt], writes=[B])
            c.barrier()
        pt = c.sb(es, (128, NL, NPAR), F32, "par"); c.dma("sp", out=pt[:], in_=par, writes=[B])
        gbt = c.sb(es, (4, NL, 2), F32, "gb"); c.dma("sp", out=gbt[:], in_=gb, writes=[B])
        c.dma("sp", out=yT, in_=xin, writes=[B])
        c.barrier()
        for l in range(nl):
            P = lambda a, b_: pt[:, l, a:b_]
            Bproj = bufs(29); Bmixed = bufs(12); Bhff = bufs(NFF)
            with ExitStack() as s1:
                hT = c.sb(s1, (128, 8, S), BF16, "hT")
                BhT = [bufs(8) for _ in range(NTT)]
                norm_pass(c, yT, hT, BhT, cst)
                inproj(c, w_in[l], P(0, 8), hT, BhT, projT, gatesT, Bproj)
            attention(c, projT, mixedT, cst, Bproj, Bmixed)
            W = {"w_out": w_out[l], "wq": wq[l], "wk": wk[l], "wv": wv[l], "wo": wo[l]}
            prm = {"gmix": P(8, 16), "post_mix": P(16, 24), "pre_mem": P(24, 32), "post_mem": P(32, 40)}
            with ExitStack() as s2:
                tw = token_weights_alloc(c, s2)
                mlstm(c, projT, gatesT, mixedT, Bproj, Bmixed, P(56, 96).rearrange("p (c f) -> p c f", f=5), gbt[:, l, :], cst,
                      hook=lambda: token_weights_issue(c, tw, W, prm))
                token_phase(c, l, W, prm, mixedT, Bmixed, yT, memT, cst, tw=tw)
            with ExitStack() as s1:
                hT = c.sb(s1, (128, 8, S), BF16, "hT")
                BhT = [bufs(8) for _ in range(NTT)]
                norm_pass(c, yT, hT, BhT, cst)
                ffn_up(c, w_up[l], P(40, 48), P(96, 272).rearrange("p (c f) -> p c f", f=4), hT, BhT, hffT, Bhff)
            ffn_down(c, w_down[l], P(48, 56), hffT, Bhff, yT, cst)
        c.barrier()
    return nc


def host_inputs(inp):
    col = lambda v: np.asarray(v, np.float32).reshape(-1, 128).T
    par = np.zeros((128, NL, NPAR), np.float32)
    gb = np.zeros((4, NL, 2), np.float32)
    for l in range(NL):
        par[:, l, 0:8] = col(inp["pre_mix_g"][l])
        par[:, l, 8:16] = col(np.concatenate([inp["attn_out_g"][l], inp["mlstm_out_g"][l]]))
        par[:, l, 16:24] = col(inp["post_mix_g"][l]); par[:, l, 24:32] = col(inp["pre_mem_g"][l]); par[:, l, 32:40] = col(inp["post_mem_g"][l])
        par[:, l, 40:48] = col(inp["pre_ffn_g"][l]); par[:, l, 48:56] = col(inp["post_ffn_g"][l])
        mc = np.concatenate([inp["mconv_w"][l], inp["mconv_b"][l][None]], 0)
        par[:, l, 56:96] = mc.reshape(5, 8, 128).transpose(2, 1, 0).reshape(128, 40)
        fc = np.concatenate([inp["fconv_w"][l], inp["fconv_b"][l][None]], 0)
        par[:, l, 96:272] = fc.reshape(4, 44, 128).transpose(2, 1, 0).reshape(128, 176)
        gb[:, l, 0] = inp["b_igate"][l]; gb[:, l, 1] = inp["b_fgate"][l]
    sel = np.zeros((65, 64), np.float32); sel[64] = 1.0
    cin = np.stack([np.full((128, 128), 1 / 1024), np.full((128, 128), 1 / 512), np.ones((128, 128))], 1).astype(np.float32)
    shared = {"par": par, "gb": gb, "cin": cin, "tab": make_tables(np.asarray(inp["rel_bias"], np.float32)), "sel65": sel}
    shared.update(mlstm_const_arrays())
    for k in ("w_in", "w_out", "wq_mem", "wk_mem", "wv_mem", "wo_mem", "w_up", "w_down"):
        shared[k] = np.ascontiguousarray(inp[k], dtype=np.float32)
    maps = []
    for b in range(4):
        m = dict(shared)
        m["xin"] = np.ascontiguousarray(np.asarray(inp["x"][b], np.float32).T)
        m["memT"] = np.ascontiguousarray(np.asarray(inp["mem"][b], np.float32).T)
        maps.append(m)
    return maps


def kernel(**inputs):
    nc = build_program(NL)
    maps = host_inputs(inputs)
    res = run_bass_kernel_spmd(nc, maps, core_ids=[0, 1, 2, 3])
    out = np.stack([np.ascontiguousarray(np.asarray(r["yT"]).T) for r in res.results], 0)
    return out.astype(np.float32)
```
